# Optimizing a Trainium2 kernel written in Bass

```python
import math
import jax
import jax.numpy as jnp
from jax import lax
import numpy as np


D_MODEL = 2048
BATCH = 4
SEQ = 4096
DEPTH = 2

GRID_W = 64
CTX_LEN = 256
N_EVEN = (DEPTH + 1) // 2
N_ODD = DEPTH // 2
N_MOD = 6
ALPHA = (2.0 * DEPTH) ** 0.25
BETA = (8.0 * DEPTH) ** -0.25
LN_EPS = 1e-5
NORM_EPS = 1e-6

NA_HEADS = 8
NA_DH = 128
NA_KR = 8
NA_KC = 16
NA_QC = 16
NA_SPAN = NA_QC + NA_KC
NA_W = NA_HEADS * NA_DH

GLA_HEADS = 4
GLA_DK = 128
GLA_DV = 256
GLA_RANK = 16
GLA_TAU = 16.0
GLA_CHUNK = 64
ROPE_BASE = 10000.0
GLA_WK = GLA_HEADS * GLA_DK
GLA_WV = GLA_HEADS * GLA_DV

EV_SPLITS = (NA_W, NA_W, NA_W, GLA_WK, GLA_WK, GLA_WV, GLA_WV, GLA_RANK, GLA_RANK)
EV_OFFSETS = tuple(int(v) for v in np.cumsum(EV_SPLITS)[:-1])
EV_IN = sum(EV_SPLITS)
EV_MIX = NA_W + GLA_WV

S5_W = D_MODEL // 2
S5_CH = 16
S5_G = S5_W // S5_CH
S5_P = 64

N_EXPERTS = 16
N_GROUPS = 4
TOP_K = 2
D_EXPERT = 1024

kernel_name = 'hybrid_na_gla_s5_moe_dit'


def _flip(a):
    return a[:, ::-1]


def split_heads(a, n):
    return a.reshape(a.shape[0], a.shape[1], n, -1)


def layer_norm(x, g, b):
    xf = x.astype(jnp.float32)
    mu = jnp.mean(xf, -1, keepdims=True)
    var = jnp.mean(jnp.square(xf - mu), -1, keepdims=True)
    return ((xf - mu) * lax.rsqrt(var + LN_EPS) * g + b).astype(x.dtype)


def axial_rope(x):
    L, dk = x.shape[1], x.shape[-1]
    half = dk // 2
    nf = half // 2
    t = jnp.arange(L)
    inv = ROPE_BASE ** (-jnp.arange(nf, dtype=jnp.float32) / nf)
    xf = x.astype(jnp.float32)

    def rot(xp, pos):
        ang = pos.astype(jnp.float32)[:, None] * inv
        cos, sin = jnp.cos(ang)[None, :, None, :], jnp.sin(ang)[None, :, None, :]
        x1, x2 = xp[..., :nf], xp[..., nf:]
        return jnp.concatenate([x1 * cos - x2 * sin, x1 * sin + x2 * cos], -1)

    out = jnp.concatenate([rot(xf[..., :half], t // GRID_W), rot(xf[..., half:], t % GRID_W)], -1)
    return out.astype(x.dtype)


def dense_attn(q, k, v):
    s = jnp.einsum('bqhd,bkhd->bhqk', q * q.shape[-1] ** -0.5, k).astype(jnp.float32)
    p = jax.nn.softmax(s, axis=-1).astype(v.dtype)
    return jnp.einsum('bhqk,bkhd->bqhd', p, v)


def na_latent(q, k, v, kc, vc, rpb):
    B, L, H, d = q.shape
    rows = L // GRID_W
    kr = min(NA_KR, rows)
    n_cb = GRID_W // NA_QC
    scale = d ** -0.5
    blk0 = np.clip(np.arange(n_cb) * NA_QC - NA_KC // 2, 0, GRID_W - NA_SPAN)
    key_cols = blk0[:, None] + np.arange(NA_SPAN)[None, :]
    q_cols = np.arange(n_cb)[:, None] * NA_QC + np.arange(NA_QC)[None, :]
    win0 = np.clip(q_cols - NA_KC // 2, 0, GRID_W - NA_KC)
    kcol = key_cols[:, None, :]
    col_ok = (kcol >= win0[:, :, None]) & (kcol < win0[:, :, None] + NA_KC)
    dc_idx = np.clip(kcol - q_cols[:, :, None] + NA_KC - 1, 0, 2 * NA_KC - 2)
    mask = np.broadcast_to(col_ok[:, :, None, :], (n_cb, NA_QC, kr, NA_SPAN)).reshape(n_cb, NA_QC, kr * NA_SPAN)

    def grid(a):
        return a.reshape(B, rows, GRID_W, H, d).transpose(0, 3, 1, 2, 4)

    qg, kg, vg = grid(q * scale), grid(k), grid(v)
    kct, vct = kc.transpose(0, 2, 1, 3), vc.transpose(0, 2, 1, 3)
    rpb32 = rpb.astype(jnp.float32)
    n_loc = kr * NA_SPAN

    def row_block(r):
        rs = jnp.clip(r - kr // 2, 0, rows - kr)
        q_blk = lax.dynamic_index_in_dim(qg, r, axis=2, keepdims=False).reshape(B, H, n_cb, NA_QC, d)

        def band(a):
            a = lax.dynamic_slice_in_dim(a, rs, kr, axis=2)[:, :, :, key_cols]
            return a.transpose(0, 1, 3, 2, 4, 5).reshape(B, H, n_cb, n_loc, d)

        k_blk, v_blk = band(kg), band(vg)
        dr_idx = rs + jnp.arange(kr) - r + NA_KR - 1
        bias = rpb32[:, dr_idx[:, None, None, None], dc_idx[None]]
        bias = bias.transpose(0, 2, 3, 1, 4).reshape(H, n_cb, NA_QC, n_loc)
        s_loc = jnp.einsum('bhnqd,bhnkd->bhnqk', q_blk, k_blk).astype(jnp.float32) + bias
        s_loc = jnp.where(mask, s_loc, -jnp.inf)
        s_ctx = jnp.einsum('bhnqd,bhkd->bhnqk', q_blk, kct).astype(jnp.float32)
        p = jax.nn.softmax(jnp.concatenate([s_loc, s_ctx], -1), axis=-1).astype(v.dtype)
        o = (jnp.einsum('bhnqk,bhnkd->bhnqd', p[..., :n_loc], v_blk)
             + jnp.einsum('bhnqk,bhkd->bhnqd', p[..., n_loc:], vct))
        return o.reshape(B, H, GRID_W, d)

    out = lax.map(row_block, jnp.arange(rows))
    return out.transpose(1, 0, 3, 2, 4).reshape(B, L, H, d)


def gla_chunked(q, k, v, g, s0):
    f32 = jnp.float32
    B, T, H, _ = q.shape
    n = T // GLA_CHUNK

    def to_chunks(a):
        return a.astype(f32).reshape(B, n, GLA_CHUNK, H, a.shape[-1]).transpose(1, 0, 3, 2, 4)

    causal = jnp.tril(jnp.ones((GLA_CHUNK, GLA_CHUNK), dtype=bool))

    def step(s, inp):
        qc, kc, vc, gc = inp
        b = jnp.cumsum(gc, axis=2)
        b_last = b[:, :, -1:, :]
        q_dec = qc * jnp.exp(b)
        att = jnp.where(causal, jnp.einsum('bhqd,bhkd->bhqk', q_dec, kc * jnp.exp(-b)), 0.0)
        o = jnp.einsum('bhqk,bhkv->bhqv', att, vc) + jnp.einsum('bhqd,bhdv->bhqv', q_dec, s)
        s_new = jnp.exp(b_last[:, :, 0, :])[..., None] * s + jnp.einsum('bhkd,bhkv->bhdv', kc * jnp.exp(b_last - b), vc)
        return s_new, o

    s_fin, o = lax.scan(step, s0.astype(f32), (to_chunks(q), to_chunks(k), to_chunks(v), to_chunks(g)))
    return o.transpose(1, 0, 3, 2, 4).reshape(B, T, H, -1), s_fin


def gla_gate_norm(o, r, g):
    of = o.astype(jnp.float32)
    of = of * lax.rsqrt(jnp.mean(jnp.square(of), -1, keepdims=True) + NORM_EPS) * g
    return (of.reshape(o.shape[0], o.shape[1], -1) * jax.nn.silu(r.astype(jnp.float32))).astype(r.dtype)


def even_mixer(h, hc, w_in, gate_w2, gate_b, rpb, norm_g, w_out, need_ctx):
    B, L, _ = h.shape
    q_a, k_a, v_a, q_b, k_b, v_b, r_b, lr_f, lr_b = jnp.split(h @ w_in, EV_OFFSETS, axis=-1)
    cq_a, ck_a, cv_a, cq_b, ck_b, cv_b, cr_b, clr_f, clr_b = jnp.split(hc @ w_in, EV_OFFSETS, axis=-1)
    ck_h, cv_h = split_heads(ck_a, NA_HEADS), split_heads(cv_a, NA_HEADS)
    a_lat = na_latent(split_heads(q_a, NA_HEADS), split_heads(k_a, NA_HEADS), split_heads(v_a, NA_HEADS), ck_h, cv_h, rpb)

    def log_gate(lr, dirn):
        z = (lr @ gate_w2[dirn] + gate_b[dirn]).astype(jnp.float32)
        return split_heads(jax.nn.log_sigmoid(z) / GLA_TAU, GLA_HEADS)

    gscale = GLA_DK ** -0.5
    zero = jnp.zeros((B, GLA_HEADS, GLA_DK, GLA_DV), jnp.float32)
    cq = split_heads(cq_b, GLA_HEADS) * gscale
    ck = split_heads(ck_b, GLA_HEADS)
    cv = split_heads(cv_b, GLA_HEADS)
    oc_f, st_f = gla_chunked(cq, ck, cv, log_gate(clr_f, 0), zero)
    oc_b, st_b = gla_chunked(_flip(cq), _flip(ck), _flip(cv), _flip(log_gate(clr_b, 1)), zero)
    q = axial_rope(split_heads(q_b, GLA_HEADS)) * gscale
    k = axial_rope(split_heads(k_b, GLA_HEADS))
    v = split_heads(v_b, GLA_HEADS)
    o_f, _ = gla_chunked(q, k, v, log_gate(lr_f, 0), st_f)
    o_b, _ = gla_chunked(_flip(q), _flip(k), _flip(v), _flip(log_gate(lr_b, 1)), st_b)
    b_lat = gla_gate_norm(o_f + _flip(o_b), r_b, norm_g)
    out = jnp.concatenate([a_lat.reshape(B, L, -1), b_lat], -1) @ w_out
    if not need_ctx:
        return out, None
    a_ctx = dense_attn(split_heads(cq_a, NA_HEADS), ck_h, cv_h)
    b_ctx = gla_gate_norm(oc_f + _flip(oc_b), cr_b, norm_g)
    out_c = jnp.concatenate([a_ctx.reshape(B, hc.shape[1], -1), b_ctx], -1) @ w_out
    return out, out_c


def zoh(lam_re, lam_im, log_dt, b_re, b_im):
    f32 = jnp.float32
    lr, li = lam_re.astype(f32), lam_im.astype(f32)
    dt = jnp.exp(log_dt.astype(f32))[:, None]
    mag = jnp.exp(lr * dt)
    lb_re, lb_im = mag * jnp.cos(li * dt), mag * jnp.sin(li * dt)
    den = lr * lr + li * li
    fr = ((lb_re - 1.0) * lr + lb_im * li) / den
    fi = (lb_im * lr - (lb_re - 1.0) * li) / den
    br, bi = b_re.astype(f32), b_im.astype(f32)
    return lb_re, lb_im, fr[..., None] * br - fi[..., None] * bi, fr[..., None] * bi + fi[..., None] * br


def diag_scan(lb_re, lb_im, bu_re, bu_im, h0, reverse):
    if h0 is not None:
        h_re, h_im = h0
        end = -1 if reverse else 0
        bu_re = bu_re.at[:, end].add(lb_re * h_re - lb_im * h_im)
        bu_im = bu_im.at[:, end].add(lb_re * h_im + lb_im * h_re)
    a_re = jnp.broadcast_to(lb_re, bu_re.shape)
    a_im = jnp.broadcast_to(lb_im, bu_im.shape)

    def combine(e1, e2):
        a1r, a1i, b1r, b1i = e1
        a2r, a2i, b2r, b2i = e2
        return (a2r * a1r - a2i * a1i, a2r * a1i + a2i * a1r,
                a2r * b1r - a2i * b1i + b2r, a2r * b1i + a2i * b1r + b2i)

    _, _, x_re, x_im = lax.associative_scan(combine, (a_re, a_im, bu_re, bu_im), reverse=reverse, axis=1)
    return x_re, x_im


def s5_bidir(u, uc, lam_re, lam_im, log_dt, b_re, b_im, c_re, c_im, d_skip, need_ctx):
    f32 = jnp.float32
    B, L, W = u.shape

    def grp(a):
        return a.astype(f32).reshape(a.shape[0], a.shape[1], S5_G, S5_CH)

    ug, ucg = grp(u), grp(uc)
    dsk = d_skip.astype(f32)
    y = dsk * ug
    yc = dsk * ucg if need_ctx else None
    for dirn in range(2):
        rev = dirn == 1
        lb_re, lb_im, bb_re, bb_im = zoh(lam_re[dirn], lam_im[dirn], log_dt[dirn], b_re[dirn], b_im[dirn])
        cr, ci = c_re[dirn].astype(f32), c_im[dirn].astype(f32)
        xc_re, xc_im = diag_scan(lb_re, lb_im, jnp.einsum('btgc,gpc->btgp', ucg, bb_re),
                                 jnp.einsum('btgc,gpc->btgp', ucg, bb_im), None, rev)
        end = 0 if rev else -1
        x_re, x_im = diag_scan(lb_re, lb_im, jnp.einsum('btgc,gpc->btgp', ug, bb_re),
                               jnp.einsum('btgc,gpc->btgp', ug, bb_im), (xc_re[:, end], xc_im[:, end]), rev)
        y = y + jnp.einsum('btgp,gcp->btgc', x_re, cr) - jnp.einsum('btgp,gcp->btgc', x_im, ci)
        if need_ctx:
            yc = yc + jnp.einsum('btgp,gcp->btgc', xc_re, cr) - jnp.einsum('btgp,gcp->btgc', xc_im, ci)
    y = y.reshape(B, L, W).astype(u.dtype)
    if need_ctx:
        yc = yc.reshape(B, uc.shape[1], W).astype(u.dtype)
    return y, yc


def odd_mixer(h, hc, w_in, lam_re, lam_im, log_dt, b_re, b_im, c_re, c_im, d_skip, w_glu, b_glu, w_out, need_ctx):
    y, yc = s5_bidir(h @ w_in, hc @ w_in, lam_re, lam_im, log_dt, b_re, b_im, c_re, c_im, d_skip, need_ctx)

    def glu_out(t):
        g = jax.nn.gelu(t)
        return (g * jax.nn.sigmoid(g @ w_glu + b_glu)) @ w_out

    return glu_out(y), (glu_out(yc) if need_ctx else None)


def moe(t, router_w, router_b, w_gate, w_up, w_down):
    n = t.shape[0]
    eg = N_EXPERTS // N_GROUPS
    aff = jax.nn.sigmoid((t @ router_w).astype(jnp.float32))
    sel = aff + router_b.astype(jnp.float32)
    grp_score = lax.top_k(sel.reshape(n, N_GROUPS, eg), TOP_K)[0].sum(-1)
    grp = jnp.argmax(grp_score, axis=-1)
    in_grp = (jnp.arange(N_EXPERTS) // eg)[None, :] == grp[:, None]
    _, idx = lax.top_k(jnp.where(in_grp, sel, -jnp.inf), TOP_K)
    w = jnp.take_along_axis(aff, idx, axis=-1)
    w = w / jnp.sum(w, -1, keepdims=True)
    comb = jnp.sum(jax.nn.one_hot(idx, N_EXPERTS, dtype=jnp.float32) * w[..., None], axis=1).astype(t.dtype)
    y = jnp.zeros_like(t)
    for e in range(N_EXPERTS):
        hid = jax.nn.silu(t @ w_gate[e]) * (t @ w_up[e])
        y = y + comb[:, e:e + 1] * (hid @ w_down[e])
    return y


def setup_inputs(seed: int = 0) -> dict:
    key = jax.random.key(seed)
    ks = iter(jax.random.split(key, 48))

    def nrm(shape, std):
        return jax.random.normal(next(ks), shape, jnp.float32) * std

    D = D_MODEL
    p_idx = jnp.arange(S5_P, dtype=jnp.float32)
    return {
        'x': nrm((BATCH, SEQ, D), 1.0),
        'c': nrm((BATCH, D), 1.0),
        'ctx': nrm((BATCH, CTX_LEN, D), 1.0),
        'c_ctx': nrm((D,), 1.0),
        'ada_w': nrm((DEPTH, D, N_MOD * D), 0.5 * D ** -0.5),
        'ada_b': nrm((DEPTH, N_MOD * D), 0.02),
        'ln_mix_g': 1.0 + nrm((DEPTH, D), 0.02),
        'ln_mix_b': nrm((DEPTH, D), 0.02),
        'ln_ffn_g': 1.0 + nrm((DEPTH, D), 0.02),
        'ln_ffn_b': nrm((DEPTH, D), 0.02),
        'ev_w_in': nrm((N_EVEN, D, EV_IN), D ** -0.5),
        'ev_gate_w2': nrm((N_EVEN, 2, GLA_RANK, GLA_WK), GLA_RANK ** -0.5),
        'ev_gate_b': nrm((N_EVEN, 2, GLA_WK), 0.02),
        'ev_rpb': nrm((N_EVEN, NA_HEADS, 2 * NA_KR - 1, 2 * NA_KC - 1), 0.2),
        'ev_norm_g': 1.0 + nrm((N_EVEN, GLA_DV), 0.02),
        'ev_w_out': nrm((N_EVEN, EV_MIX, D), BETA * EV_MIX ** -0.5),
        'od_w_in': nrm((N_ODD, D, S5_W), D ** -0.5),
        'od_lam_re': -0.5 + nrm((N_ODD, 2, S5_G, S5_P), 0.01),
        'od_lam_im': math.pi * p_idx + nrm((N_ODD, 2, S5_G, S5_P), 0.01),
        'od_log_dt': jax.random.uniform(next(ks), (N_ODD, 2, S5_G), jnp.float32, math.log(1e-3), math.log(1e-1)),
        'od_b_re': nrm((N_ODD, 2, S5_G, S5_P, S5_CH), (2 * S5_CH) ** -0.5),
        'od_b_im': nrm((N_ODD, 2, S5_G, S5_P, S5_CH), (2 * S5_CH) ** -0.5),
        'od_c_re': nrm((N_ODD, 2, S5_G, S5_CH, S5_P), 0.5),
        'od_c_im': nrm((N_ODD, 2, S5_G, S5_CH, S5_P), 0.5),
        'od_d': nrm((N_ODD, S5_G, S5_CH), 0.5),
        'od_w_glu': nrm((N_ODD, S5_W, S5_W), S5_W ** -0.5),
        'od_b_glu': nrm((N_ODD, S5_W), 0.02),
        'od_w_out': nrm((N_ODD, S5_W, D), BETA * S5_W ** -0.5),
        'router_w': nrm((D, N_EXPERTS), D ** -0.5),
        'router_b': nrm((N_EXPERTS,), 0.01),
        'moe_w_gate': nrm((DEPTH, N_EXPERTS, D, D_EXPERT), D ** -0.5),
        'moe_w_up': nrm((DEPTH, N_EXPERTS, D, D_EXPERT), D ** -0.5),
        'moe_w_down': nrm((DEPTH, N_EXPERTS, D_EXPERT, D), BETA * D_EXPERT ** -0.5),
    }


def reference(x, c, ctx, c_ctx, ada_w, ada_b, ln_mix_g, ln_mix_b, ln_ffn_g, ln_ffn_b,
              ev_w_in, ev_gate_w2, ev_gate_b, ev_rpb, ev_norm_g, ev_w_out,
              od_w_in, od_lam_re, od_lam_im, od_log_dt, od_b_re, od_b_im, od_c_re, od_c_im, od_d,
              od_w_glu, od_b_glu, od_w_out, router_w, router_b, moe_w_gate, moe_w_up, moe_w_down):
    B, L, D = x.shape
    xc = ctx
    sc = jax.nn.silu(c)
    scc = jax.nn.silu(c_ctx)
    for layer in range(DEPTH):
        last = layer == DEPTH - 1
        m = jnp.split(sc @ ada_w[layer] + ada_b[layer], N_MOD, axis=-1)
        mc = jnp.split(scc @ ada_w[layer] + ada_b[layer], N_MOD, axis=-1)
        h = x * (1.0 + m[1][:, None]) + m[0][:, None]
        hc = xc * (1.0 + mc[1]) + mc[0]
        if layer % 2 == 0:
            i = layer // 2
            out, out_c = even_mixer(h, hc, ev_w_in[i], ev_gate_w2[i], ev_gate_b[i], ev_rpb[i],
                                    ev_norm_g[i], ev_w_out[i], not last)
        else:
            i = layer // 2
            out, out_c = odd_mixer(h, hc, od_w_in[i], od_lam_re[i], od_lam_im[i], od_log_dt[i],
                                   od_b_re[i], od_b_im[i], od_c_re[i], od_c_im[i], od_d[i],
                                   od_w_glu[i], od_b_glu[i], od_w_out[i], not last)
        x = layer_norm(ALPHA * x + m[2][:, None] * out, ln_mix_g[layer], ln_mix_b[layer])
        h = x * (1.0 + m[4][:, None]) + m[3][:, None]
        if last:
            y = moe(h.reshape(-1, D), router_w, router_b, moe_w_gate[layer], moe_w_up[layer],
                    moe_w_down[layer]).reshape(B, L, D)
        else:
            xc = layer_norm(ALPHA * xc + mc[2] * out_c, ln_mix_g[layer], ln_mix_b[layer])
            hc = xc * (1.0 + mc[4]) + mc[3]
            tokens = jnp.concatenate([h.reshape(-1, D), hc.reshape(-1, D)], axis=0)
            y_all = moe(tokens, router_w, router_b, moe_w_gate[layer], moe_w_up[layer], moe_w_down[layer])
            y = y_all[:B * L].reshape(B, L, D)
            y_c = y_all[B * L:].reshape(xc.shape)
            xc = layer_norm(ALPHA * xc + mc[5] * y_c, ln_ffn_g[layer], ln_ffn_b[layer])
        x = layer_norm(ALPHA * x + m[5][:, None] * y, ln_ffn_g[layer], ln_ffn_b[layer])
    return x
```

```python
import numpy as np
import concourse.bass as bass
import concourse.mybir as mybir
from concourse.bass_utils import run_bass_kernel_spmd
from contextlib import ExitStack

F32 = mybir.dt.float32
BF16 = mybir.dt.bfloat16
I32 = mybir.dt.int32
AF = mybir.ActivationFunctionType
ALU = mybir.AluOpType
AX = mybir.AxisListType


class Res:
    __slots__ = ("name", "w", "r")

    def __init__(self, name=""):
        self.name = name
        self.w = None
        self.r = {}


class TT:
    def __init__(self, t, res=None, name=""):
        self.t = t
        self.res = res if res is not None else Res(name)

    def __getitem__(self, idx):
        return self.t[idx]


class EngS:
    def __init__(self, name, obj, sem):
        self.name = name
        self.obj = obj
        self.sem = sem
        self.count = 0
        self.waited = {}


class K:
    def __init__(self, nc, es, n_dma_sems=12):
        self.nc = nc
        self.es = es
        self.sems = {}
        self.engs = {}
        for name, obj in (("pe", nc.tensor), ("act", nc.scalar), ("dve", nc.vector),
                          ("pool", nc.gpsimd), ("sp", nc.sync)):
            s = es.enter_context(nc.semaphore("s_" + name))
            self.sems[name] = s
            self.engs[name] = EngS(name, obj, s)
        self.dq = {}
        for q in ("sp", "pool", "act"):
            pool = []
            for i in range(n_dma_sems):
                key = "d_%s_%d" % (q, i)
                s = es.enter_context(nc.semaphore(key))
                self.sems[key] = s
                pool.append([key, 0])
            self.dq[q] = [pool, 0]
        self.nid = 0
        self.out_tokens = []

    def sb(self, name, shape, dtype):
        self.nid += 1
        name = "%s_%d" % (name, self.nid)
        t = self.es.enter_context(self.nc.sbuf_tensor(name, list(shape), dtype))
        return TT(t, name=name)

    def ps(self, name, shape, dtype=F32):
        self.nid += 1
        name = "%s_%d" % (name, self.nid)
        t = self.es.enter_context(self.nc.psum_tensor(name, list(shape), dtype))
        return TT(t, name=name)

    def dram(self, name, shape, dtype, kind="Internal"):
        t = self.nc.dram_tensor(name, list(shape), dtype, kind=kind)
        return TT(t, name=name)

    def _wait(self, E, key, val):
        if E.waited.get(key, 0) >= val:
            return
        E.obj.wait_ge(self.sems[key], val)
        E.waited[key] = val

    def _deps(self, E, reads, writes):
        toks = {}
        def add(tok):
            if tok is None:
                return
            k, v = tok
            if toks.get(k, 0) < v:
                toks[k] = v
        for r in reads:
            add(r.w)
        for w in writes:
            add(w.w)
            for k, v in w.r.items():
                add((k, v))
        for k, v in toks.items():
            if k == E.name and E.name == "pe":
                continue
            self._wait(E, k, v)

    def _mark(self, tok, reads, writes):
        k, v = tok
        for r in reads:
            if r.r.get(k, 0) < v:
                r.r[k] = v
        for w in writes:
            w.w = tok
            w.r = {}

    @staticmethod
    def _res(xs):
        out = []
        for x in xs:
            if x is None:
                continue
            out.append(x.res if isinstance(x, TT) else x)
        return out

    def op(self, eng, reads, writes, fn):
        E = self.engs[eng]
        reads = self._res(reads)
        writes = self._res(writes)
        self._deps(E, reads, writes)
        ins = fn(E.obj)
        E.count += 1
        ins.then_inc(E.sem, 1)
        self._mark((E.name, E.count), reads, writes)
        return ins

    def dma(self, q, out_ap, in_ap, reads, writes, is_output=False, **kw):
        E = self.engs[q]
        reads = self._res(reads)
        writes = self._res(writes)
        pool, idx = self.dq[q]
        slot = pool[idx % len(pool)]
        self.dq[q][1] = idx + 1
        key, cnt = slot
        if cnt > 0:
            self._wait(E, key, cnt)
        self._deps(E, reads, writes)
        ins = E.obj.dma_start(out=out_ap, in_=in_ap, **kw)
        slot[1] = cnt + 16
        ins.then_inc(self.sems[key], 16)
        tok = (key, cnt + 16)
        self._mark(tok, reads, writes)
        if is_output:
            self.out_tokens.append(tok)
        return tok

    def idma_gather(self, out_ap, in_ap, idx_ap, reads, writes):
        E = self.engs["pool"]
        reads = self._res(reads)
        writes = self._res(writes)
        pool, idx = self.dq["pool"]
        slot = pool[idx % len(pool)]
        self.dq["pool"][1] = idx + 1
        key, cnt = slot
        if cnt > 0:
            self._wait(E, key, cnt)
        self._deps(E, reads, writes)
        ins = E.obj.indirect_dma_start(out=out_ap, out_offset=None, in_=in_ap,
                                       in_offset=bass.IndirectOffsetOnAxis(ap=idx_ap, axis=0))
        slot[1] = cnt + 16
        ins.then_inc(self.sems[key], 16)
        self._mark((key, cnt + 16), reads, writes)

    def finish(self):
        E = self.engs["sp"]
        for q in self.dq:
            for key, cnt in self.dq[q][0]:
                if cnt > 0:
                    self._wait(E, key, cnt)
        for n in ("pe", "act", "dve", "pool"):
            En = self.engs[n]
            if En.count > 0:
                self._wait(E, n, En.count)

from contextlib import contextmanager

def barrier(k):
    names = ("pe", "act", "dve", "pool", "sp")
    for n in names:
        E = k.engs[n]
        for m in names:
            if m != n and k.engs[m].count > 0:
                k._wait(E, m, k.engs[m].count)
        for q in k.dq:
            for key, cnt in k.dq[q][0]:
                if cnt > 0:
                    k._wait(E, key, cnt)

@contextmanager
def phase(k):
    old = k.es
    with ExitStack() as es:
        k.es = es
        try:
            yield
        finally:
            barrier(k)
            k.es = old
K.phase = phase
K.barrier = barrier


D = 2048
NT = 4352
NTILE = 34
TBLKS = [(i * 512, 512) for i in range(8)] + [(4096, 256)]
ALPHA = (2.0 * 2) ** 0.25
LN_EPS = 1e-5


def phase_mod(k, layer, ada_w, ada_b, cT, modrow):
    with k.phase():
        cs = k.sb("cs", [128, 32], F32)
        sc = k.sb("sc", [128, 32], F32)
        ones = k.sb("ones1", [1, 128], F32)
        k.dma("sp", cs[:], cT[:], [cT], [cs])
        k.op("act", [cs], [sc], lambda e: e.activation(sc[:], cs[:], AF.Silu))
        k.op("dve", [], [ones], lambda e: e.memset(ones[:], 1.0))
        NB = 6
        wbuf = [k.sb("adw%d" % i, [128, 512], F32) for i in range(NB)]
        bbuf = [k.sb("adb%d" % i, [1, 512], F32) for i in range(2)]
        rbuf = [k.sb("adr%d" % i, [1, 1024], F32) for i in range(2)]
        pss = [k.ps("adp%d" % i, [1, 1024], F32) for i in range(2)]
        cnt = 0
        for nt in range(24):
            ps = pss[nt % 2]
            bb = bbuf[nt % 2]
            rb = rbuf[nt % 2]
            k.dma("sp", bb[:], ada_b[layer:layer + 1, nt * 512:(nt + 1) * 512], [ada_b], [bb])
            for j in range(16):
                wt = wbuf[cnt % NB]
                cnt += 1
                k.dma("sp", wt[:], ada_w[layer, j * 128:(j + 1) * 128, nt * 512:(nt + 1) * 512], [ada_w], [wt])
                k.op("pe", [sc, wt], [ps], lambda e: e.matmul(ps[0:1, 0:512], sc[:, j:j + 1], wt[:], start=(j == 0), stop=False))
                k.op("pe", [sc, wt], [ps], lambda e: e.matmul(ps[0:1, 512:1024], sc[:, 16 + j:17 + j], wt[:], start=(j == 0), stop=False))
            k.op("pe", [ones, bb], [ps], lambda e: e.matmul(ps[0:1, 0:512], ones[0:1, 0:1], bb[:], start=False, stop=True))
            k.op("pe", [ones, bb], [ps], lambda e: e.matmul(ps[0:1, 512:1024], ones[0:1, 0:1], bb[:], start=False, stop=True))
            seg = nt // 4
            addv = 1.0 if seg in (1, 4) else 0.0
            k.op("dve", [ps], [rb], lambda e: e.tensor_scalar_add(rb[:], ps[:], addv))
            k.dma("pool", modrow[layer, 0:1, nt * 512:(nt + 1) * 512], rb[0:1, 0:512], [rb], [modrow])
            k.dma("pool", modrow[layer, 1:2, nt * 512:(nt + 1) * 512], rb[0:1, 512:1024], [rb], [modrow])


def load_bc(k, q, dst, row_ap, deps):
    k.dma(q, dst[:], row_ap.partition_broadcast(128), deps, [dst])


def build_hT(k, layer, xcur, modrow, hT_all, ident_b, seg_shift, seg_scale):
    with k.phase():
        mA = k.sb("mA", [128, D], F32)
        mB = k.sb("mB", [128, D], F32)
        xt = [k.sb("xt%d" % i, [128, D], F32) for i in range(2)]
        hb = [k.sb("hb%d" % i, [128, D], BF16) for i in range(2)]
        ptr = [k.ps("ptr%d" % i, [128, D], BF16) for i in range(2)]
        for i in range(NTILE):
            if i == 0 or i == 32:
                r = 0 if i == 0 else 1
                load_bc(k, "sp", mA, modrow[layer, r:r + 1, seg_scale * D:(seg_scale + 1) * D], [modrow])
                load_bc(k, "sp", mB, modrow[layer, r:r + 1, seg_shift * D:(seg_shift + 1) * D], [modrow])
            x_ = xt[i % 2]
            h_ = hb[i % 2]
            p_ = ptr[i % 2]
            k.dma("sp", x_[:], xcur[i * 128:(i + 1) * 128, :], [xcur], [x_])
            k.op("dve", [x_, mA], [x_], lambda e: e.tensor_tensor(x_[:], x_[:], mA[:], ALU.mult))
            k.op("pool", [x_, mB], [h_], lambda e: e.tensor_tensor(h_[:], x_[:], mB[:], ALU.add))
            for j in range(16):
                k.op("pe", [h_, ident_b], [p_], lambda e: e.transpose(p_[:, j * 128:(j + 1) * 128], h_[:, j * 128:(j + 1) * 128], ident_b[:]))
            k.op("act", [p_], [hT_all], lambda e: e.activation(
                hT_all[:, :, i * 128:(i + 1) * 128], p_[:].rearrange("p (j t) -> p j t", j=16), AF.Copy))


def load_w_block(k, w_ap, stage, wb, ncols, conv_eng="pool"):
    k.dma("sp", stage[:, :, 0:ncols], w_ap.rearrange("(j p) c -> p j c", p=128), [], [stage])
    k.op("act", [stage], [wb], lambda e: e.activation(wb[:, :, 0:ncols], stage[:, :, 0:ncols], AF.Copy))


def phase_inproj0(k, hT_all, ev_w_in, S):
    with k.phase():
        stg = [k.sb("wst%d" % i, [128, 16, 128], F32) for i in range(2)]
        wbs = [k.sb("wbb%d" % i, [128, 16, 128], BF16) for i in range(2)]
        obuf_b = k.sb("obufb", [128, NT], BF16)
        obuf_f = k.sb("obuff", [128, NT], F32)
        pss = [k.ps("ipp%d" % i, [128, 512], F32) for i in range(4)]
        pc = 0
        for cb in range(49):
            st = stg[cb % 2]
            wb = wbs[cb % 2]
            ncols = 128 if cb < 48 else 32
            load_w_block(k, ev_w_in[:, cb * 128:cb * 128 + ncols], st, wb, ncols)
            if cb < 16:
                fo = obuf_b
                for (t0, tn) in TBLKS:
                    ps = pss[pc % 4]; pc += 1
                    for j in range(16):
                        k.op("pe", [wb, hT_all], [ps], lambda e: e.matmul(ps[:, 0:tn], wb[:, j, :], hT_all[:, j, t0:t0 + tn], start=(j == 0), stop=(j == 15)))
                    k.op("act", [ps], [fo], lambda e: e.activation(fo[:, t0:t0 + tn], ps[:, 0:tn], AF.Copy))
                dst = S["qaT"] if cb < 8 else S["kaT"]
                k.dma("pool", dst[cb % 8], fo[:], [fo], [dst])
            elif cb < 48:
                isb = (16 <= cb < 24) or (32 <= cb < 40)
                tor = obuf_b if isb else obuf_f
                to = tor[:].rearrange("p (i c) -> p i c", c=128)
                for i4 in range(0, NTILE, 4):
                    n4 = min(4, NTILE - i4)
                    ps = pss[pc % 4]; pc += 1
                    for ii in range(n4):
                        i = i4 + ii
                        for j in range(16):
                            k.op("pe", [wb, hT_all], [ps], lambda e: e.matmul(ps[:, ii * 128:(ii + 1) * 128], hT_all[:, j, i * 128:(i + 1) * 128], wb[:, j, :], start=(j == 0), stop=(j == 15)))
                    eng = "act" if (i4 // 4) % 2 == 0 else "dve"
                    if eng == "act":
                        k.op("act", [ps], [tor], lambda e: e.activation(to[:, i4:i4 + n4, :], ps[:, 0:n4 * 128].rearrange("p (a b) -> p a b", a=n4), AF.Copy))
                    else:
                        k.op("dve", [ps], [tor], lambda e: e.tensor_copy(to[:, i4:i4 + n4, :], ps[:, 0:n4 * 128].rearrange("p (a b) -> p a b", a=n4)))
                if cb < 24:
                    dst, c0 = S["va"], (cb - 16) * 128
                elif cb < 28:
                    dst, c0 = S["qb"], (cb - 24) * 128
                elif cb < 32:
                    dst, c0 = S["kb"], (cb - 28) * 128
                elif cb < 40:
                    dst, c0 = S["vb"], (cb - 32) * 128
                else:
                    dst, c0 = S["rb"], (cb - 40) * 128
                k.dma("pool", dst[:, c0:c0 + 128].rearrange("(i p) c -> p i c", p=128), to, [tor], [dst])
            else:
                for d in range(2):
                    for (t0, tn) in TBLKS:
                        ps = pss[pc % 4]; pc += 1
                        for j in range(16):
                            k.op("pe", [wb, hT_all], [ps], lambda e: e.matmul(ps[0:16, 0:tn], wb[:, j, d * 16:(d + 1) * 16], hT_all[:, j, t0:t0 + tn], start=(j == 0), stop=(j == 15)))
                        k.op("act", [ps], [obuf_f], lambda e: e.activation(obuf_f[0:16, t0:t0 + tn], ps[0:16, 0:tn], AF.Copy))
                    k.dma("pool", S["lrT"][d], obuf_f[0:16, :], [obuf_f], [S["lrT"]])


def phase_na(k, S, biasg, idb):
    scale = 128 ** -0.5
    with k.phase():
        HB = 2
        qT = [k.sb("na_qT%d" % i, [128, NT], BF16) for i in range(HB)]
        kT = [k.sb("na_kT%d" % i, [128, NT], BF16) for i in range(HB)]
        Ve = [k.sb("na_Ve%d" % i, [128, 34, 128], BF16) for i in range(HB)]
        Vo = [k.sb("na_Vo%d" % i, [128, 31, 128], BF16) for i in range(HB)]
        bias = [k.sb("na_bias%d" % i, [64, 8, 512], F32) for i in range(HB)]
        aT = [k.sb("na_aT%d" % i, [128, NT], BF16) for i in range(HB)]
        DP = 4
        Ssb = [k.sb("na_S%d" % i, [128, 768], F32) for i in range(DP)]
        Pn = [k.sb("na_P%d" % i, [128, 768], BF16) for i in range(DP)]
        PT = [k.sb("na_PT%d" % i, [128, 768], BF16) for i in range(DP)]
        st = [k.sb("na_st%d" % i, [128, 4], F32) for i in range(DP)]
        ps_s = [k.ps("na_pss%d" % i, [128, 512], F32) for i in range(2)]
        ps_c = [k.ps("na_psc%d" % i, [128, 512], F32) for i in range(2)]
        ps_t = [k.ps("na_pst%d" % i, [128, 1024], BF16) for i in range(2)]
        ps_o = [k.ps("na_pso%d" % i, [128, 512], F32) for i in range(2)]
        va = S["va"]
        items = [(h, r) for h in range(8) for r in range(66)]
        NI = len(items)

        def geom(r):
            if r < 64:
                rs = min(max(r - 4, 0), 56)
                pat = r if r < 4 else (4 if r <= 60 else r - 56)
                return 64, r * 64, 768, rs, pat
            return 128, 4096 + (r - 64) * 128, 256, 0, 0

        def s1a(i):
            h, r = items[i]
            hb = h % HB
            if r == 0:
                k.dma("sp", qT[hb][:], S["qaT"][h], [S["qaT"]], [qT[hb]])
                k.dma("sp", kT[hb][:], S["kaT"][h], [S["kaT"]], [kT[hb]])
                k.dma("sp", Ve[hb][:], va[:, h * 128:(h + 1) * 128].rearrange("(c p) d -> p c d", p=128), [va], [Ve[hb]])
                k.dma("sp", Vo[hb][:], va[64:64 + 31 * 128, h * 128:(h + 1) * 128].rearrange("(c p) d -> p c d", p=128), [va], [Vo[hb]])
                k.dma("sp", bias[hb][:], biasg[h], [biasg], [bias[hb]])
            nq, q0, nk, rs, pat = geom(r)
            S_, st_ = Ssb[i % DP], st[i % DP]
            pss, psc = ps_s[i % 2], ps_c[i % 2]
            q_, k_ = qT[hb], kT[hb]
            if r < 64:
                k.op("pe", [q_, k_], [pss], lambda e: e.matmul(pss[0:64, :], q_[:, q0:q0 + 64], k_[:, rs * 64:rs * 64 + 512], start=True, stop=True))
                k.op("pe", [q_, k_], [psc], lambda e: e.matmul(psc[0:64, 0:256], q_[:, q0:q0 + 64], k_[:, 4096:4352], start=True, stop=True))
                k.op("dve", [pss, bias[hb]], [S_], lambda e: e.scalar_tensor_tensor(S_[0:64, 0:512], pss[0:64, :], scale, bias[hb][:, pat, :], ALU.mult, ALU.add))
                k.op("act", [psc], [S_], lambda e: e.activation(S_[0:64, 512:768], psc[0:64, 0:256], AF.Copy, scale=scale))
            else:
                k.op("pe", [q_, k_], [psc], lambda e: e.matmul(psc[:, 0:256], q_[:, q0:q0 + 128], k_[:, 4096:4352], start=True, stop=True))
                k.op("act", [psc], [S_], lambda e: e.activation(S_[:, 0:256], psc[:, 0:256], AF.Copy, scale=scale))
            k.op("dve", [S_], [st_], lambda e: e.reduce_max(st_[0:nq, 0:1], S_[0:nq, 0:nk], AX.X, negate=True))

        def s1b(i):
            h, r = items[i]
            nq, q0, nk, rs, pat = geom(r)
            S_, P_, st_ = Ssb[i % DP], Pn[i % DP], st[i % DP]
            k.op("act", [S_, st_], [P_, st_], lambda e: e.activation(P_[0:nq, 0:nk], S_[0:nq, 0:nk], AF.Exp, bias=st_[0:nq, 0:1], accum_out=st_[0:nq, 1:2]))
            k.op("dve", [st_], [st_], lambda e: e.reciprocal(st_[0:nq, 2:3], st_[0:nq, 1:2]))
            k.op("dve", [P_, st_], [P_], lambda e: e.tensor_scalar_mul(P_[0:nq, 0:nk], P_[0:nq, 0:nk], st_[0:nq, 2:3]))

        def s2(i):
            h, r = items[i]
            P_, PT_ = Pn[i % DP], PT[i % DP]
            pst = ps_t[i % 2]
            if r < 64:
                for c in range(6):
                    k.op("pe", [P_, idb], [pst], lambda e: e.transpose(pst[:, c * 64:(c + 1) * 64], P_[0:64, c * 128:(c + 1) * 128], idb[0:64, 0:64]))
                k.op("act", [pst], [PT_], lambda e: e.activation(PT_[:, 0:384], pst[:, 0:384], AF.Copy))
            else:
                for c in range(2):
                    k.op("pe", [P_, idb], [pst], lambda e: e.transpose(pst[:, c * 128:(c + 1) * 128], P_[:, c * 128:(c + 1) * 128], idb[:]))
                k.op("act", [pst], [PT_], lambda e: e.activation(PT_[:, 0:256], pst[:, 0:256], AF.Copy))

        def s3(i):
            h, r = items[i]
            hb = h % HB
            nq, q0, nk, rs, pat = geom(r)
            PT_ = PT[i % DP]
            pso = ps_o[i % 2]
            Ve_, Vo_, aT_ = Ve[hb], Vo[hb], aT[hb]
            if r < 64:
                for c in range(6):
                    if c < 4:
                        vch = Ve_[:, rs // 2 + c, :] if rs % 2 == 0 else Vo_[:, (rs - 1) // 2 + c, :]
                    else:
                        vch = Ve_[:, 32 + (c - 4), :]
                    k.op("pe", [Ve_, Vo_, PT_], [pso], lambda e: e.matmul(pso[:, 0:64], vch, PT_[:, c * 64:(c + 1) * 64], start=(c == 0), stop=(c == 5)))
                k.op("dve", [pso], [aT_], lambda e: e.tensor_copy(aT_[:, q0:q0 + 64], pso[:, 0:64]))
            else:
                for c in range(2):
                    k.op("pe", [Ve_, PT_], [pso], lambda e: e.matmul(pso[:, 0:128], Ve_[:, 32 + c, :], PT_[:, c * 128:(c + 1) * 128], start=(c == 0), stop=(c == 1)))
                k.op("dve", [pso], [aT_], lambda e: e.tensor_copy(aT_[:, q0:q0 + 128], pso[:, 0:128]))
            if r == 65:
                k.dma("pool", S["mixT"][h], aT_[:], [aT_], [S["mixT"]])

        for i in range(NI + 3):
            if i < NI:
                s1a(i)
            if 0 <= i - 1 < NI:
                s1b(i - 1)
            if 0 <= i - 2 < NI:
                s2(i - 2)
            if 0 <= i - 3 < NI:
                s3(i - 3)


def na_bias_host(rpb):
    ext = np.concatenate([rpb.reshape(8, -1), np.full((8, 1), -30000.0, np.float32)], axis=1)
    idx = np.full((8, 64, 8 * 64), 15 * 31, np.int64)
    pats = [0, 1, 2, 3, 4, 61, 62, 63]
    for pi, r in enumerate(pats):
        rs = min(max(r - 4, 0), 56)
        for i in range(8):
            dr = rs + i - r + 7
            for q in range(64):
                w0 = min(max(q - 8, 0), 48)
                for kc in range(w0, w0 + 16):
                    idx[pi, q, i * 64 + kc] = dr * 31 + (kc - q + 15)
    g = ext[:, idx]
    return np.ascontiguousarray(g.transpose(0, 2, 1, 3)).astype(np.float32)


def gla_consts_host():
    out = np.zeros((2, 3, 128, 128), np.float32)
    s = np.arange(128)[:, None]
    t = np.arange(128)[None, :]
    same = (s // 64) == (t // 64)
    v = -1.0 / 16.0
    out[0, 0] = np.where(same & (s <= t), v, 0)
    out[0, 1] = np.where(same & (s > t), v, 0)
    out[0, 2] = np.where(same & (t >= s), 1.0, 0)
    out[1, 0] = np.where(same & (s >= t), v, 0)
    out[1, 1] = np.where(same & (s < t), v, 0)
    out[1, 2] = np.where(same & (t <= s), 1.0, 0)
    return out


def rope_tables_host():
    t = np.arange(4096)
    nf = 32
    inv = (10000.0 ** (-np.arange(nf, dtype=np.float32) / nf)).astype(np.float32)
    row = (t // 64).astype(np.float32)[:, None] * inv
    col = (t % 64).astype(np.float32)[:, None] * inv
    ang = np.stack([row, col], 1)
    c = np.cos(ang).astype(np.float32)
    s = np.sin(ang).astype(np.float32)
    c = np.broadcast_to(c[:, None], (4096, 4, 2, 32)).reshape(4096, 256)
    s = np.broadcast_to(s[:, None], (4096, 4, 2, 32)).reshape(4096, 256)
    return np.ascontiguousarray(np.stack([c, s], 1)).astype(np.float32)


def phase_gla_prep(k, S, gate_w2, gate_b, ropet):
    gscale = 128 ** -0.5
    with k.phase():
        gw = k.sb("gp_gw", [16, 2, 512], F32)
        gb = k.sb("gp_gb", [1, 2, 512], F32)
        ones = k.sb("gp_ones", [1, 128], F32)
        k.dma("sp", gw[:], gate_w2[:].rearrange("d r c -> r d c"), [gate_w2], [gw])
        k.dma("sp", gb[:], gate_b[:].rearrange("(o d) c -> o d c", o=1), [gate_b], [gb])
        k.op("dve", [], [ones], lambda e: e.memset(ones[:], 1.0))
        qk = [k.sb("gp_qk%d" % i, [128, 2, 512], F32) for i in range(2)]
        qo = [k.sb("gp_qo%d" % i, [128, 2, 512], F32) for i in range(2)]
        cs = [k.sb("gp_cs%d" % i, [128, 2, 256], F32) for i in range(2)]
        tmp = [k.sb("gp_tmp%d" % i, [128, 4, 256], F32) for i in range(2)]
        lr = [k.sb("gp_lr%d" % i, [16, 2, 128], F32) for i in range(2)]
        ee = [k.sb("gp_e%d" % i, [128, 512], F32) for i in range(2)]
        spo = [k.sb("gp_sp%d" % i, [128, 512], F32) for i in range(2)]
        psz = [k.ps("gp_ps%d" % i, [128, 512], F32) for i in range(2)]
        n = 0
        for i in range(NTILE):
            u = i % 2
            q_, o_, c_, t_ = qk[u], qo[u], cs[u], tmp[u]
            rows = slice(i * 128, (i + 1) * 128)
            k.dma("sp", q_[:, 0, :], S["qb"][rows, :], [S["qb"]], [q_])
            k.dma("sp", q_[:, 1, :], S["kb"][rows, :], [S["kb"]], [q_])
            k.op("act", [q_], [q_], lambda e: e.mul(q_[:, 0, :], q_[:, 0, :], gscale))
            if i < 32:
                k.dma("sp", c_[:], ropet[rows], [ropet], [c_])
                for a in range(2):
                    xv = q_[:, a, :].rearrange("p (h a b f) -> p h a b f", h=4, a=2, b=2)
                    ov = o_[:, a, :].rearrange("p (h a b f) -> p h a b f", h=4, a=2, b=2)
                    x1, x2 = xv[:, :, :, 0, :], xv[:, :, :, 1, :]
                    C = c_[:, 0, :].rearrange("p (h a f) -> p h a f", h=4, a=2)
                    Sn = c_[:, 1, :].rearrange("p (h a f) -> p h a f", h=4, a=2)
                    tv = [t_[:, j, :].rearrange("p (h a f) -> p h a f", h=4, a=2) for j in range(4)]
                    k.op("dve", [q_, c_], [t_], lambda e: e.tensor_tensor(tv[0], x1, C, ALU.mult))
                    k.op("pool", [q_, c_], [t_], lambda e: e.tensor_tensor(tv[1], x2, Sn, ALU.mult))
                    k.op("dve", [q_, c_], [t_], lambda e: e.tensor_tensor(tv[2], x1, Sn, ALU.mult))
                    k.op("pool", [q_, c_], [t_], lambda e: e.tensor_tensor(tv[3], x2, C, ALU.mult))
                    k.op("dve", [t_], [o_], lambda e: e.tensor_tensor(ov[:, :, :, 0, :], tv[0], tv[1], ALU.subtract))
                    k.op("pool", [t_], [o_], lambda e: e.tensor_tensor(ov[:, :, :, 1, :], tv[2], tv[3], ALU.add))
                src = o_
            else:
                src = q_
            k.dma("pool", S["qbr"][rows, :], src[:, 0, :], [src], [S["qbr"]])
            k.dma("pool", S["kbr"][rows, :], src[:, 1, :], [src], [S["kbr"]])
            l_ = lr[u]
            k.dma("sp", l_[:], S["lrT"][:, :, i * 128:(i + 1) * 128].rearrange("d r t -> r d t"), [S["lrT"]], [l_])
            for d in range(2):
                ps = psz[n % 2]; e_ = ee[n % 2]; s_ = spo[n % 2]; n += 1
                k.op("pe", [l_, gw], [ps], lambda e: e.matmul(ps[:], l_[:, d, :], gw[:, d, :], start=True, stop=False))
                k.op("pe", [ones, gb], [ps], lambda e: e.matmul(ps[:], ones[0:1, :], gb[0:1, d, :], start=False, stop=True))
                k.op("act", [ps], [e_], lambda e: e.activation(e_[:], ps[:], AF.Exp, scale=-1.0))
                k.op("act", [e_], [s_], lambda e: e.activation(s_[:], e_[:], AF.Ln, bias=1.0))
                k.dma("pool", S["sp"][d, rows, :], s_[:], [s_], [S["sp"]])


def phase_gla(k, S, glac, idf):
    with k.phase():
        cst = k.sb("gl_c", [128, 6, 128], F32)
        k.dma("sp", cst[:], glac[:].rearrange("d m s t -> s (d m) t"), [glac], [cst])
        S32 = [[k.sb("gl_S%d%d" % (h, d), [128, 256], F32) for d in range(2)] for h in range(4)]
        Sbf = [[k.sb("gl_Sb%d%d" % (h, d), [128, 256], BF16) for d in range(2)] for h in range(4)]
        for h in range(4):
            for d in range(2):
                k.op("dve", [], [S32[h][d]], lambda e: e.memset(S32[h][d][:], 0.0))
                k.op("dve", [], [Sbf[h][d]], lambda e: e.memset(Sbf[h][d][:], 0.0))
        NB = 2
        inp = [[{"sp": k.sb("gl_sp%d%d" % (b, d), [128, 512], F32),
                 "q": k.sb("gl_q%d%d" % (b, d), [128, 512], F32),
                 "k": k.sb("gl_k%d%d" % (b, d), [128, 512], F32),
                 "v": k.sb("gl_v%d%d" % (b, d), [128, 1024], BF16)} for d in range(2)] for b in range(NB)]
        tmps = []
        for sl in range(2):
            tmps.append({
                "E1": k.sb("gl_E1%d" % sl, [128, 128], F32), "E2": k.sb("gl_E2%d" % sl, [128, 128], F32),
                "E3": k.sb("gl_E3%d" % sl, [128, 128], F32),
                "qd": k.sb("gl_qd%d" % sl, [128, 128], BF16), "kd": k.sb("gl_kd%d" % sl, [128, 128], BF16),
                "ktA": k.sb("gl_ktA%d" % sl, [128, 128], BF16), "ktB": k.sb("gl_ktB%d" % sl, [128, 128], BF16),
                "qdA": k.sb("gl_qdA%d" % sl, [128, 128], BF16), "qdB": k.sb("gl_qdB%d" % sl, [128, 128], BF16), "am": k.sb("gl_am%d" % sl, [128, 128], BF16),
                "o": k.sb("gl_o%d" % sl, [128, 256], F32), "Bsb": k.sb("gl_Bsb%d" % sl, [128, 256], F32),
                "pA": k.ps("gl_pA%d" % sl, [128, 512], F32), "pB": k.ps("gl_pB%d" % sl, [128, 512], F32),
                "pO": k.ps("gl_pO%d" % sl, [128, 512], F32)})
        for T in tmps:
            for nm in ("ktA", "ktB", "qdA", "qdB"):
                k.op("dve", [], [T[nm]], lambda e: e.memset(T[nm][:], 0.0))
        order = [[32, 33] + list(range(32)), [33, 32] + list(range(31, -1, -1))]
        for s in range(NTILE):
            b = s % NB
            for d in range(2):
                ti = order[d][s]
                rows = slice(ti * 128, (ti + 1) * 128)
                I = inp[b][d]
                k.dma("sp", I["sp"][:], S["sp"][d, rows, :], [S["sp"]], [I["sp"]])
                k.dma("sp", I["q"][:], S["qbr"][rows, :], [S["qbr"]], [I["q"]])
                k.dma("sp", I["k"][:], S["kbr"][rows, :], [S["kbr"]], [I["k"]])
                k.dma("sp", I["v"][:], S["vb"][rows, :], [S["vb"]], [I["v"]])
            streams = [(h, d) for h in range(4) for d in range(2)]
            for pi in range(0, 8, 2):
                pair = streams[pi:pi + 2]
                ctxs = []
                for sl, (h, d) in enumerate(pair):
                    T = tmps[sl]
                    I = inp[b][d]
                    hs = slice(h * 128, (h + 1) * 128)
                    TRI, SUF, MSK = cst[:, d * 3 + 0, :], cst[:, d * 3 + 1, :], cst[:, d * 3 + 2, :]
                    ctxs.append((h, d, T, I, hs, TRI, SUF, MSK))
                for (h, d, T, I, hs, TRI, SUF, MSK) in ctxs:
                    pA = T["pA"]
                    k.op("pe", [I["sp"], cst], [pA], lambda e: e.matmul(pA[:, 0:128], I["sp"][:, hs], TRI, start=True, stop=True))
                    k.op("pe", [I["sp"], cst], [pA], lambda e: e.matmul(pA[:, 128:256], SUF, I["sp"][:, hs], start=True, stop=True))
                    k.op("pe", [I["q"], idf], [pA], lambda e: e.transpose(pA[:, 256:384], I["q"][:, hs], idf[:]))
                    k.op("pe", [I["k"], idf], [pA], lambda e: e.transpose(pA[:, 384:512], I["k"][:, hs], idf[:]))
                for (h, d, T, I, hs, TRI, SUF, MSK) in ctxs:
                    pA = T["pA"]
                    EXPF = AF.Exp
                    Bsb = T["Bsb"]
                    k.op("dve", [pA], [Bsb], lambda e: e.tensor_copy(Bsb[:], pA[:, 0:256]))
                    k.op("act", [Bsb], [T["E1"]], lambda e: e.activation(T["E1"][:], Bsb[:, 0:128], EXPF))
                    k.op("act", [Bsb], [T["E2"]], lambda e: e.activation(T["E2"][:], Bsb[:, 0:128], EXPF, scale=-1.0))
                    k.op("act", [Bsb], [T["E3"]], lambda e: e.activation(T["E3"][:], Bsb[:, 128:256], EXPF))
                    k.op("dve", [pA, T["E1"]], [T["qd"]], lambda e: e.tensor_tensor(T["qd"][:], pA[:, 256:384], T["E1"][:], ALU.mult))
                    k.op("dve", [pA, T["E2"]], [T["kd"]], lambda e: e.tensor_tensor(T["kd"][:], pA[:, 384:512], T["E2"][:], ALU.mult))
                    ca = slice(0, 64) if d == 0 else slice(64, 128)
                    cb_ = slice(64, 128) if d == 0 else slice(0, 64)
                    k.op("pool", [], [T["ktA"]], lambda e: e.memset(T["ktA"][cb_, :], 0.0))
                    k.op("pool", [], [T["ktB"]], lambda e: e.memset(T["ktB"][ca, :], 0.0))
                    k.op("pool", [I["k"], T["E3"]], [T["ktA"]], lambda e: e.tensor_tensor(T["ktA"][ca, :], I["k"][ca, hs], T["E3"][ca, :], ALU.mult))
                    k.op("pool", [I["k"], T["E3"]], [T["ktB"]], lambda e: e.tensor_tensor(T["ktB"][cb_, :], I["k"][cb_, hs], T["E3"][cb_, :], ALU.mult))
                    k.op("pool", [], [T["qdA"]], lambda e: e.memset(T["qdA"][:, cb_], 0.0))
                    k.op("pool", [], [T["qdB"]], lambda e: e.memset(T["qdB"][:, ca], 0.0))
                    k.op("pool", [T["qd"]], [T["qdA"]], lambda e: e.tensor_copy(T["qdA"][:, ca], T["qd"][:, ca]))
                    k.op("pool", [T["qd"]], [T["qdB"]], lambda e: e.tensor_copy(T["qdB"][:, cb_], T["qd"][:, cb_]))
                for (h, d, T, I, hs, TRI, SUF, MSK) in ctxs:
                    pB = T["pB"]
                    k.op("pe", [T["kd"], T["qd"]], [pB], lambda e: e.matmul(pB[:, 0:128], T["kd"][:], T["qd"][:], start=True, stop=True))
                    k.op("dve", [pB, cst], [T["am"]], lambda e: e.tensor_tensor(T["am"][:], pB[:, 0:128], MSK, ALU.mult))
                for (h, d, T, I, hs, TRI, SUF, MSK) in ctxs:
                    pB, pO = T["pB"], T["pO"]
                    vh = I["v"][:, h * 256:(h + 1) * 256]
                    S3, Sb = S32[h][d], Sbf[h][d]
                    ca = slice(0, 64) if d == 0 else slice(64, 128)
                    cb_ = slice(64, 128) if d == 0 else slice(0, 64)
                    la = 63 if d == 0 else 64
                    lb = 127 if d == 0 else 0
                    k.op("pe", [T["am"], I["v"]], [pO], lambda e: e.matmul(pO[:, 0:256], T["am"][:], vh, start=True, stop=False))
                    k.op("pe", [T["qdA"], Sb], [pO], lambda e: e.matmul(pO[:, 0:256], T["qdA"][:], Sb[:], start=False, stop=False))
                    k.op("pe", [T["ktA"], I["v"]], [pB], lambda e: e.matmul(pB[:, 128:384], T["ktA"][:], vh, start=True, stop=True))
                    k.op("dve", [S3, T["E1"], pB], [S3], lambda e: e.scalar_tensor_tensor(S3[:], S3[:], T["E1"][:, la:la + 1], pB[:, 128:384], ALU.mult, ALU.add))
                    k.op("act", [S3], [Sb], lambda e: e.activation(Sb[:], S3[:], AF.Copy))
                    k.op("pe", [T["qdB"], Sb], [pO], lambda e: e.matmul(pO[:, 0:256], T["qdB"][:], Sb[:], start=False, stop=True))
                    k.op("pe", [T["ktB"], I["v"]], [pB], lambda e: e.matmul(pB[:, 128:384], T["ktB"][:], vh, start=True, stop=True))
                    k.op("dve", [S3, T["E1"], pB], [S3], lambda e: e.scalar_tensor_tensor(S3[:], S3[:], T["E1"][:, lb:lb + 1], pB[:, 128:384], ALU.mult, ALU.add))
                    k.op("act", [S3], [Sb], lambda e: e.activation(Sb[:], S3[:], AF.Copy))
                    k.op("act", [pO], [T["o"]], lambda e: e.activation(T["o"][:], pO[:, 0:256], AF.Copy))
                    ti = order[d][s]
                    k.dma("pool", S["ob"][d, ti * 128:(ti + 1) * 128, h * 256:(h + 1) * 256], T["o"][:], [T["o"]], [S["ob"]])


def phase_gatenorm(k, S, norm_g, idb):
    with k.phase():
        ng = k.sb("gn_ng", [128, 4, 256], F32)
        for h in range(4):
            k.dma("sp", ng[:, h, :], norm_g[:].partition_broadcast(128), [norm_g], [ng])
        bT = k.sb("gn_bT", [128, 8, NT], BF16)
        of = [k.sb("gn_of%d" % i, [128, 1024], F32) for i in range(2)]
        ob = [k.sb("gn_ob%d" % i, [128, 1024], F32) for i in range(2)]
        rr = [k.sb("gn_r%d" % i, [128, 1024], F32) for i in range(2)]
        sq = k.sb("gn_sq", [128, 256], F32)
        stt = [k.sb("gn_st%d" % i, [128, 8], F32) for i in range(2)]
        bl = [k.sb("gn_bl%d" % i, [128, 1024], BF16) for i in range(2)]
        pt = [k.ps("gn_pt%d" % i, [128, 1024], BF16) for i in range(2)]
        for i in range(NTILE):
            u = i % 2
            rows = slice(i * 128, (i + 1) * 128)
            a_, b_, r_, s_, l_, p_ = of[u], ob[u], rr[u], stt[u], bl[u], pt[u]
            k.dma("sp", a_[:], S["ob"][0, rows, :], [S["ob"]], [a_])
            k.dma("sp", b_[:], S["ob"][1, rows, :], [S["ob"]], [b_])
            k.dma("sp", r_[:], S["rb"][rows, :], [S["rb"]], [r_])
            k.op("dve", [a_, b_], [a_], lambda e: e.tensor_tensor(a_[:], a_[:], b_[:], ALU.add))
            for h in range(4):
                k.op("act", [a_], [sq, s_], lambda e: e.activation(sq[:], a_[:, h * 256:(h + 1) * 256], AF.Square, accum_out=s_[:, h:h + 1]))
            k.op("dve", [s_], [s_], lambda e: e.tensor_scalar(s_[:, 4:8], s_[:, 0:4], 1.0 / 256, 1e-6, ALU.mult, ALU.add))
            k.op("act", [s_], [s_], lambda e: e.activation(s_[:, 4:8], s_[:, 4:8], AF.Sqrt))
            k.op("dve", [s_], [s_], lambda e: e.reciprocal(s_[:, 4:8], s_[:, 4:8]))
            for h in range(4):
                k.op("dve", [a_, s_], [a_], lambda e: e.tensor_scalar_mul(a_[:, h * 256:(h + 1) * 256], a_[:, h * 256:(h + 1) * 256], s_[:, 4 + h:5 + h]))
            k.op("pool", [a_, ng], [a_], lambda e: e.tensor_tensor(a_[:], a_[:], ng[:].rearrange("p h c -> p (h c)"), ALU.mult))
            k.op("act", [r_], [r_], lambda e: e.activation(r_[:], r_[:], AF.Silu))
            k.op("dve", [a_, r_], [l_], lambda e: e.tensor_tensor(l_[:], a_[:], r_[:], ALU.mult))
            for j in range(8):
                k.op("pe", [l_, idb], [p_], lambda e: e.transpose(p_[:, j * 128:(j + 1) * 128], l_[:, j * 128:(j + 1) * 128], idb[:]))
            k.op("act", [p_], [bT], lambda e: e.activation(bT[:, :, i * 128:(i + 1) * 128], p_[:].rearrange("p (j t) -> p j t", j=8), AF.Copy))
        for j in range(8):
            k.dma("pool", S["mixT"][8 + j], bT[:, j, :], [bT], [S["mixT"]])


def resid_ln(k, srcs, x_, mv2, g_, b_, t1, stats, aggr):
    src_aps, src_res = srcs
    for n in range(4):
        k.op("dve", src_res + [mv2], [t1], lambda e: e.tensor_tensor(t1[:, n * 512:(n + 1) * 512], src_aps[n], mv2[:, n * 512:(n + 1) * 512], ALU.mult))
    k.op("dve", [x_, t1], [t1], lambda e: e.scalar_tensor_tensor(t1[:], x_[:], ALPHA, t1[:], ALU.mult, ALU.add))
    sq = stats[:].rearrange("p a b -> p (a b)")
    k.op("dve", [t1], [aggr], lambda e: e.reduce_sum(aggr[:, 0:1], t1[:], AX.X))
    k.op("dve", [aggr], [aggr], lambda e: e.tensor_scalar_mul(aggr[:, 0:1], aggr[:, 0:1], 1.0 / D))
    k.op("dve", [t1, aggr], [t1], lambda e: e.tensor_scalar(t1[:], t1[:], aggr[:, 0:1], None, ALU.subtract))
    k.op("act", [t1], [x_, aggr], lambda e: e.activation(x_[:], t1[:], AF.Square, accum_out=aggr[:, 1:2]))
    k.op("dve", [aggr], [aggr], lambda e: e.tensor_scalar(aggr[:, 2:3], aggr[:, 1:2], 1.0 / D, LN_EPS, ALU.mult, ALU.add))
    k.op("dve", [aggr], [aggr], lambda e: e.memset(aggr[:, 0:1], 0.0))
    k.op("act", [aggr], [aggr], lambda e: e.activation(aggr[:, 2:3], aggr[:, 2:3], AF.Sqrt))
    k.op("dve", [aggr], [aggr], lambda e: e.reciprocal(aggr[:, 2:3], aggr[:, 2:3]))
    k.op("dve", [t1, aggr], [t1], lambda e: e.tensor_scalar(t1[:], t1[:], aggr[:, 0:1], aggr[:, 2:3], ALU.subtract, ALU.mult))
    k.op("pool", [t1, g_], [t1], lambda e: e.tensor_tensor(t1[:], t1[:], g_[:], ALU.mult))
    k.op("pool", [t1, b_], [t1], lambda e: e.tensor_tensor(t1[:], t1[:], b_[:], ALU.add))


def routing(k, lg, rbb, comb_out, W):
    BIG = 1.0e4
    aff = W[:, 0:16]; sel = W[:, 16:32]; pr = W[:, 32:56]; sc = W[:, 56:60]; gm = W[:, 60:61]
    gs = W[:, 61:65]; pen = W[:, 65:69]; selm = W[:, 72:88]; m1 = W[:, 88:89]; oh = W[:, 96:112]
    sel2 = W[:, 112:128]; oh2 = W[:, 128:144]; ws = W[:, 144:145]
    Wr = W.res
    def dv(fn):
        k.op("dve", [Wr], [Wr], fn)
    lgs = W[:, 145:161] if False else W[:, 144:160]
    k.op("dve", [lg[1]], [Wr], lambda e: e.tensor_copy(sel, lg[0]))
    k.op("act", [Wr], [Wr], lambda e: e.activation(aff, sel, AF.Sigmoid))
    k.op("dve", [Wr, rbb], [Wr], lambda e: e.tensor_tensor(sel, aff, rbb[:], ALU.add))
    s3 = sel.rearrange("p (g e) -> p g e", g=4)
    pairs = [(0, 1), (0, 2), (0, 3), (1, 2), (1, 3), (2, 3)]
    p3 = pr.rearrange("p (n g) -> p n g", n=6)
    for n, (a, b) in enumerate(pairs):
        dv(lambda e: e.tensor_tensor(p3[:, n, :], s3[:, :, a], s3[:, :, b], ALU.add))
    dv(lambda e: e.tensor_tensor(sc, p3[:, 0, :], p3[:, 1, :], ALU.max))
    for n in range(2, 6):
        dv(lambda e: e.tensor_tensor(sc, sc, p3[:, n, :], ALU.max))
    dv(lambda e: e.reduce_max(gm, sc, AX.X))
    dv(lambda e: e.tensor_scalar(gs, sc, gm, None, ALU.is_ge))
    dv(lambda e: e.tensor_scalar(pen, gs, -1.0, BIG, ALU.add, ALU.mult))
    sm3 = selm.rearrange("p (g e) -> p g e", g=4)
    for g in range(4):
        dv(lambda e: e.tensor_scalar(sm3[:, g, :], s3[:, g, :], pen[:, g:g + 1], None, ALU.add))
    dv(lambda e: e.reduce_max(m1, selm, AX.X))
    dv(lambda e: e.tensor_scalar(oh, selm, m1, None, ALU.is_ge))
    dv(lambda e: e.scalar_tensor_tensor(sel2, oh, -BIG, selm, ALU.mult, ALU.add))
    dv(lambda e: e.reduce_max(m1, sel2, AX.X))
    dv(lambda e: e.tensor_scalar(oh2, sel2, m1, None, ALU.is_ge))
    dv(lambda e: e.tensor_tensor(oh, oh, oh2, ALU.add))
    dv(lambda e: e.tensor_tensor(oh, oh, aff, ALU.mult))
    dv(lambda e: e.reduce_sum(ws, oh, AX.X))
    dv(lambda e: e.reciprocal(ws, ws))
    k.op("dve", [Wr], [comb_out[1]], lambda e: e.tensor_scalar_mul(comb_out[0], oh, ws))


def post_mixer(k, layer, KD, mix_src, w_out_ap, xcur, modrow, ln_g, ln_b, router_w, router_b, S, idf, ntiles, xg=None, xout=None):
    with k.phase():
        wo = k.sb("pm_wo", [128, KD, 2048], BF16)
        stg = [k.sb("pm_stg%d" % i, [128, KD, 128], F32) for i in range(2)]
        for cb in range(16):
            st = stg[cb % 2]
            k.dma("sp", st[:], w_out_ap[:, cb * 128:(cb + 1) * 128].rearrange("(j p) c -> p j c", p=128), [], [st])
            k.op("act", [st], [wo], lambda e: e.activation(wo[:, :, cb * 128:(cb + 1) * 128], st[:], AF.Copy))
        m2 = k.sb("pm_m2", [128, D], F32); m3 = k.sb("pm_m3", [128, D], F32); m4 = k.sb("pm_m4", [128, D], F32)
        g_ = k.sb("pm_g", [128, D], F32); b_ = k.sb("pm_b", [128, D], F32)
        rw = k.sb("pm_rw", [128, 16, 16], F32)
        rbb = k.sb("pm_rbb", [128, 16], F32)
        load_bc(k, "sp", g_, ln_g[layer:layer + 1, :], [ln_g])
        load_bc(k, "sp", b_, ln_b[layer:layer + 1, :], [ln_b])
        k.dma("sp", rw[:], router_w[:].rearrange("(j p) e -> p j e", p=128), [router_w], [rw])
        load_bc(k, "sp", rbb, router_b[:], [router_b])
        xt = [k.sb("pm_x%d" % i, [128, D], F32) for i in range(3)]
        t1s = [k.sb("pm_t%d" % i, [128, D], F32) for i in range(2)]
        mt = [k.sb("pm_mt%d" % i, [128, KD, 128], BF16) for i in range(2)]
        hTb = [k.sb("pm_hTb%d" % i, [128, 16, 128], BF16) for i in range(2)]
        hTf = k.sb("pm_hTf", [128, 16, 128], F32)
        stats = k.sb("pm_stats", [128, 4, 6], F32)
        aggr = k.sb("pm_aggr", [128, 4], F32)
        W = k.sb("pm_W", [128, 160], F32)
        lgs = [k.sb("pm_lg%d" % i, [128, 16], F32) for i in range(2)]
        cb_ = [k.sb("pm_comb%d" % i, [128, 16], F32) for i in range(2)]
        pso = [k.ps("pm_pso%d" % i, [128, 512], F32) for i in range(4)]
        pst = [k.ps("pm_pst%d" % i, [128, 512], F32) for i in range(4)]

        def s0(i):
            if i == 0 or i == 32:
                r = 0 if i == 0 else 1
                load_bc(k, "sp", m2, modrow[layer, r:r + 1, 2 * D:3 * D], [modrow])
            x_, t1, m_ = xt[i % 3], t1s[i % 2], mt[i % 2]
            rows = slice(i * 128, (i + 1) * 128)
            if xg is None:
                k.dma("sp", x_[:], xcur[rows, :], [xcur], [x_])
            else:
                k.idma_gather(x_[:], xcur[:], xg[:, i:i + 1], [xcur, xg], [x_])
            k.dma("sp", m_[:], mix_src[:, :, rows].rearrange("j p t -> p j t"), [mix_src], [m_])
            for n in range(4):
                for j in range(KD):
                    k.op("pe", [m_, wo], [pso[n]], lambda e: e.matmul(pso[n][:], m_[:, j, :], wo[:, j, n * 512:(n + 1) * 512], start=(j == 0), stop=(j == KD - 1)))
            for n in range(4):
                k.op("dve", [pso[n], m2], [t1], lambda e: e.tensor_tensor(t1[:, n * 512:(n + 1) * 512], pso[n][:], m2[:, n * 512:(n + 1) * 512], ALU.mult))

        def s1(i):
            if i == 0 or i == 32:
                r = 0 if i == 0 else 1
                load_bc(k, "sp", m3, modrow[layer, r:r + 1, 3 * D:4 * D], [modrow])
                load_bc(k, "sp", m4, modrow[layer, r:r + 1, 4 * D:5 * D], [modrow])
            x_, t1 = xt[i % 3], t1s[i % 2]
            rows = slice(i * 128, (i + 1) * 128)
            k.op("dve", [x_, t1], [t1], lambda e: e.scalar_tensor_tensor(t1[:], x_[:], ALPHA, t1[:], ALU.mult, ALU.add))
            k.op("dve", [t1], [aggr], lambda e: e.reduce_sum(aggr[:, 0:1], t1[:], AX.X))
            k.op("dve", [aggr], [aggr], lambda e: e.tensor_scalar_mul(aggr[:, 0:1], aggr[:, 0:1], 1.0 / D))
            k.op("dve", [t1, aggr], [t1], lambda e: e.tensor_scalar(t1[:], t1[:], aggr[:, 0:1], None, ALU.subtract))
            k.op("act", [t1], [x_, aggr], lambda e: e.activation(x_[:], t1[:], AF.Square, accum_out=aggr[:, 1:2]))
            k.op("dve", [aggr], [aggr], lambda e: e.tensor_scalar(aggr[:, 2:3], aggr[:, 1:2], 1.0 / D, LN_EPS, ALU.mult, ALU.add))
            k.op("act", [aggr], [aggr], lambda e: e.activation(aggr[:, 2:3], aggr[:, 2:3], AF.Sqrt))
            k.op("dve", [aggr], [aggr], lambda e: e.reciprocal(aggr[:, 2:3], aggr[:, 2:3]))
            k.op("dve", [t1, aggr], [t1], lambda e: e.tensor_scalar_mul(t1[:], t1[:], aggr[:, 2:3]))
            k.op("pool", [t1, g_], [t1], lambda e: e.tensor_tensor(t1[:], t1[:], g_[:], ALU.mult))
            k.op("pool", [t1, b_], [t1], lambda e: e.tensor_tensor(t1[:], t1[:], b_[:], ALU.add))
            xo = xcur if xout is None else xout
            k.dma("pool", xo[rows, :], t1[:], [t1], [xo])
            k.op("dve", [t1, m4], [x_], lambda e: e.tensor_tensor(x_[:], t1[:], m4[:], ALU.mult))
            k.op("pool", [x_, m3], [x_], lambda e: e.tensor_tensor(x_[:], x_[:], m3[:], ALU.add))

        def s2(i):
            x_, hb, lg = xt[i % 3], hTb[i % 2], lgs[i % 2]
            for j in range(16):
                k.op("pe", [x_, idf], [pst[j // 4]], lambda e: e.transpose(pst[j // 4][:, (j % 4) * 128:(j % 4 + 1) * 128], x_[:, j * 128:(j + 1) * 128], idf[:]))
            for n in range(4):
                k.op("dve", [pst[n]], [hTf], lambda e: e.tensor_copy(hTf[:, n * 4:(n + 1) * 4, :], pst[n][:].rearrange("p (a t) -> p a t", a=4)))
                k.op("act", [hTf], [hb], lambda e: e.activation(hb[:, n * 4:(n + 1) * 4, :], hTf[:, n * 4:(n + 1) * 4, :], AF.Copy))
            k.dma("pool", S["hT_moe"][i], hb[:], [hb], [S["hT_moe"]])
            for j in range(16):
                k.op("pe", [hTf, rw], [pst[0]], lambda e: e.matmul(pst[0][:, 0:16], hTf[:, j, :], rw[:, j, :], start=(j == 0), stop=(j == 15)))
            k.op("dve", [pst[0]], [lg], lambda e: e.tensor_copy(lg[:], pst[0][:, 0:16]))

        def s3(i):
            lg, c_ = lgs[i % 2], cb_[i % 2]
            rows = slice(i * 128, (i + 1) * 128)
            routing(k, (lg[:], lg.res), rbb, (c_[:], c_.res), W)
            k.dma("pool", S["comb"][rows, :], c_[:], [c_], [S["comb"]])

        for step in range(ntiles + 3):
            if step < ntiles:
                s0(step)
            if 0 <= step - 1 < ntiles:
                s1(step - 1)
            if 0 <= step - 2 < ntiles:
                s2(step - 2)
            if 0 <= step - 3 < ntiles:
                s3(step - 3)


def phase_moe(k, layer, blocks, w_gate, w_up, w_down, xcur, modrow, ln_g, ln_b, S, out_dst, n_exp=16, gidx=None):
    with k.phase():
        TBmax = max(blocks) * 128
        hTb = k.sb("mo_hT", [128, 16, TBmax], BF16)
        yacc = k.sb("mo_y", [128, max(blocks), D], F32)
        hid = k.sb("mo_hid", [128, 8, TBmax], BF16)
        comb = k.sb("mo_comb", [128, max(blocks), 16], F32)
        wd = k.sb("mo_wd", [128, 8, 1024], BF16)
        wdf = [TT(wd.t, Res("wd%d" % i)) for i in range(8)]
        wg = [k.sb("mo_wg%d" % i, [128, 16, 128], BF16) for i in range(2)]
        wu = [k.sb("mo_wu%d" % i, [128, 16, 128], BF16) for i in range(2)]
        stg = [k.sb("mo_stg%d" % i, [128, D], F32) for i in range(3)]
        sg = [k.sb("mo_sg%d" % i, [128, 512], F32) for i in range(2)]
        xe = k.sb("mo_xe", [128, D], F32)
        gstg = [TT(stg[i][:].bitcast(BF16)[:, 0:D], stg[i].res) for i in range(2)] if gidx is not None else None
        stats = k.sb("mo_stats", [128, 4, 6], F32)
        aggr = k.sb("mo_aggr", [128, 4], F32)
        psg = [k.ps("mo_psg%d" % i, [128, 512], F32) for i in range(2)]
        psu = [k.ps("mo_psu%d" % i, [128, 512], F32) for i in range(2)]
        psy = [k.ps("mo_psy%d" % i, [128, 512], F32) for i in range(4)]
        sc = 0
        t0 = 0
        cnt = 0
        for nb in blocks:
            TB = nb * 128
            if gidx is None:
                for t in range(nb):
                    k.dma("sp", hTb[:, :, t * 128:(t + 1) * 128], S["hT_moe"][t0 + t], [S["hT_moe"]], [hTb])
                k.dma("sp", comb[:, 0:nb, :], S["comb"][t0 * 128:(t0 + nb) * 128, :].rearrange("(t p) e -> p t e", p=128), [S["comb"]], [comb])
            else:
                hview = S["hT_moe"][:].rearrange("n p j t -> (n p) (j t)")
                for t in range(nb):
                    gb_ = gstg[t % 2]
                    k.idma_gather(gb_[:], hview, gidx[:, t0 + t:t0 + t + 1], [S["hT_moe"], gidx], [gb_])
                    k.op("act", [gb_], [hTb], lambda e: e.activation(hTb[:, :, t * 128:(t + 1) * 128], gb_[:].rearrange("p (j c) -> p j c", j=16), AF.Copy))
                    k.idma_gather(comb[:, t, :], S["comb"][:], gidx[:, t0 + t:t0 + t + 1], [S["comb"], gidx], [comb])
            k.op("pool", [], [yacc], lambda e: e.memset(yacc[:], 0.0))
            subs = [(s0, min(512, TB - s0)) for s0 in range(0, TB, 512)]
            for ex in range(n_exp):
                for ft in range(8):
                    g_, u_ = wg[ft % 2], wu[ft % 2]
                    for (wsrc, wdst) in ((w_gate, g_), (w_up, u_)):
                        st = stg[sc % 3]; sc += 1
                        k.dma("sp", st[:].rearrange("p (j c) -> p j c", j=16), wsrc[layer, ex, :, ft * 128:(ft + 1) * 128].rearrange("(j p) c -> p j c", p=128), [], [st])
                        k.op("act", [st], [wdst], lambda e: e.activation(wdst[:].rearrange("p j c -> p (j c)"), st[:], AF.Copy))
                    for (s0, sn) in subs:
                        pg, pu, s_ = psg[cnt % 2], psu[cnt % 2], sg[cnt % 2]
                        cnt += 1
                        for j in range(16):
                            k.op("pe", [g_, hTb], [pg], lambda e: e.matmul(pg[:, 0:sn], g_[:, j, :], hTb[:, j, s0:s0 + sn], start=(j == 0), stop=(j == 15)))
                        for j in range(16):
                            k.op("pe", [u_, hTb], [pu], lambda e: e.matmul(pu[:, 0:sn], u_[:, j, :], hTb[:, j, s0:s0 + sn], start=(j == 0), stop=(j == 15)))
                        k.op("act", [pg], [s_], lambda e: e.activation(s_[:, 0:sn], pg[:, 0:sn], AF.Silu))
                        k.op("dve", [s_, pu], [hid], lambda e: e.tensor_tensor(hid[:, ft, s0:s0 + sn], s_[:, 0:sn], pu[:, 0:sn], ALU.mult))
                for hf in range(2):
                    for fc in range(8):
                        st = stg[sc % 3]; sc += 1
                        k.dma("sp", st[:, 0:1024], w_down[layer, ex, fc * 128:(fc + 1) * 128, hf * 1024:(hf + 1) * 1024], [], [st])
                        k.op("act", [st], [wdf[fc]], lambda e: e.activation(wd[:, fc, :], st[:, 0:1024], AF.Copy))
                    for t in range(nb):
                        for n2 in range(2):
                            n = hf * 2 + n2
                            py = psy[(t * 4 + n) % 4]
                            for fc in range(8):
                                k.op("pe", [hid, wdf[fc]], [py], lambda e: e.matmul(py[:], hid[:, fc, t * 128:(t + 1) * 128], wd[:, fc, n2 * 512:(n2 + 1) * 512], start=(fc == 0), stop=(fc == 7)))
                            k.op("dve", [py, comb, yacc], [yacc], lambda e: e.scalar_tensor_tensor(
                                yacc[:, t, n * 512:(n + 1) * 512], py[:], comb[:, t, ex:ex + 1], yacc[:, t, n * 512:(n + 1) * 512], ALU.mult, ALU.add))
            m5, g_, b_ = stg[0], stg[1], stg[2]
            xt = xe[:]
            load_bc(k, "sp", g_, ln_g[layer:layer + 1, :], [ln_g])
            load_bc(k, "sp", b_, ln_b[layer:layer + 1, :], [ln_b])
            for t in range(nb):
                ti = t0 + t
                if t == 0 or ti == 32:
                    r = 0 if ti < 32 else 1
                    load_bc(k, "sp", m5, modrow[layer, r:r + 1, 5 * D:6 * D], [modrow])
                rows = slice(ti * 128, (ti + 1) * 128)
                if gidx is None:
                    k.dma("sp", xt, xcur[rows, :], [xcur], [xe])
                else:
                    k.idma_gather(xt, xcur[:], gidx[:, ti:ti + 1], [xcur, gidx], [xe])
                resid_ln_ap(k, yacc, t, xe, xt, m5, g_, b_, stats, aggr)
                k.dma("pool", out_dst[rows, :], yacc[:, t, :], [yacc], [out_dst], is_output=True)
            t0 += nb


def resid_ln_ap(k, yacc, t, xres, xt, mv2, g_, b_, stats, aggr):
    y = yacc[:, t, :]
    k.op("dve", [yacc, mv2], [yacc], lambda e: e.tensor_tensor(y, y, mv2[:], ALU.mult))
    k.op("dve", [xres, yacc], [yacc], lambda e: e.scalar_tensor_tensor(y, xt, ALPHA, y, ALU.mult, ALU.add))
    k.op("dve", [yacc], [aggr], lambda e: e.reduce_sum(aggr[:, 0:1], y, AX.X))
    k.op("dve", [aggr], [aggr], lambda e: e.tensor_scalar_mul(aggr[:, 0:1], aggr[:, 0:1], 1.0 / D))
    k.op("dve", [yacc, aggr], [yacc], lambda e: e.tensor_scalar(y, y, aggr[:, 0:1], None, ALU.subtract))
    k.op("act", [yacc], [xres, aggr], lambda e: e.activation(xt, y, AF.Square, accum_out=aggr[:, 1:2]))
    k.op("dve", [aggr], [aggr], lambda e: e.tensor_scalar(aggr[:, 2:3], aggr[:, 1:2], 1.0 / D, LN_EPS, ALU.mult, ALU.add))
    k.op("dve", [aggr], [aggr], lambda e: e.memset(aggr[:, 0:1], 0.0))
    k.op("act", [aggr], [aggr], lambda e: e.activation(aggr[:, 2:3], aggr[:, 2:3], AF.Sqrt))
    k.op("dve", [aggr], [aggr], lambda e: e.reciprocal(aggr[:, 2:3], aggr[:, 2:3]))
    k.op("dve", [yacc, aggr], [yacc], lambda e: e.tensor_scalar(y, y, aggr[:, 0:1], aggr[:, 2:3], ALU.subtract, ALU.mult))
    k.op("pool", [yacc, g_], [yacc], lambda e: e.tensor_tensor(y, y, g_[:], ALU.mult))
    k.op("pool", [yacc, b_], [yacc], lambda e: e.tensor_tensor(y, y, b_[:], ALU.add))


TWO_PI = 6.283185307179586
SIN_SC = 6.28318


def s5_host_params(lam_re, lam_im, log_dt, b_re, b_im, c_re, c_im):
    f = np.float32
    def padl(a):
        o = a.reshape(2, 16, 4, 64).transpose(2, 0, 1, 3)
        o = np.broadcast_to(o[:, None], (4, 32, 2, 16, 64)).reshape(128, 2, 16, 64)
        return np.ascontiguousarray(o).astype(f)
    lamre_bc = padl(lam_re)
    lamim_bc = padl(lam_im)
    dt_bc = padl(np.broadcast_to(log_dt[:, :, None], (2, 64, 64)))
    def padb(bb):
        o = np.zeros((4, 32, 2, 16, 64), f)
        t = bb.reshape(2, 16, 4, 64, 16).transpose(2, 4, 0, 1, 3)
        o[:, 0:16] = t
        return o.reshape(128, 2, 16, 64)
    bre_pad = padb(b_re)
    bim_pad = padb(b_im)
    def pl(a):
        o = a.transpose(2, 0, 1).reshape(64, 128)
        return np.ascontiguousarray(np.concatenate([o, o], 0)).astype(f)
    lamre_p = pl(lam_re)
    lamim_p = pl(lam_im)
    dt_p = pl(np.broadcast_to(log_dt[:, :, None], (2, 64, 64)))
    cr = c_re.transpose(3, 0, 1, 2).reshape(64, 128, 16)
    ci = c_im.transpose(3, 0, 1, 2).reshape(64, 128, 16)
    craw1 = np.ascontiguousarray(np.concatenate([cr, ci], 0)).astype(f)
    craw2 = np.ascontiguousarray(np.concatenate([ci, cr], 0)).astype(f)
    pa = np.ascontiguousarray(np.stack([lamre_bc, lamim_bc, dt_bc, bre_pad, bim_pad], 1)).reshape(128, 5, 2048)
    pb = np.ascontiguousarray(np.stack([lamre_p, lamim_p, dt_p], 1))
    return pa, pb, craw1, craw2


def sincos_small(k, eng, turns, sn, cs, tmpi, tmpf):
    k.op(eng, [turns], [tmpi], lambda e: e.tensor_copy(tmpi[:], turns[:]))
    k.op(eng, [tmpi], [tmpf], lambda e: e.tensor_copy(tmpf[:], tmpi[:]))
    k.op(eng, [turns, tmpf], [tmpf], lambda e: e.tensor_tensor(tmpf[:], turns[:], tmpf[:], ALU.subtract))
    k.op("act", [tmpf], [sn], lambda e: e.activation(sn[:], tmpf[:], AF.Sin, scale=SIN_SC))
    k.op(eng, [tmpf], [tmpf], lambda e: e.tensor_scalar_add(tmpf[:], tmpf[:], 0.25))
    k.op(eng, [tmpf], [turns], lambda e: e.tensor_single_scalar(turns[:], tmpf[:], 0.5, ALU.is_gt))
    k.op(eng, [tmpf, turns], [tmpf], lambda e: e.tensor_tensor(tmpf[:], tmpf[:], turns[:], ALU.subtract))
    k.op("act", [tmpf], [cs], lambda e: e.activation(cs[:], tmpf[:], AF.Sin, scale=SIN_SC))


def phase_l1_inproj(k, hT_all, od_w_in, S, idb, jb):
    with k.phase():
        wb = k.sb("l1_wb", [128, 16, 1024], BF16)
        with k.phase():
            stg = [k.sb("l1_stg%d" % i, [128, 16, 128], F32) for i in range(2)]
            for cb in range(8):
                st = stg[cb % 2]
                k.dma("sp", st[:], od_w_in[:, cb * 128:(cb + 1) * 128].rearrange("(j p) c -> p j c", p=128), [], [st])
                k.op("act", [st], [wb], lambda e: e.activation(wb[:, :, cb * 128:(cb + 1) * 128], st[:], AF.Copy))
        uf = [k.sb("l1_uf%d" % i, [128, 1024], F32) for i in range(2)]
        upad = [k.sb("l1_up%d" % i, [128, 64, 32], BF16) for i in range(2)]
        for u_ in upad:
            k.op("pool", [], [u_], lambda e: e.memset(u_[:], 0.0))
        stF = [k.sb("l1_sF%d" % i, [128, 16, 128], BF16) for i in range(1)] * 2
        stB = [k.sb("l1_sB%d" % i, [128, 16, 128], BF16) for i in range(1)] * 2
        psu = [k.ps("l1_psu%d" % i, [128, 512], F32) for i in range(2)]
        pst = [k.ps("l1_pst%d" % i, [128, 512], F32) for i in range(4)]
        pc = 0
        for i in range(NTILE):
            u = i % 2
            f_, p_, sF, sB = uf[u], upad[u], stF[u], stB[u]
            for n in range(2):
                for j in range(16):
                    k.op("pe", [hT_all, wb], [psu[n]], lambda e: e.matmul(psu[n][:], hT_all[:, j, i * 128:(i + 1) * 128], wb[:, j, n * 512:(n + 1) * 512], start=(j == 0), stop=(j == 15)))
                k.op("act", [psu[n]], [f_], lambda e: e.activation(f_[:, n * 512:(n + 1) * 512], psu[n][:], AF.Copy))
                k.op("dve", [f_], [p_], lambda e: e.tensor_copy(p_[:, n * 32:(n + 1) * 32, 0:16], f_[:, n * 512:(n + 1) * 512].rearrange("p (g c) -> p g c", c=16)))
            if i < 32:
                k.dma("pool", S["u_tm"][i * 128:(i + 1) * 128, :], f_[:], [f_], [S["u_tm"]])
            pv = p_[:].rearrange("p (ch q) c -> p ch (q c)", q=4)
            for (mat, dst) in ((idb, sF), (jb, sB)):
                for c4 in range(4):
                    ps = pst[pc % 4]; pc += 1
                    for cc in range(4):
                        ch = c4 * 4 + cc
                        k.op("pe", [p_, mat], [ps], lambda e: e.matmul(ps[:, cc * 128:(cc + 1) * 128], pv[:, ch, :], mat[:], start=True, stop=True))
                    eng = "act" if c4 % 2 == 0 else "dve"
                    if eng == "act":
                        k.op("act", [ps], [dst], lambda e: e.activation(dst[:, c4 * 4:(c4 + 1) * 4, :], ps[:].rearrange("p (a t) -> p a t", a=4), AF.Copy))
                    else:
                        k.op("dve", [ps], [dst], lambda e: e.tensor_copy(dst[:, c4 * 4:(c4 + 1) * 4, :], ps[:].rearrange("p (a t) -> p a t", a=4)))
            cf = (i - 32) * 128 if i >= 32 else 256 + i * 128
            cbk = NT - (i + 1) * 128
            k.dma("pool", S["uTf"][:, :, cf:cf + 128].rearrange("j p t -> p j t"), sF[:], [sF], [S["uTf"]])
            k.dma("pool", S["uTb"][:, :, cbk:cbk + 128].rearrange("j p t -> p j t"), sB[:], [sB], [S["uTb"]])


def phase_s5(k, S, pa_d, pb_d, craw1_d, craw2_d, ngroups=64):
    with k.phase():
        BT1 = k.sb("s5_BT1", [128, 32, 128], BF16)
        BT2 = k.sb("s5_BT2", [128, 32, 128], BF16)
        M1 = k.sb("s5_M1", [128, 128, 16], BF16)
        M2 = k.sb("s5_M2", [128, 128, 16], BF16)
        rdec = k.sb("s5_rdec", [128, 128], F32)
        thn = k.sb("s5_thn", [128, 128], F32)
        with k.phase():
            pa = k.sb("s5_pa", [128, 5, 2048], F32)
            k.dma("sp", pa[:], pa_d[:], [pa_d], [pa])
            T = [k.sb("s5_T%d" % i, [128, 2048], F32) for i in range(8)]
            Ti = k.sb("s5_Ti", [128, 2048], I32)
            lre, lim, ldt, bre, bim = [TT(pa[:, j, :], pa.res) for j in range(5)]
            dt, ang, mag, sn, cs, t5, t6, t7 = T
            def P(fn, r, w):
                k.op("pool", r, w, fn)
            k.op("act", [pa], [dt], lambda e: e.activation(dt[:], pa[:, 2, :], AF.Exp))
            P(lambda e: e.tensor_tensor(ang[:], pa[:, 1, :], dt[:], ALU.mult), [pa, dt], [ang])
            P(lambda e: e.tensor_scalar_mul(ang[:], ang[:], 1.0 / TWO_PI), [ang], [ang])
            P(lambda e: e.tensor_tensor(mag[:], pa[:, 0, :], dt[:], ALU.mult), [pa, dt], [mag])
            k.op("act", [mag], [mag], lambda e: e.activation(mag[:], mag[:], AF.Exp))
            sincos_small(k, "pool", ang, sn, cs, Ti, t5)
            P(lambda e: e.tensor_tensor(cs[:], cs[:], mag[:], ALU.mult), [cs, mag], [cs])
            P(lambda e: e.tensor_scalar_add(cs[:], cs[:], -1.0), [cs], [cs])
            P(lambda e: e.tensor_tensor(sn[:], sn[:], mag[:], ALU.mult), [sn, mag], [sn])
            P(lambda e: e.tensor_tensor(t5[:], pa[:, 0, :], pa[:, 0, :], ALU.mult), [pa], [t5])
            P(lambda e: e.tensor_tensor(t6[:], pa[:, 1, :], pa[:, 1, :], ALU.mult), [pa], [t6])
            P(lambda e: e.tensor_tensor(t5[:], t5[:], t6[:], ALU.add), [t5, t6], [t5])
            k.op("dve", [t5], [t5], lambda e: e.reciprocal(t5[:], t5[:]))
            P(lambda e: e.tensor_tensor(t6[:], cs[:], pa[:, 0, :], ALU.mult), [cs, pa], [t6])
            P(lambda e: e.tensor_tensor(t7[:], sn[:], pa[:, 1, :], ALU.mult), [sn, pa], [t7])
            P(lambda e: e.tensor_tensor(t6[:], t6[:], t7[:], ALU.add), [t6, t7], [t6])
            P(lambda e: e.tensor_tensor(t6[:], t6[:], t5[:], ALU.mult), [t6, t5], [t6])
            P(lambda e: e.tensor_tensor(t7[:], sn[:], pa[:, 0, :], ALU.mult), [sn, pa], [t7])
            P(lambda e: e.tensor_tensor(mag[:], cs[:], pa[:, 1, :], ALU.mult), [cs, pa], [mag])
            P(lambda e: e.tensor_tensor(t7[:], t7[:], mag[:], ALU.subtract), [t7, mag], [t7])
            P(lambda e: e.tensor_tensor(t7[:], t7[:], t5[:], ALU.mult), [t7, t5], [t7])
            P(lambda e: e.tensor_tensor(dt[:], t6[:], pa[:, 3, :], ALU.mult), [t6, pa], [dt])
            P(lambda e: e.tensor_tensor(mag[:], t7[:], pa[:, 4, :], ALU.mult), [t7, pa], [mag])
            P(lambda e: e.tensor_tensor(dt[:], dt[:], mag[:], ALU.subtract), [dt, mag], [dt])
            P(lambda e: e.tensor_tensor(ang[:], t6[:], pa[:, 4, :], ALU.mult), [t6, pa], [ang])
            P(lambda e: e.tensor_tensor(mag[:], t7[:], pa[:, 3, :], ALU.mult), [t7, pa], [mag])
            P(lambda e: e.tensor_tensor(ang[:], ang[:], mag[:], ALU.add), [ang, mag], [ang])
            bre_v = dt[:].rearrange("p (a b) -> p a b", b=64)
            bim_v = ang[:].rearrange("p (a b) -> p a b", b=64)
            k.op("dve", [dt], [BT1], lambda e: e.tensor_copy(BT1[:, :, 0:64], bre_v))
            k.op("dve", [ang], [BT1], lambda e: e.tensor_copy(BT1[:, :, 64:128], bim_v))
            k.op("dve", [ang], [BT2], lambda e: e.tensor_copy(BT2[:, :, 0:64], bim_v))
            k.op("dve", [dt], [BT2], lambda e: e.tensor_scalar_mul(BT2[:, :, 64:128], bre_v, -1.0))
            pb = k.sb("s5_pb", [128, 3, 128], F32)
            k.dma("sp", pb[:], pb_d[:], [pb_d], [pb])
            k.op("act", [pb], [thn], lambda e: e.activation(thn[:], pb[:, 2, :], AF.Exp))
            k.op("dve", [pb, thn], [rdec], lambda e: e.tensor_tensor(rdec[:], pb[:, 0, :], thn[:], ALU.mult))
            k.op("act", [rdec], [rdec], lambda e: e.activation(rdec[:], rdec[:], AF.Exp))
            k.op("dve", [pb, thn], [thn], lambda e: e.tensor_tensor(thn[:], pb[:, 1, :], thn[:], ALU.mult))
            k.op("dve", [thn], [thn], lambda e: e.tensor_scalar_mul(thn[:], thn[:], 1.0 / TWO_PI))
            cr1 = T[0]; cr2 = T[1]
            k.dma("sp", cr1[:], craw1_d[:].rearrange("p a b -> p (a b)"), [craw1_d], [cr1])
            k.dma("sp", cr2[:], craw2_d[:].rearrange("p a b -> p (a b)"), [craw2_d], [cr2])
            M1f = M1[:].rearrange("p a b -> p (a b)")
            M2f = M2[:].rearrange("p a b -> p (a b)")
            k.op("dve", [cr1], [M1], lambda e: e.tensor_copy(M1f[0:64, :], cr1[0:64, :]))
            k.op("dve", [cr1], [M1], lambda e: e.tensor_scalar_mul(M1f[64:128, :], cr1[64:128, :], -1.0))
            k.op("dve", [cr2], [M2], lambda e: e.tensor_scalar_mul(M2f, cr2[:], -1.0))
        BT1z = k.sb("s5_BT1z", [128, 32, 128], BF16)
        BT2z = k.sb("s5_BT2z", [128, 32, 128], BF16)
        k.op("dve", [BT1], [BT1z], lambda e: e.tensor_copy(BT1z[64:128], BT1[64:128]))
        k.op("dve", [BT2], [BT2z], lambda e: e.tensor_copy(BT2z[64:128], BT2[64:128]))
        k.op("dve", [], [BT1z], lambda e: e.memset(BT1z[64:96], 0.0))
        k.op("dve", [], [BT2z], lambda e: e.memset(BT2z[64:96], 0.0))
        PI_SC = 3.14159
        MAGIC = 12582912.0
        iot_i = k.sb("s5_ioti", [128, NT], I32)
        k.op("pool", [], [iot_i], lambda e: e.iota(iot_i[:], pattern=[[1, NT]], base=0, channel_multiplier=0))
        ones = k.sb("s5_ones", [128, 512], F32)
        k.op("dve", [], [ones], lambda e: e.memset(ones[:], 1.0))
        ND = 4
        uT = [k.sb("s5_uT%d" % i, [128, NT], BF16) for i in range(2)]
        frq = [k.sb("s5_fr%d" % i, [128, 512], F32) for i in range(ND)]
        kiq = [k.sb("s5_ki%d" % i, [128, 512], I32) for i in range(2)]
        S2q = [k.sb("s5_S2%d" % i, [128, 512], F32) for i in range(ND)]
        C2q = [k.sb("s5_C2%d" % i, [128, 512], F32) for i in range(ND)]
        shq = [k.sb("s5_sh%d" % i, [128, 512], F32) for i in range(2)]
        bq = [k.sb("s5_bq%d" % i, [128, 512], F32) for i in range(3)]
        tq = [k.sb("s5_tq%d" % i, [128, 512], F32) for i in range(3)]
        wq = [k.sb("s5_w%d" % i, [128, 512], F32) for i in range(3)]
        z1q = [k.sb("s5_z1%d" % i, [128, 512], BF16) for i in range(3)]
        z2q = [k.sb("s5_z2%d" % i, [128, 512], BF16) for i in range(3)]
        Rtq = [k.sb("s5_Rt%d" % i, [128, 512], F32) for i in range(2)]
        y8 = [k.sb("s5_y8%d" % i, [128, NTILE, 128], F32) for i in range(2)]
        pp1 = [k.ps("s5_p1%d" % i, [128, 512], F32) for i in range(2)]
        pp2 = [k.ps("s5_p2%d" % i, [128, 512], F32) for i in range(2)]
        psy = [k.ps("s5_py%d" % i, [128, 512], F32) for i in range(4)]
        items = []
        for d in range(2):
            for g in range(ngroups):
                for bi, (t0, tn) in enumerate(TBLKS):
                    items.append((d, g, bi, t0, tn))
        NI = len(items)

        def stage_T(i):
            d, g, bi, t0, tn = items[i]
            dg = d * 64 + g
            fr_, ki_, S_, C_, sh_ = frq[i % ND], kiq[i % 2], S2q[i % ND], C2q[i % ND], shq[i % 2]
            if bi == 0:
                ch, q = g // 4, g % 4
                if q == 0:
                    usrc = S["uTf"] if d == 0 else S["uTb"]
                    ut = uT[(d * 16 + ch) % 2]
                    k.dma("sp", ut[:], usrc[ch], [usrc], [ut])
                Rt_ = Rtq[dg % 2]
                k.op("dve", [ones, rdec], [Rt_], lambda e: e.tensor_scalar_mul(Rt_[:], ones[:], rdec[:, dg:dg + 1]))
            k.op("act", [iot_i, thn], [fr_], lambda e: e.activation(fr_[:, 0:tn], iot_i[:, t0:t0 + tn], AF.Copy, scale=thn[:, dg:dg + 1]))
            kf_ = ki_[:].bitcast(F32)
            k.op("act", [fr_], [ki_], lambda e: e.activation(kf_[:, 0:tn], fr_[:, 0:tn], AF.Copy, bias=MAGIC))
            k.op("dve", [fr_, ki_], [fr_], lambda e: e.scalar_tensor_tensor(fr_[:, 0:tn], kf_[:, 0:tn], MAGIC, fr_[:, 0:tn], ALU.subtract, ALU.subtract))
            k.op("act", [fr_], [S_], lambda e: e.activation(S_[:, 0:tn], fr_[:, 0:tn], AF.Sin, scale=-SIN_SC))
            k.op("act", [fr_], [sh_], lambda e: e.activation(sh_[:, 0:tn], fr_[:, 0:tn], AF.Sin, scale=PI_SC))
            k.op("act", [sh_], [sh_], lambda e: e.activation(sh_[:, 0:tn], sh_[:, 0:tn], AF.Square))
            k.op("act", [sh_], [C_], lambda e: e.activation(C_[:, 0:tn], sh_[:, 0:tn], AF.Copy, scale=-2.0, bias=1.0))

        def stage_A(i):
            d, g, bi, t0, tn = items[i]
            ch, q = g // 4, g % 4
            dc = d * 16 + ch
            ut = uT[dc % 2]
            p1, p2, b_, t_ = pp1[i % 2], pp2[i % 2], bq[i % 3], tq[i % 3]
            S_, C_ = S2q[i % ND], C2q[i % ND]
            if q < 3:
                ps_ = slice(32 * q, 32 * q + 32)
                k.op("pe", [BT1, ut], [p1], lambda e: e.matmul(p1[:, 0:tn], BT1[ps_, dc, :], ut[ps_, t0:t0 + tn], start=True, stop=True))
                k.op("pe", [BT2, ut], [p2], lambda e: e.matmul(p2[:, 0:tn], BT2[ps_, dc, :], ut[ps_, t0:t0 + tn], start=True, stop=True))
            else:
                k.op("pe", [BT1z, ut], [p1], lambda e: e.matmul(p1[:, 0:tn], BT1z[64:128, dc, :], ut[64:128, t0:t0 + tn], start=True, stop=True))
                k.op("pe", [BT2z, ut], [p2], lambda e: e.matmul(p2[:, 0:tn], BT2z[64:128, dc, :], ut[64:128, t0:t0 + tn], start=True, stop=True))
            k.op("dve", [p1, C_], [b_], lambda e: e.tensor_tensor(b_[:, 0:tn], p1[:, 0:tn], C_[:, 0:tn], ALU.mult))
            k.op("dve", [p2, S_], [t_], lambda e: e.tensor_tensor(t_[:, 0:tn], p2[:, 0:tn], S_[:, 0:tn], ALU.mult))
            k.op("pool", [b_, t_], [b_], lambda e: e.tensor_tensor(b_[:, 0:tn], b_[:, 0:tn], t_[:, 0:tn], ALU.add))

        def stage_B(i):
            d, g, bi, t0, tn = items[i]
            dg = d * 64 + g
            b_, w_, z1_, z2_ = bq[i % 3], wq[i % 3], z1q[i % 3], z2q[i % 3]
            S_, C_ = S2q[i % ND], C2q[i % ND]
            Rt_ = Rtq[dg % 2]
            if bi == 0:
                init = 0.0
                rd = [Rt_, b_]
            else:
                wp = wq[(i - 1) % 3]
                ptn = items[i - 1][4]
                init = wp[:, ptn - 1:ptn]
                rd = [Rt_, b_, wp]
            k.op("dve", rd, [w_], lambda e: e.tensor_tensor_scan(w_[:, 0:tn], Rt_[:, 0:tn], b_[:, 0:tn], init, ALU.mult, ALU.add))
            k.op("dve", [w_, C_], [z1_], lambda e: e.tensor_tensor(z1_[:, 0:tn], w_[:, 0:tn], C_[:, 0:tn], ALU.mult))
            k.op("pool", [w_, S_], [z2_], lambda e: e.tensor_tensor(z2_[:, 0:tn], w_[:, 0:tn], S_[:, 0:tn], ALU.mult))
            pys = psy[(dg % 2) * 2:(dg % 2) * 2 + 2]
            for tt in range(tn // 128):
                ti = t0 // 128 + tt
                py = pys[ti // 17]
                cc = (ti % 17) * 16
                k.op("pe", [z1_, M1], [py], lambda e: e.matmul(py[:, cc:cc + 16], z1_[:, tt * 128:(tt + 1) * 128], M1[:, dg, :], start=True, stop=False))
                k.op("pe", [z2_, M2], [py], lambda e: e.matmul(py[:, cc:cc + 16], z2_[:, tt * 128:(tt + 1) * 128], M2[:, dg, :], start=False, stop=True))
            if bi == len(TBLKS) - 1:
                yb = y8[(dg // 8) % 2]
                for half in range(2):
                    py = pys[half]
                    k.op("act", [py], [yb], lambda e: e.activation(yb[:, half * 17:(half + 1) * 17, (g % 8) * 16:(g % 8 + 1) * 16],
                                                                   py[:, 0:272].rearrange("p (t c) -> p t c", c=16), AF.Copy))
                if g % 8 == 7:
                    ydst = S["yF"] if d == 0 else S["yB"]
                    g0 = (g // 8) * 128
                    k.dma("pool", ydst[:, g0:g0 + 128].rearrange("(t p) c -> p t c", p=128), yb[:], [yb], [ydst])

        for i in range(NI + 2):
            if i < NI:
                stage_T(i)
            if 0 <= i - 1 < NI:
                stage_A(i - 1)
            if 0 <= i - 2 < NI:
                stage_B(i - 2)


def phase_glu(k, S, d_skip, w_glu, b_glu, idf, jf, idb, ntiles=32, gtabs=None):
    with k.phase():
        wg = k.sb("gu_wg", [128, 8, 1024], BF16)
        stg = [k.sb("gu_stg%d" % i, [128, 8, 128], F32) for i in range(2)]
        for cb in range(8):
            st = stg[cb % 2]
            k.dma("sp", st[:], w_glu[:, cb * 128:(cb + 1) * 128].rearrange("(j p) c -> p j c", p=128), [], [st])
            k.op("act", [st], [wg], lambda e: e.activation(wg[:, :, cb * 128:(cb + 1) * 128], st[:], AF.Copy))
        dsk = k.sb("gu_d", [128, 1024], F32)
        load_bc(k, "sp", dsk, d_skip[:], [d_skip])
        bg = k.sb("gu_bg", [1, 1024], F32)
        k.dma("sp", bg[:], b_glu[:], [b_glu], [bg])
        ones = k.sb("gu_ones", [1, 128], BF16)
        k.op("dve", [], [ones], lambda e: e.memset(ones[:], 1.0))
        bgb = k.sb("gu_bgb", [1, 1024], BF16)
        k.op("dve", [bg], [bgb], lambda e: e.tensor_copy(bgb[:], bg[:]))
        ggT = k.sb("gu_ggT", [128, 8, 4096], BF16)
        yf = [k.sb("gu_yf%d" % i, [128, 1024], F32) for i in range(2)]
        yb = [k.sb("gu_yb%d" % i, [128, 1024], F32) for i in range(2)]
        uu = [k.sb("gu_u%d" % i, [128, 1024], F32) for i in range(2)]
        t2 = [k.sb("gu_t%d" % i, [128, 1024], F32) for i in range(2)]
        gb = [k.sb("gu_gb%d" % i, [128, 1024], BF16) for i in range(2)]
        gT = [k.sb("gu_gT%d" % i, [128, 8, 128], BF16) for i in range(2)]
        psa = [k.ps("gu_psa%d" % i, [128, 512], F32) for i in range(2)]
        pst = [k.ps("gu_pst%d" % i, [128, 1024], BF16) for i in range(2)]
        psz = [k.ps("gu_psz%d" % i, [128, 512], F32) for i in range(2)]
        for i in range(ntiles):
            u = i % 2
            f_, b_, u_, t_, g_, gt_ = yf[u], yb[u], uu[u], t2[u], gb[u], gT[u]
            rows = slice(i * 128, (i + 1) * 128)
            if gtabs is None:
                k.dma("sp", f_[:], S["yF"][256 + i * 128:256 + (i + 1) * 128, :], [S["yF"]], [f_])
                r0 = NT - (i + 1) * 128
                k.dma("sp", b_[:], S["yB"][r0:r0 + 128, :], [S["yB"]], [b_])
                k.dma("sp", u_[:], S["u_tm"][rows, :], [S["u_tm"]], [u_])
            else:
                gF, gB, gN = gtabs
                k.idma_gather(f_[:], S["yF"][:], gF[:, i:i + 1], [S["yF"], gF], [f_])
                k.idma_gather(b_[:], S["yB"][:], gB[:, i:i + 1], [S["yB"], gB], [b_])
                k.idma_gather(u_[:], S["u_tm"][:], gN[:, i:i + 1], [S["u_tm"], gN], [u_])
            for n in range(2):
                k.op("pe", [f_, idf], [psa[n]], lambda e: e.matmul(psa[n][:], idf[:], f_[:, n * 512:(n + 1) * 512], start=True, stop=False))
                k.op("pe", [b_, jf], [psa[n]], lambda e: e.matmul(psa[n][:], jf[:], b_[:, n * 512:(n + 1) * 512], start=False, stop=True))
            k.op("pool", [u_, dsk], [u_], lambda e: e.tensor_tensor(u_[:], u_[:], dsk[:], ALU.mult))
            for n in range(2):
                k.op("dve", [psa[n], u_], [t_], lambda e: e.tensor_tensor(t_[:, n * 512:(n + 1) * 512], psa[n][:], u_[:, n * 512:(n + 1) * 512], ALU.add))
            k.op("pool", [t_], [f_], lambda e: e.tensor_tensor(f_[:], t_[:], t_[:], ALU.mult))
            k.op("dve", [f_], [f_], lambda e: e.tensor_scalar(f_[:], f_[:], 0.044715, 1.0, ALU.mult, ALU.add))
            k.op("pool", [f_, t_], [f_], lambda e: e.tensor_tensor(f_[:], f_[:], t_[:], ALU.mult))
            k.op("act", [f_], [f_], lambda e: e.activation(f_[:], f_[:], AF.Sigmoid, scale=1.5957691216057308))
            k.op("dve", [f_, t_], [t_], lambda e: e.tensor_tensor(t_[:], t_[:], f_[:], ALU.mult))
            k.op("pool", [t_], [g_], lambda e: e.tensor_copy(g_[:], t_[:]))
            for j in range(8):
                k.op("pe", [g_, idb], [pst[u]], lambda e: e.transpose(pst[u][:, j * 128:(j + 1) * 128], g_[:, j * 128:(j + 1) * 128], idb[:]))
            k.op("act", [pst[u]], [gt_], lambda e: e.activation(gt_[:], pst[u][:].rearrange("p (j t) -> p j t", j=8), AF.Copy))
            for n in range(2):
                for j in range(8):
                    k.op("pe", [gt_, wg], [psz[n]], lambda e: e.matmul(psz[n][:], gt_[:, j, :], wg[:, j, n * 512:(n + 1) * 512], start=(j == 0), stop=False))
                k.op("pe", [ones, bgb], [psz[n]], lambda e: e.matmul(psz[n][:], ones[0:1, :], bgb[0:1, n * 512:(n + 1) * 512], start=False, stop=True))
                k.op("act", [psz[n]], [f_], lambda e: e.activation(f_[:, n * 512:(n + 1) * 512], psz[n][:], AF.Sigmoid))
            k.op("dve", [f_, t_], [g_], lambda e: e.tensor_tensor(g_[:], t_[:], f_[:], ALU.mult))
            for j in range(8):
                k.op("pe", [g_, idb], [pst[u]], lambda e: e.transpose(pst[u][:, j * 128:(j + 1) * 128], g_[:, j * 128:(j + 1) * 128], idb[:]))
            k.op("act", [pst[u]], [ggT], lambda e: e.activation(ggT[:, :, i * 128:(i + 1) * 128], pst[u][:].rearrange("p (j t) -> p j t", j=8), AF.Copy))
        for j in range(8):
            k.dma("pool", S["ggT"][j, :, 0:ntiles * 128], ggT[:, j, 0:ntiles * 128], [ggT], [S["ggT"]])


def build_program():
    nc = bass.Bass("TRN2", target_bir_lowering=False)
    with ExitStack() as es:
        k = K(nc, es)
        dr = lambda n, s, dt=F32, kind="ExternalInput": k.dram(n, s, dt, kind=kind)
        xin = dr("xin", [NT, D])
        cT = dr("cT", [128, 32]); identf = dr("identf", [128, 128]); jmatf = dr("jmatf", [128, 128])
        ada_w = dr("ada_w", [2, D, 6 * D]); ada_b = dr("ada_b", [2, 6 * D])
        ev_w_in = dr("ev_w_in", [D, 6176]); gate_w2 = dr("gate_w2", [2, 16, 512]); gate_b = dr("gate_b", [2, 512])
        norm_g = dr("norm_g", [1, 256]); ropet = dr("ropet", [4096, 2, 256]); glac = dr("glac", [2, 3, 128, 128])
        biasg = dr("biasg", [8, 64, 8, 512]); ev_w_out = dr("ev_w_out", [D, D])
        ln_mix_g = dr("ln_mix_g", [2, D]); ln_mix_b = dr("ln_mix_b", [2, D]); ln_ffn_g = dr("ln_ffn_g", [2, D]); ln_ffn_b = dr("ln_ffn_b", [2, D])
        router_w = dr("router_w", [D, 16]); router_b = dr("router_b", [1, 16])
        w_gate = dr("w_gate", [2, 16, D, 1024]); w_up = dr("w_up", [2, 16, D, 1024]); w_down = dr("w_down", [2, 16, 1024, D])
        od_w_in = dr("od_w_in", [D, 1024]); od_w_glu = dr("od_w_glu", [1024, 1024]); od_b_glu = dr("od_b_glu", [1, 1024])
        od_w_out = dr("od_w_out", [1024, D]); od_d = dr("od_d", [1, 1024])
        s5pa = dr("s5pa", [128, 5, 2048]); s5pb = dr("s5pb", [128, 3, 128]); s5c1 = dr("s5c1", [128, 128, 16]); s5c2 = dr("s5c2", [128, 128, 16])
        yout = dr("yout", [2048, D], kind="ExternalOutput")
        idxtab = dr("idxtab", [128, 48], mybir.dt.uint32)
        xloc = dr("xloc", [2048, D], kind="Internal")
        modrow = dr("modrow", [2, 2, 6 * D], kind="Internal")
        xall = dr("xall", [NT, D], kind="Internal")
        S = {}
        for n, s, dt in (("qaT", [8, 128, NT], BF16), ("kaT", [8, 128, NT], BF16), ("va", [NT, 1024], BF16), ("qb", [NT, 512], F32),
                         ("kb", [NT, 512], F32), ("qbr", [NT, 512], F32), ("kbr", [NT, 512], F32), ("sp", [2, NT, 512], F32),
                         ("ob", [2, NT, 1024], F32), ("vb", [NT, 1024], BF16), ("rb", [NT, 1024], F32), ("lrT", [2, 16, NT], F32),
                         ("mixT", [16, 128, NT], BF16), ("hT_moe", [NTILE, 128, 16, 128], BF16), ("comb", [NT, 16], F32),
                         ("u_tm", [4096, 1024], F32), ("uTf", [16, 128, NT], BF16), ("uTb", [16, 128, NT], BF16),
                         ("yF", [NT, 1024], F32), ("yB", [NT, 1024], F32), ("ggT", [8, 128, NT], BF16)):
            S[n] = dr(n, s, dt, kind="Internal")
        idf = k.sb("idf", [128, 128], F32); idb = k.sb("idb", [128, 128], BF16)
        jf = k.sb("jf", [128, 128], F32); jb = k.sb("jb", [128, 128], BF16)
        k.dma("sp", idf[:], identf[:], [identf], [idf])
        k.dma("sp", jf[:], jmatf[:], [jmatf], [jf])
        k.op("dve", [idf], [idb], lambda e: e.tensor_copy(idb[:], idf[:]))
        k.op("dve", [jf], [jb], lambda e: e.tensor_copy(jb[:], jf[:]))
        for i in range(NTILE):
            k.dma("sp", xall[i * 128:(i + 1) * 128, :], xin[i * 128:(i + 1) * 128, :], [xin], [xall])
        phase_mod(k, 0, ada_w, ada_b, cT, modrow)
        with k.phase():
            hT_all = k.sb("hT_all", [128, 16, NT], BF16)
            build_hT(k, 0, xall, modrow, hT_all, idb, 0, 1)
            phase_inproj0(k, hT_all, ev_w_in, S)
        phase_na(k, S, biasg, idb)
        phase_gla_prep(k, S, gate_w2, gate_b, ropet)
        phase_gla(k, S, glac, idf)
        phase_gatenorm(k, S, norm_g, idb)
        import os
        STOP = int(os.environ.get("MK_STOP", "99"))
        SKIP0 = int(os.environ.get("MK_SKIP0", "0"))
        post_mixer(k, 0, 16, S["mixT"], ev_w_out, xall, modrow, ln_mix_g, ln_mix_b, router_w, router_b, S, idf, NTILE)
        if STOP == 1:
            k.finish(); return nc
        phase_moe(k, 0, [9, 9, 8, 8] if STOP > 2 else [2], w_gate, w_up, w_down, xall, modrow, ln_ffn_g, ln_ffn_b, S, xall)
        if STOP == 2:
            k.finish(); return nc
        phase_mod(k, 1, ada_w, ada_b, cT, modrow)
        with k.phase():
            hT_all = k.sb("hT_all1", [128, 16, NT], BF16)
            build_hT(k, 1, xall, modrow, hT_all, idb, 0, 1)
            phase_l1_inproj(k, hT_all, od_w_in, S, idb, jb)
        if STOP == 3:
            k.finish(); return nc
        phase_s5(k, S, s5pa, s5pb, s5c1, s5c2)
        if STOP == 4:
            k.finish(); return nc
        gidx = k.sb("gidx", [128, 48], mybir.dt.uint32)
        k.dma("sp", gidx[:], idxtab[:], [idxtab], [gidx])
        gN = TT(gidx[:, 0:16], gidx.res); gF = TT(gidx[:, 16:32], gidx.res); gB = TT(gidx[:, 32:48], gidx.res)
        phase_glu(k, S, od_d, od_w_glu, od_b_glu, idf, jf, idb, ntiles=16, gtabs=(gF, gB, gN))
        if STOP == 5:
            k.finish(); return nc
        post_mixer(k, 1, 8, S["ggT"], od_w_out, xall, modrow, ln_mix_g, ln_mix_b, router_w, router_b, S, idf, 16, xg=gN, xout=xloc)
        phase_moe(k, 1, [8, 8], w_gate, w_up, w_down, xloc, modrow, ln_ffn_g, ln_ffn_b, S, yout)
        k.finish()
    return nc


def kernel(x, c, ctx, c_ctx, ada_w, ada_b, ln_mix_g, ln_mix_b, ln_ffn_g, ln_ffn_b,
           ev_w_in, ev_gate_w2, ev_gate_b, ev_rpb, ev_norm_g, ev_w_out,
           od_w_in, od_lam_re, od_lam_im, od_log_dt, od_b_re, od_b_im, od_c_re, od_c_im, od_d,
           od_w_glu, od_b_glu, od_w_out, router_w, router_b, moe_w_gate, moe_w_up, moe_w_down):
    import os
    f = lambda a: np.ascontiguousarray(np.asarray(a, dtype=np.float32))
    x, c, ctx, c_ctx = f(x), f(c), f(ctx), f(c_ctx)
    pa, pb, c1, c2 = s5_host_params(f(od_lam_re)[0], f(od_lam_im)[0], f(od_log_dt)[0], f(od_b_re)[0], f(od_b_im)[0], f(od_c_re)[0], f(od_c_im)[0])
    shared = {"identf": np.eye(128, dtype=np.float32), "jmatf": np.ascontiguousarray(np.eye(128, dtype=np.float32)[::-1]),
              "ada_w": f(ada_w), "ada_b": f(ada_b), "ev_w_in": f(ev_w_in)[0],
              "gate_w2": f(ev_gate_w2)[0], "gate_b": f(ev_gate_b)[0], "norm_g": f(ev_norm_g)[0][None], "ropet": rope_tables_host(), "glac": gla_consts_host(),
              "biasg": na_bias_host(f(ev_rpb)[0]), "ev_w_out": f(ev_w_out)[0], "ln_mix_g": f(ln_mix_g), "ln_mix_b": f(ln_mix_b),
              "ln_ffn_g": f(ln_ffn_g), "ln_ffn_b": f(ln_ffn_b), "router_w": f(router_w), "router_b": f(router_b)[None],
              "w_gate": f(moe_w_gate), "w_up": f(moe_w_up), "w_down": f(moe_w_down),
              "od_w_in": f(od_w_in)[0], "od_w_glu": f(od_w_glu)[0], "od_b_glu": f(od_b_glu)[0][None], "od_w_out": f(od_w_out)[0],
              "od_d": f(od_d)[0].reshape(1, 1024), "s5pa": pa, "s5pb": pb, "s5c1": c1, "s5c2": c2}
    ncores = int(os.environ.get("MK_NCORES", "8"))
    in_maps = []
    for core in range(ncores):
        b = core % 4
        m = dict(shared)
        m["xin"] = np.ascontiguousarray(np.concatenate([x[b], ctx[b]], 0))
        m["cT"] = np.ascontiguousarray(np.concatenate([c[b].reshape(16, 128).T, c_ctx.reshape(16, 128).T], 1))
        half = core // 4
        gi = (half * 16 + np.arange(16))[None, :]
        pp = np.arange(128)[:, None]
        m["idxtab"] = np.ascontiguousarray(np.concatenate([gi * 128 + pp, 256 + gi * 128 + pp, NT - (gi + 1) * 128 + pp], 1).astype(np.uint32))
        in_maps.append(m)
    nc = build_program()
    res = run_bass_kernel_spmd(nc, in_maps, core_ids=list(range(ncores)))
    out = np.zeros((4, 4096, D), np.float32)
    for core in range(ncores):
        b, half = core % 4, core // 4
        out[b, half * 2048:(half + 1) * 2048] = res.results[core]["yout"]
    return out
```

```python
import numpy as np
import concourse.bass as bass
import concourse.mybir as mybir
from concourse.bass_utils import run_bass_kernel_spmd
from contextlib import ExitStack

F32 = mybir.dt.float32
BF16 = mybir.dt.bfloat16
I32 = mybir.dt.int32
AF = mybir.ActivationFunctionType
ALU = mybir.AluOpType
AX = mybir.AxisListType


class Res:
    __slots__ = ("name", "w", "r")

    def __init__(self, name=""):
        self.name = name
        self.w = None
        self.r = {}


class TT:
    def __init__(self, t, res=None, name=""):
        self.t = t
        self.res = res if res is not None else Res(name)

    def __getitem__(self, idx):
        return self.t[idx]


class EngS:
    def __init__(self, name, obj, sem):
        self.name = name
        self.obj = obj
        self.sem = sem
        self.count = 0
        self.waited = {}


class K:
    def __init__(self, nc, es, n_dma_sems=12):
        self.nc = nc
        self.es = es
        self.sems = {}
        self.engs = {}
        for name, obj in (("pe", nc.tensor), ("act", nc.scalar), ("dve", nc.vector),
                          ("pool", nc.gpsimd), ("sp", nc.sync)):
            s = es.enter_context(nc.semaphore("s_" + name))
            self.sems[name] = s
            self.engs[name] = EngS(name, obj, s)
        self.dq = {}
        for q in ("sp", "pool", "act"):
            pool = []
            for i in range(n_dma_sems):
                key = "d_%s_%d" % (q, i)
                s = es.enter_context(nc.semaphore(key))
                self.sems[key] = s
                pool.append([key, 0])
            self.dq[q] = [pool, 0]
        self.nid = 0
        self.out_tokens = []

    def sb(self, name, shape, dtype):
        self.nid += 1
        name = "%s_%d" % (name, self.nid)
        t = self.es.enter_context(self.nc.sbuf_tensor(name, list(shape), dtype))
        return TT(t, name=name)

    def ps(self, name, shape, dtype=F32):
        self.nid += 1
        name = "%s_%d" % (name, self.nid)
        t = self.es.enter_context(self.nc.psum_tensor(name, list(shape), dtype))
        return TT(t, name=name)

    def dram(self, name, shape, dtype, kind="Internal"):
        t = self.nc.dram_tensor(name, list(shape), dtype, kind=kind)
        return TT(t, name=name)

    def _wait(self, E, key, val):
        if E.waited.get(key, 0) >= val:
            return
        E.obj.wait_ge(self.sems[key], val)
        E.waited[key] = val

    def _deps(self, E, reads, writes):
        toks = {}
        def add(tok):
            if tok is None:
                return
            k, v = tok
            if toks.get(k, 0) < v:
                toks[k] = v
        for r in reads:
            add(r.w)
        for w in writes:
            add(w.w)
            for k, v in w.r.items():
                add((k, v))
        for k, v in toks.items():
            if k == E.name and E.name == "pe":
                continue
            self._wait(E, k, v)

    def _mark(self, tok, reads, writes):
        k, v = tok
        for r in reads:
            if r.r.get(k, 0) < v:
                r.r[k] = v
        for w in writes:
            w.w = tok
            w.r = {}

    @staticmethod
    def _res(xs):
        out = []
        for x in xs:
            if x is None:
                continue
            out.append(x.res if isinstance(x, TT) else x)
        return out

    def op(self, eng, reads, writes, fn):
        E = self.engs[eng]
        reads = self._res(reads)
        writes = self._res(writes)
        self._deps(E, reads, writes)
        ins = fn(E.obj)
        E.count += 1
        ins.then_inc(E.sem, 1)
        self._mark((E.name, E.count), reads, writes)
        return ins

    def dma(self, q, out_ap, in_ap, reads, writes, is_output=False, **kw):
        E = self.engs[q]
        reads = self._res(reads)
        writes = self._res(writes)
        pool, idx = self.dq[q]
        slot = pool[idx % len(pool)]
        self.dq[q][1] = idx + 1
        key, cnt = slot
        if cnt > 0:
            self._wait(E, key, cnt)
        self._deps(E, reads, writes)
        ins = E.obj.dma_start(out=out_ap, in_=in_ap, **kw)
        slot[1] = cnt + 16
        ins.then_inc(self.sems[key], 16)
        tok = (key, cnt + 16)
        self._mark(tok, reads, writes)
        if is_output:
            self.out_tokens.append(tok)
        return tok

    def idma_gather(self, out_ap, in_ap, idx_ap, reads, writes):
        E = self.engs["pool"]
        reads = self._res(reads)
        writes = self._res(writes)
        pool, idx = self.dq["pool"]
        slot = pool[idx % len(pool)]
        self.dq["pool"][1] = idx + 1
        key, cnt = slot
        if cnt > 0:
            self._wait(E, key, cnt)
        self._deps(E, reads, writes)
        ins = E.obj.indirect_dma_start(out=out_ap, out_offset=None, in_=in_ap,
                                       in_offset=bass.IndirectOffsetOnAxis(ap=idx_ap, axis=0))
        slot[1] = cnt + 16
        ins.then_inc(self.sems[key], 16)
        self._mark((key, cnt + 16), reads, writes)

    def finish(self):
        E = self.engs["sp"]
        for q in self.dq:
            for key, cnt in self.dq[q][0]:
                if cnt > 0:
                    self._wait(E, key, cnt)
        for n in ("pe", "act", "dve", "pool"):
            En = self.engs[n]
            if En.count > 0:
                self._wait(E, n, En.count)

from contextlib import contextmanager

def barrier(k):
    names = ("pe", "act", "dve", "pool", "sp")
    for n in names:
        E = k.engs[n]
        for m in names:
            if m != n and k.engs[m].count > 0:
                k._wait(E, m, k.engs[m].count)
        for q in k.dq:
            for key, cnt in k.dq[q][0]:
                if cnt > 0:
                    k._wait(E, key, cnt)

@contextmanager
def phase(k):
    old = k.es
    with ExitStack() as es:
        k.es = es
        try:
            yield
        finally:
            barrier(k)
            k.es = old
K.phase = phase
K.barrier = barrier


D = 2048
NT = 4352
NTILE = 34
TBLKS = [(i * 512, 512) for i in range(8)] + [(4096, 256)]
ALPHA = (2.0 * 2) ** 0.25
LN_EPS = 1e-5


def phase_mod(k, layer, ada_w, ada_b, cT, modrow):
    with k.phase():
        cs = k.sb("cs", [128, 32], F32)
        sc = k.sb("sc", [128, 32], F32)
        ones = k.sb("ones1", [1, 128], F32)
        k.dma("sp", cs[:], cT[:], [cT], [cs])
        k.op("act", [cs], [sc], lambda e: e.activation(sc[:], cs[:], AF.Silu))
        k.op("dve", [], [ones], lambda e: e.memset(ones[:], 1.0))
        NB = 6
        wbuf = [k.sb("adw%d" % i, [128, 512], F32) for i in range(NB)]
        bbuf = [k.sb("adb%d" % i, [1, 512], F32) for i in range(2)]
        rbuf = [k.sb("adr%d" % i, [1, 1024], F32) for i in range(2)]
        pss = [k.ps("adp%d" % i, [1, 1024], F32) for i in range(2)]
        cnt = 0
        for nt in range(24):
            ps = pss[nt % 2]
            bb = bbuf[nt % 2]
            rb = rbuf[nt % 2]
            k.dma("sp", bb[:], ada_b[layer:layer + 1, nt * 512:(nt + 1) * 512], [ada_b], [bb])
            for j in range(16):
                wt = wbuf[cnt % NB]
                cnt += 1
                k.dma("sp", wt[:], ada_w[layer, j * 128:(j + 1) * 128, nt * 512:(nt + 1) * 512], [ada_w], [wt])
                k.op("pe", [sc, wt], [ps], lambda e: e.matmul(ps[0:1, 0:512], sc[:, j:j + 1], wt[:], start=(j == 0), stop=False))
                k.op("pe", [sc, wt], [ps], lambda e: e.matmul(ps[0:1, 512:1024], sc[:, 16 + j:17 + j], wt[:], start=(j == 0), stop=False))
            k.op("pe", [ones, bb], [ps], lambda e: e.matmul(ps[0:1, 0:512], ones[0:1, 0:1], bb[:], start=False, stop=True))
            k.op("pe", [ones, bb], [ps], lambda e: e.matmul(ps[0:1, 512:1024], ones[0:1, 0:1], bb[:], start=False, stop=True))
            seg = nt // 4
            addv = 1.0 if seg in (1, 4) else 0.0
            k.op("dve", [ps], [rb], lambda e: e.tensor_scalar_add(rb[:], ps[:], addv))
            k.dma("pool", modrow[layer, 0:1, nt * 512:(nt + 1) * 512], rb[0:1, 0:512], [rb], [modrow])
            k.dma("pool", modrow[layer, 1:2, nt * 512:(nt + 1) * 512], rb[0:1, 512:1024], [rb], [modrow])


def load_bc(k, q, dst, row_ap, deps):
    k.dma(q, dst[:], row_ap.partition_broadcast(128), deps, [dst])


def build_hT(k, layer, xcur, modrow, hT_all, ident_b, seg_shift, seg_scale):
    with k.phase():
        mA = k.sb("mA", [128, D], F32)
        mB = k.sb("mB", [128, D], F32)
        xt = [k.sb("xt%d" % i, [128, D], F32) for i in range(2)]
        hb = [k.sb("hb%d" % i, [128, D], BF16) for i in range(2)]
        ptr = [k.ps("ptr%d" % i, [128, D], BF16) for i in range(2)]
        for i in range(NTILE):
            if i == 0 or i == 32:
                r = 0 if i == 0 else 1
                load_bc(k, "sp", mA, modrow[layer, r:r + 1, seg_scale * D:(seg_scale + 1) * D], [modrow])
                load_bc(k, "sp", mB, modrow[layer, r:r + 1, seg_shift * D:(seg_shift + 1) * D], [modrow])
            x_ = xt[i % 2]
            h_ = hb[i % 2]
            p_ = ptr[i % 2]
            k.dma("sp", x_[:], xcur[i * 128:(i + 1) * 128, :], [xcur], [x_])
            k.op("dve", [x_, mA], [x_], lambda e: e.tensor_tensor(x_[:], x_[:], mA[:], ALU.mult))
            k.op("pool", [x_, mB], [h_], lambda e: e.tensor_tensor(h_[:], x_[:], mB[:], ALU.add))
            for j in range(16):
                k.op("pe", [h_, ident_b], [p_], lambda e: e.transpose(p_[:, j * 128:(j + 1) * 128], h_[:, j * 128:(j + 1) * 128], ident_b[:]))
            k.op("act", [p_], [hT_all], lambda e: e.activation(
                hT_all[:, :, i * 128:(i + 1) * 128], p_[:].rearrange("p (j t) -> p j t", j=16), AF.Copy))


def load_w_block(k, w_ap, stage, wb, ncols, conv_eng="pool"):
    k.dma("sp", stage[:, :, 0:ncols], w_ap.rearrange("(j p) c -> p j c", p=128), [], [stage])
    k.op("act", [stage], [wb], lambda e: e.activation(wb[:, :, 0:ncols], stage[:, :, 0:ncols], AF.Copy))


def phase_inproj0(k, hT_all, ev_w_in, S):
    with k.phase():
        stg = [k.sb("wst%d" % i, [128, 16, 128], F32) for i in range(2)]
        wbs = [k.sb("wbb%d" % i, [128, 16, 128], BF16) for i in range(2)]
        obuf_b = k.sb("obufb", [128, NT], BF16)
        obuf_f = k.sb("obuff", [128, NT], F32)
        pss = [k.ps("ipp%d" % i, [128, 512], F32) for i in range(4)]
        pc = 0
        for cb in range(49):
            st = stg[cb % 2]
            wb = wbs[cb % 2]
            ncols = 128 if cb < 48 else 32
            load_w_block(k, ev_w_in[:, cb * 128:cb * 128 + ncols], st, wb, ncols)
            if cb < 16:
                fo = obuf_b
                for (t0, tn) in TBLKS:
                    ps = pss[pc % 4]; pc += 1
                    for j in range(16):
                        k.op("pe", [wb, hT_all], [ps], lambda e: e.matmul(ps[:, 0:tn], wb[:, j, :], hT_all[:, j, t0:t0 + tn], start=(j == 0), stop=(j == 15)))
                    k.op("act", [ps], [fo], lambda e: e.activation(fo[:, t0:t0 + tn], ps[:, 0:tn], AF.Copy))
                dst = S["qaT"] if cb < 8 else S["kaT"]
                k.dma("pool", dst[cb % 8], fo[:], [fo], [dst])
            elif cb < 48:
                isb = (16 <= cb < 24) or (32 <= cb < 40)
                tor = obuf_b if isb else obuf_f
                to = tor[:].rearrange("p (i c) -> p i c", c=128)
                for i4 in range(0, NTILE, 4):
                    n4 = min(4, NTILE - i4)
                    ps = pss[pc % 4]; pc += 1
                    for ii in range(n4):
                        i = i4 + ii
                        for j in range(16):
                            k.op("pe", [wb, hT_all], [ps], lambda e: e.matmul(ps[:, ii * 128:(ii + 1) * 128], hT_all[:, j, i * 128:(i + 1) * 128], wb[:, j, :], start=(j == 0), stop=(j == 15)))
                    eng = "act" if (i4 // 4) % 2 == 0 else "dve"
                    if eng == "act":
                        k.op("act", [ps], [tor], lambda e: e.activation(to[:, i4:i4 + n4, :], ps[:, 0:n4 * 128].rearrange("p (a b) -> p a b", a=n4), AF.Copy))
                    else:
                        k.op("dve", [ps], [tor], lambda e: e.tensor_copy(to[:, i4:i4 + n4, :], ps[:, 0:n4 * 128].rearrange("p (a b) -> p a b", a=n4)))
                if cb < 24:
                    dst, c0 = S["va"], (cb - 16) * 128
                elif cb < 28:
                    dst, c0 = S["qb"], (cb - 24) * 128
                elif cb < 32:
                    dst, c0 = S["kb"], (cb - 28) * 128
                elif cb < 40:
                    dst, c0 = S["vb"], (cb - 32) * 128
                else:
                    dst, c0 = S["rb"], (cb - 40) * 128
                k.dma("pool", dst[:, c0:c0 + 128].rearrange("(i p) c -> p i c", p=128), to, [tor], [dst])
            else:
                for d in range(2):
                    for (t0, tn) in TBLKS:
                        ps = pss[pc % 4]; pc += 1
                        for j in range(16):
                            k.op("pe", [wb, hT_all], [ps], lambda e: e.matmul(ps[0:16, 0:tn], wb[:, j, d * 16:(d + 1) * 16], hT_all[:, j, t0:t0 + tn], start=(j == 0), stop=(j == 15)))
                        k.op("act", [ps], [obuf_f], lambda e: e.activation(obuf_f[0:16, t0:t0 + tn], ps[0:16, 0:tn], AF.Copy))
                    k.dma("pool", S["lrT"][d], obuf_f[0:16, :], [obuf_f], [S["lrT"]])


def phase_na(k, S, biasg, idb):
    scale = 128 ** -0.5
    with k.phase():
        HB = 2
        qT = [k.sb("na_qT%d" % i, [128, NT], BF16) for i in range(HB)]
        kT = [k.sb("na_kT%d" % i, [128, NT], BF16) for i in range(HB)]
        Ve = [k.sb("na_Ve%d" % i, [128, 34, 128], BF16) for i in range(HB)]
        Vo = [k.sb("na_Vo%d" % i, [128, 31, 128], BF16) for i in range(HB)]
        bias = [k.sb("na_bias%d" % i, [64, 8, 512], F32) for i in range(HB)]
        aT = [k.sb("na_aT%d" % i, [128, NT], BF16) for i in range(HB)]
        DP = 4
        Ssb = [k.sb("na_S%d" % i, [128, 768], F32) for i in range(DP)]
        Pn = [k.sb("na_P%d" % i, [128, 768], BF16) for i in range(DP)]
        PT = [k.sb("na_PT%d" % i, [128, 768], BF16) for i in range(DP)]
        st = [k.sb("na_st%d" % i, [128, 4], F32) for i in range(DP)]
        ps_s = [k.ps("na_pss%d" % i, [128, 512], F32) for i in range(2)]
        ps_c = [k.ps("na_psc%d" % i, [128, 512], F32) for i in range(2)]
        ps_t = [k.ps("na_pst%d" % i, [128, 1024], BF16) for i in range(2)]
        ps_o = [k.ps("na_pso%d" % i, [128, 512], F32) for i in range(2)]
        va = S["va"]
        items = [(h, r) for h in range(8) for r in range(66)]
        NI = len(items)

        def geom(r):
            if r < 64:
                rs = min(max(r - 4, 0), 56)
                pat = r if r < 4 else (4 if r <= 60 else r - 56)
                return 64, r * 64, 768, rs, pat
            return 128, 4096 + (r - 64) * 128, 256, 0, 0

        def s1a(i):
            h, r = items[i]
            hb = h % HB
            if r == 0:
                k.dma("sp", qT[hb][:], S["qaT"][h], [S["qaT"]], [qT[hb]])
                k.dma("sp", kT[hb][:], S["kaT"][h], [S["kaT"]], [kT[hb]])
                k.dma("sp", Ve[hb][:], va[:, h * 128:(h + 1) * 128].rearrange("(c p) d -> p c d", p=128), [va], [Ve[hb]])
                k.dma("sp", Vo[hb][:], va[64:64 + 31 * 128, h * 128:(h + 1) * 128].rearrange("(c p) d -> p c d", p=128), [va], [Vo[hb]])
                k.dma("sp", bias[hb][:], biasg[h], [biasg], [bias[hb]])
            nq, q0, nk, rs, pat = geom(r)
            S_, st_ = Ssb[i % DP], st[i % DP]
            pss, psc = ps_s[i % 2], ps_c[i % 2]
            q_, k_ = qT[hb], kT[hb]
            if r < 64:
                k.op("pe", [q_, k_], [pss], lambda e: e.matmul(pss[0:64, :], q_[:, q0:q0 + 64], k_[:, rs * 64:rs * 64 + 512], start=True, stop=True))
                k.op("pe", [q_, k_], [psc], lambda e: e.matmul(psc[0:64, 0:256], q_[:, q0:q0 + 64], k_[:, 4096:4352], start=True, stop=True))
                k.op("dve", [pss, bias[hb]], [S_], lambda e: e.scalar_tensor_tensor(S_[0:64, 0:512], pss[0:64, :], scale, bias[hb][:, pat, :], ALU.mult, ALU.add))
                k.op("act", [psc], [S_], lambda e: e.activation(S_[0:64, 512:768], psc[0:64, 0:256], AF.Copy, scale=scale))
            else:
                k.op("pe", [q_, k_], [psc], lambda e: e.matmul(psc[:, 0:256], q_[:, q0:q0 + 128], k_[:, 4096:4352], start=True, stop=True))
                k.op("act", [psc], [S_], lambda e: e.activation(S_[:, 0:256], psc[:, 0:256], AF.Copy, scale=scale))
            k.op("dve", [S_], [st_], lambda e: e.reduce_max(st_[0:nq, 0:1], S_[0:nq, 0:nk], AX.X, negate=True))

        def s1b(i):
            h, r = items[i]
            nq, q0, nk, rs, pat = geom(r)
            S_, P_, st_ = Ssb[i % DP], Pn[i % DP], st[i % DP]
            k.op("act", [S_, st_], [P_, st_], lambda e: e.activation(P_[0:nq, 0:nk], S_[0:nq, 0:nk], AF.Exp, bias=st_[0:nq, 0:1], accum_out=st_[0:nq, 1:2]))
            k.op("dve", [st_], [st_], lambda e: e.reciprocal(st_[0:nq, 2:3], st_[0:nq, 1:2]))
            k.op("dve", [P_, st_], [P_], lambda e: e.tensor_scalar_mul(P_[0:nq, 0:nk], P_[0:nq, 0:nk], st_[0:nq, 2:3]))

        def s2(i):
            h, r = items[i]
            P_, PT_ = Pn[i % DP], PT[i % DP]
            pst = ps_t[i % 2]
            if r < 64:
                for c in range(6):
                    k.op("pe", [P_, idb], [pst], lambda e: e.transpose(pst[:, c * 64:(c + 1) * 64], P_[0:64, c * 128:(c + 1) * 128], idb[0:64, 0:64]))
                k.op("act", [pst], [PT_], lambda e: e.activation(PT_[:, 0:384], pst[:, 0:384], AF.Copy))
            else:
                for c in range(2):
                    k.op("pe", [P_, idb], [pst], lambda e: e.transpose(pst[:, c * 128:(c + 1) * 128], P_[:, c * 128:(c + 1) * 128], idb[:]))
                k.op("act", [pst], [PT_], lambda e: e.activation(PT_[:, 0:256], pst[:, 0:256], AF.Copy))

        def s3(i):
            h, r = items[i]
            hb = h % HB
            nq, q0, nk, rs, pat = geom(r)
            PT_ = PT[i % DP]
            pso = ps_o[i % 2]
            Ve_, Vo_, aT_ = Ve[hb], Vo[hb], aT[hb]
            if r < 64:
                for c in range(6):
                    if c < 4:
                        vch = Ve_[:, rs // 2 + c, :] if rs % 2 == 0 else Vo_[:, (rs - 1) // 2 + c, :]
                    else:
                        vch = Ve_[:, 32 + (c - 4), :]
                    k.op("pe", [Ve_, Vo_, PT_], [pso], lambda e: e.matmul(pso[:, 0:64], vch, PT_[:, c * 64:(c + 1) * 64], start=(c == 0), stop=(c == 5)))
                k.op("dve", [pso], [aT_], lambda e: e.tensor_copy(aT_[:, q0:q0 + 64], pso[:, 0:64]))
            else:
                for c in range(2):
                    k.op("pe", [Ve_, PT_], [pso], lambda e: e.matmul(pso[:, 0:128], Ve_[:, 32 + c, :], PT_[:, c * 128:(c + 1) * 128], start=(c == 0), stop=(c == 1)))
                k.op("dve", [pso], [aT_], lambda e: e.tensor_copy(aT_[:, q0:q0 + 128], pso[:, 0:128]))
            if r == 65:
                k.dma("pool", S["mixT"][h], aT_[:], [aT_], [S["mixT"]])

        for i in range(NI + 3):
            if i < NI:
                s1a(i)
            if 0 <= i - 1 < NI:
                s1b(i - 1)
            if 0 <= i - 2 < NI:
                s2(i - 2)
            if 0 <= i - 3 < NI:
                s3(i - 3)


def na_bias_host(rpb):
    ext = np.concatenate([rpb.reshape(8, -1), np.full((8, 1), -30000.0, np.float32)], axis=1)
    idx = np.full((8, 64, 8 * 64), 15 * 31, np.int64)
    pats = [0, 1, 2, 3, 4, 61, 62, 63]
    for pi, r in enumerate(pats):
        rs = min(max(r - 4, 0), 56)
        for i in range(8):
            dr = rs + i - r + 7
            for q in range(64):
                w0 = min(max(q - 8, 0), 48)
                for kc in range(w0, w0 + 16):
                    idx[pi, q, i * 64 + kc] = dr * 31 + (kc - q + 15)
    g = ext[:, idx]
    return np.ascontiguousarray(g.transpose(0, 2, 1, 3)).astype(np.float32)


def gla_consts_host():
    out = np.zeros((2, 3, 128, 128), np.float32)
    s = np.arange(128)[:, None]
    t = np.arange(128)[None, :]
    same = (s // 64) == (t // 64)
    v = -1.0 / 16.0
    out[0, 0] = np.where(same & (s <= t), v, 0)
    out[0, 1] = np.where(same & (s > t), v, 0)
    out[0, 2] = np.where(same & (t >= s), 1.0, 0)
    out[1, 0] = np.where(same & (s >= t), v, 0)
    out[1, 1] = np.where(same & (s < t), v, 0)
    out[1, 2] = np.where(same & (t <= s), 1.0, 0)
    return out


def rope_tables_host():
    t = np.arange(4096)
    nf = 32
    inv = (10000.0 ** (-np.arange(nf, dtype=np.float32) / nf)).astype(np.float32)
    row = (t // 64).astype(np.float32)[:, None] * inv
    col = (t % 64).astype(np.float32)[:, None] * inv
    ang = np.stack([row, col], 1)
    c = np.cos(ang).astype(np.float32)
    s = np.sin(ang).astype(np.float32)
    c = np.broadcast_to(c[:, None], (4096, 4, 2, 32)).reshape(4096, 256)
    s = np.broadcast_to(s[:, None], (4096, 4, 2, 32)).reshape(4096, 256)
    return np.ascontiguousarray(np.stack([c, s], 1)).astype(np.float32)


def phase_gla_prep(k, S, gate_w2, gate_b, ropet):
    gscale = 128 ** -0.5
    with k.phase():
        gw = k.sb("gp_gw", [16, 2, 512], F32)
        gb = k.sb("gp_gb", [1, 2, 512], F32)
        ones = k.sb("gp_ones", [1, 128], F32)
        k.dma("sp", gw[:], gate_w2[:].rearrange("d r c -> r d c"), [gate_w2], [gw])
        k.dma("sp", gb[:], gate_b[:].rearrange("(o d) c -> o d c", o=1), [gate_b], [gb])
        k.op("dve", [], [ones], lambda e: e.memset(ones[:], 1.0))
        qk = [k.sb("gp_qk%d" % i, [128, 2, 512], F32) for i in range(2)]
        qo = [k.sb("gp_qo%d" % i, [128, 2, 512], F32) for i in range(2)]
        cs = [k.sb("gp_cs%d" % i, [128, 2, 256], F32) for i in range(2)]
        tmp = [k.sb("gp_tmp%d" % i, [128, 4, 256], F32) for i in range(2)]
        lr = [k.sb("gp_lr%d" % i, [16, 2, 128], F32) for i in range(2)]
        ee = [k.sb("gp_e%d" % i, [128, 512], F32) for i in range(2)]
        spo = [k.sb("gp_sp%d" % i, [128, 512], F32) for i in range(2)]
        psz = [k.ps("gp_ps%d" % i, [128, 512], F32) for i in range(2)]
        n = 0
        for i in range(NTILE):
            u = i % 2
            q_, o_, c_, t_ = qk[u], qo[u], cs[u], tmp[u]
            rows = slice(i * 128, (i + 1) * 128)
            k.dma("sp", q_[:, 0, :], S["qb"][rows, :], [S["qb"]], [q_])
            k.dma("sp", q_[:, 1, :], S["kb"][rows, :], [S["kb"]], [q_])
            k.op("act", [q_], [q_], lambda e: e.mul(q_[:, 0, :], q_[:, 0, :], gscale))
            if i < 32:
                k.dma("sp", c_[:], ropet[rows], [ropet], [c_])
                for a in range(2):
                    xv = q_[:, a, :].rearrange("p (h a b f) -> p h a b f", h=4, a=2, b=2)
                    ov = o_[:, a, :].rearrange("p (h a b f) -> p h a b f", h=4, a=2, b=2)
                    x1, x2 = xv[:, :, :, 0, :], xv[:, :, :, 1, :]
                    C = c_[:, 0, :].rearrange("p (h a f) -> p h a f", h=4, a=2)
                    Sn = c_[:, 1, :].rearrange("p (h a f) -> p h a f", h=4, a=2)
                    tv = [t_[:, j, :].rearrange("p (h a f) -> p h a f", h=4, a=2) for j in range(4)]
                    k.op("dve", [q_, c_], [t_], lambda e: e.tensor_tensor(tv[0], x1, C, ALU.mult))
                    k.op("pool", [q_, c_], [t_], lambda e: e.tensor_tensor(tv[1], x2, Sn, ALU.mult))
                    k.op("dve", [q_, c_], [t_], lambda e: e.tensor_tensor(tv[2], x1, Sn, ALU.mult))
                    k.op("pool", [q_, c_], [t_], lambda e: e.tensor_tensor(tv[3], x2, C, ALU.mult))
                    k.op("dve", [t_], [o_], lambda e: e.tensor_tensor(ov[:, :, :, 0, :], tv[0], tv[1], ALU.subtract))
                    k.op("pool", [t_], [o_], lambda e: e.tensor_tensor(ov[:, :, :, 1, :], tv[2], tv[3], ALU.add))
                src = o_
            else:
                src = q_
            k.dma("pool", S["qbr"][rows, :], src[:, 0, :], [src], [S["qbr"]])
            k.dma("pool", S["kbr"][rows, :], src[:, 1, :], [src], [S["kbr"]])
            l_ = lr[u]
            k.dma("sp", l_[:], S["lrT"][:, :, i * 128:(i + 1) * 128].rearrange("d r t -> r d t"), [S["lrT"]], [l_])
            for d in range(2):
                ps = psz[n % 2]; e_ = ee[n % 2]; s_ = spo[n % 2]; n += 1
                k.op("pe", [l_, gw], [ps], lambda e: e.matmul(ps[:], l_[:, d, :], gw[:, d, :], start=True, stop=False))
                k.op("pe", [ones, gb], [ps], lambda e: e.matmul(ps[:], ones[0:1, :], gb[0:1, d, :], start=False, stop=True))
                k.op("act", [ps], [e_], lambda e: e.activation(e_[:], ps[:], AF.Exp, scale=-1.0))
                k.op("act", [e_], [s_], lambda e: e.activation(s_[:], e_[:], AF.Ln, bias=1.0))
                k.dma("pool", S["sp"][d, rows, :], s_[:], [s_], [S["sp"]])


def phase_gla(k, S, glac, idf):
    with k.phase():
        cst = k.sb("gl_c", [128, 6, 128], F32)
        k.dma("sp", cst[:], glac[:].rearrange("d m s t -> s (d m) t"), [glac], [cst])
        S32 = [[k.sb("gl_S%d%d" % (h, d), [128, 256], F32) for d in range(2)] for h in range(4)]
        Sbf = [[k.sb("gl_Sb%d%d" % (h, d), [128, 256], BF16) for d in range(2)] for h in range(4)]
        for h in range(4):
            for d in range(2):
                k.op("dve", [], [S32[h][d]], lambda e: e.memset(S32[h][d][:], 0.0))
                k.op("dve", [], [Sbf[h][d]], lambda e: e.memset(Sbf[h][d][:], 0.0))
        NB = 2
        inp = [[{"sp": k.sb("gl_sp%d%d" % (b, d), [128, 512], F32),
                 "q": k.sb("gl_q%d%d" % (b, d), [128, 512], F32),
                 "k": k.sb("gl_k%d%d" % (b, d), [128, 512], F32),
                 "v": k.sb("gl_v%d%d" % (b, d), [128, 1024], BF16)} for d in range(2)] for b in range(NB)]
        tmps = []
        for sl in range(2):
            tmps.append({
                "E1": k.sb("gl_E1%d" % sl, [128, 128], F32), "E2": k.sb("gl_E2%d" % sl, [128, 128], F32),
                "E3": k.sb("gl_E3%d" % sl, [128, 128], F32),
                "qd": k.sb("gl_qd%d" % sl, [128, 128], BF16), "kd": k.sb("gl_kd%d" % sl, [128, 128], BF16),
                "ktA": k.sb("gl_ktA%d" % sl, [128, 128], BF16), "ktB": k.sb("gl_ktB%d" % sl, [128, 128], BF16),
                "qdA": k.sb("gl_qdA%d" % sl, [128, 128], BF16), "qdB": k.sb("gl_qdB%d" % sl, [128, 128], BF16), "am": k.sb("gl_am%d" % sl, [128, 128], BF16),
                "o": k.sb("gl_o%d" % sl, [128, 256], F32), "Bsb": k.sb("gl_Bsb%d" % sl, [128, 256], F32),
                "pA": k.ps("gl_pA%d" % sl, [128, 512], F32), "pB": k.ps("gl_pB%d" % sl, [128, 512], F32),
                "pO": k.ps("gl_pO%d" % sl, [128, 512], F32)})
        for T in tmps:
            for nm in ("ktA", "ktB", "qdA", "qdB"):
                k.op("dve", [], [T[nm]], lambda e: e.memset(T[nm][:], 0.0))
        order = [[32, 33] + list(range(32)), [33, 32] + list(range(31, -1, -1))]
        for s in range(NTILE):
            b = s % NB
            for d in range(2):
                ti = order[d][s]
                rows = slice(ti * 128, (ti + 1) * 128)
                I = inp[b][d]
                k.dma("sp", I["sp"][:], S["sp"][d, rows, :], [S["sp"]], [I["sp"]])
                k.dma("sp", I["q"][:], S["qbr"][rows, :], [S["qbr"]], [I["q"]])
                k.dma("sp", I["k"][:], S["kbr"][rows, :], [S["kbr"]], [I["k"]])
                k.dma("sp", I["v"][:], S["vb"][rows, :], [S["vb"]], [I["v"]])
            streams = [(h, d) for h in range(4) for d in range(2)]
            for pi in range(0, 8, 2):
                pair = streams[pi:pi + 2]
                ctxs = []
                for sl, (h, d) in enumerate(pair):
                    T = tmps[sl]
                    I = inp[b][d]
                    hs = slice(h * 128, (h + 1) * 128)
                    TRI, SUF, MSK = cst[:, d * 3 + 0, :], cst[:, d * 3 + 1, :], cst[:, d * 3 + 2, :]
                    ctxs.append((h, d, T, I, hs, TRI, SUF, MSK))
                for (h, d, T, I, hs, TRI, SUF, MSK) in ctxs:
                    pA = T["pA"]
                    k.op("pe", [I["sp"], cst], [pA], lambda e: e.matmul(pA[:, 0:128], I["sp"][:, hs], TRI, start=True, stop=True))
                    k.op("pe", [I["sp"], cst], [pA], lambda e: e.matmul(pA[:, 128:256], SUF, I["sp"][:, hs], start=True, stop=True))
                    k.op("pe", [I["q"], idf], [pA], lambda e: e.transpose(pA[:, 256:384], I["q"][:, hs], idf[:]))
                    k.op("pe", [I["k"], idf], [pA], lambda e: e.transpose(pA[:, 384:512], I["k"][:, hs], idf[:]))
                for (h, d, T, I, hs, TRI, SUF, MSK) in ctxs:
                    pA = T["pA"]
                    EXPF = AF.Exp
                    Bsb = T["Bsb"]
                    k.op("dve", [pA], [Bsb], lambda e: e.tensor_copy(Bsb[:], pA[:, 0:256]))
                    k.op("act", [Bsb], [T["E1"]], lambda e: e.activation(T["E1"][:], Bsb[:, 0:128], EXPF))
                    k.op("act", [Bsb], [T["E2"]], lambda e: e.activation(T["E2"][:], Bsb[:, 0:128], EXPF, scale=-1.0))
                    k.op("act", [Bsb], [T["E3"]], lambda e: e.activation(T["E3"][:], Bsb[:, 128:256], EXPF))
                    k.op("dve", [pA, T["E1"]], [T["qd"]], lambda e: e.tensor_tensor(T["qd"][:], pA[:, 256:384], T["E1"][:], ALU.mult))
                    k.op("dve", [pA, T["E2"]], [T["kd"]], lambda e: e.tensor_tensor(T["kd"][:], pA[:, 384:512], T["E2"][:], ALU.mult))
                    ca = slice(0, 64) if d == 0 else slice(64, 128)
                    cb_ = slice(64, 128) if d == 0 else slice(0, 64)
                    k.op("pool", [], [T["ktA"]], lambda e: e.memset(T["ktA"][cb_, :], 0.0))
                    k.op("pool", [], [T["ktB"]], lambda e: e.memset(T["ktB"][ca, :], 0.0))
                    k.op("pool", [I["k"], T["E3"]], [T["ktA"]], lambda e: e.tensor_tensor(T["ktA"][ca, :], I["k"][ca, hs], T["E3"][ca, :], ALU.mult))
                    k.op("pool", [I["k"], T["E3"]], [T["ktB"]], lambda e: e.tensor_tensor(T["ktB"][cb_, :], I["k"][cb_, hs], T["E3"][cb_, :], ALU.mult))
                    k.op("pool", [], [T["qdA"]], lambda e: e.memset(T["qdA"][:, cb_], 0.0))
                    k.op("pool", [], [T["qdB"]], lambda e: e.memset(T["qdB"][:, ca], 0.0))
                    k.op("pool", [T["qd"]], [T["qdA"]], lambda e: e.tensor_copy(T["qdA"][:, ca], T["qd"][:, ca]))
                    k.op("pool", [T["qd"]], [T["qdB"]], lambda e: e.tensor_copy(T["qdB"][:, cb_], T["qd"][:, cb_]))
                for (h, d, T, I, hs, TRI, SUF, MSK) in ctxs:
                    pB = T["pB"]
                    k.op("pe", [T["kd"], T["qd"]], [pB], lambda e: e.matmul(pB[:, 0:128], T["kd"][:], T["qd"][:], start=True, stop=True))
                    k.op("dve", [pB, cst], [T["am"]], lambda e: e.tensor_tensor(T["am"][:], pB[:, 0:128], MSK, ALU.mult))
                for (h, d, T, I, hs, TRI, SUF, MSK) in ctxs:
                    pB, pO = T["pB"], T["pO"]
                    vh = I["v"][:, h * 256:(h + 1) * 256]
                    S3, Sb = S32[h][d], Sbf[h][d]
                    ca = slice(0, 64) if d == 0 else slice(64, 128)
                    cb_ = slice(64, 128) if d == 0 else slice(0, 64)
                    la = 63 if d == 0 else 64
                    lb = 127 if d == 0 else 0
                    k.op("pe", [T["am"], I["v"]], [pO], lambda e: e.matmul(pO[:, 0:256], T["am"][:], vh, start=True, stop=False))
                    k.op("pe", [T["qdA"], Sb], [pO], lambda e: e.matmul(pO[:, 0:256], T["qdA"][:], Sb[:], start=False, stop=False))
                    k.op("pe", [T["ktA"], I["v"]], [pB], lambda e: e.matmul(pB[:, 128:384], T["ktA"][:], vh, start=True, stop=True))
                    k.op("dve", [S3, T["E1"], pB], [S3], lambda e: e.scalar_tensor_tensor(S3[:], S3[:], T["E1"][:, la:la + 1], pB[:, 128:384], ALU.mult, ALU.add))
                    k.op("act", [S3], [Sb], lambda e: e.activation(Sb[:], S3[:], AF.Copy))
                    k.op("pe", [T["qdB"], Sb], [pO], lambda e: e.matmul(pO[:, 0:256], T["qdB"][:], Sb[:], start=False, stop=True))
                    k.op("pe", [T["ktB"], I["v"]], [pB], lambda e: e.matmul(pB[:, 128:384], T["ktB"][:], vh, start=True, stop=True))
                    k.op("dve", [S3, T["E1"], pB], [S3], lambda e: e.scalar_tensor_tensor(S3[:], S3[:], T["E1"][:, lb:lb + 1], pB[:, 128:384], ALU.mult, ALU.add))
                    k.op("act", [S3], [Sb], lambda e: e.activation(Sb[:], S3[:], AF.Copy))
                    k.op("act", [pO], [T["o"]], lambda e: e.activation(T["o"][:], pO[:, 0:256], AF.Copy))
                    ti = order[d][s]
                    k.dma("pool", S["ob"][d, ti * 128:(ti + 1) * 128, h * 256:(h + 1) * 256], T["o"][:], [T["o"]], [S["ob"]])


def phase_gatenorm(k, S, norm_g, idb):
    with k.phase():
        ng = k.sb("gn_ng", [128, 4, 256], F32)
        for h in range(4):
            k.dma("sp", ng[:, h, :], norm_g[:].partition_broadcast(128), [norm_g], [ng])
        bT = k.sb("gn_bT", [128, 8, NT], BF16)
        of = [k.sb("gn_of%d" % i, [128, 1024], F32) for i in range(2)]
        ob = [k.sb("gn_ob%d" % i, [128, 1024], F32) for i in range(2)]
        rr = [k.sb("gn_r%d" % i, [128, 1024], F32) for i in range(2)]
        sq = k.sb("gn_sq", [128, 256], F32)
        stt = [k.sb("gn_st%d" % i, [128, 8], F32) for i in range(2)]
        bl = [k.sb("gn_bl%d" % i, [128, 1024], BF16) for i in range(2)]
        pt = [k.ps("gn_pt%d" % i, [128, 1024], BF16) for i in range(2)]
        for i in range(NTILE):
            u = i % 2
            rows = slice(i * 128, (i + 1) * 128)
            a_, b_, r_, s_, l_, p_ = of[u], ob[u], rr[u], stt[u], bl[u], pt[u]
            k.dma("sp", a_[:], S["ob"][0, rows, :], [S["ob"]], [a_])
            k.dma("sp", b_[:], S["ob"][1, rows, :], [S["ob"]], [b_])
            k.dma("sp", r_[:], S["rb"][rows, :], [S["rb"]], [r_])
            k.op("dve", [a_, b_], [a_], lambda e: e.tensor_tensor(a_[:], a_[:], b_[:], ALU.add))
            for h in range(4):
                k.op("act", [a_], [sq, s_], lambda e: e.activation(sq[:], a_[:, h * 256:(h + 1) * 256], AF.Square, accum_out=s_[:, h:h + 1]))
            k.op("dve", [s_], [s_], lambda e: e.tensor_scalar(s_[:, 4:8], s_[:, 0:4], 1.0 / 256, 1e-6, ALU.mult, ALU.add))
            k.op("act", [s_], [s_], lambda e: e.activation(s_[:, 4:8], s_[:, 4:8], AF.Sqrt))
            k.op("dve", [s_], [s_], lambda e: e.reciprocal(s_[:, 4:8], s_[:, 4:8]))
            for h in range(4):
                k.op("dve", [a_, s_], [a_], lambda e: e.tensor_scalar_mul(a_[:, h * 256:(h + 1) * 256], a_[:, h * 256:(h + 1) * 256], s_[:, 4 + h:5 + h]))
            k.op("pool", [a_, ng], [a_], lambda e: e.tensor_tensor(a_[:], a_[:], ng[:].rearrange("p h c -> p (h c)"), ALU.mult))
            k.op("act", [r_], [r_], lambda e: e.activation(r_[:], r_[:], AF.Silu))
            k.op("dve", [a_, r_], [l_], lambda e: e.tensor_tensor(l_[:], a_[:], r_[:], ALU.mult))
            for j in range(8):
                k.op("pe", [l_, idb], [p_], lambda e: e.transpose(p_[:, j * 128:(j + 1) * 128], l_[:, j * 128:(j + 1) * 128], idb[:]))
            k.op("act", [p_], [bT], lambda e: e.activation(bT[:, :, i * 128:(i + 1) * 128], p_[:].rearrange("p (j t) -> p j t", j=8), AF.Copy))
        for j in range(8):
            k.dma("pool", S["mixT"][8 + j], bT[:, j, :], [bT], [S["mixT"]])


def resid_ln(k, srcs, x_, mv2, g_, b_, t1, stats, aggr):
    src_aps, src_res = srcs
    for n in range(4):
        k.op("dve", src_res + [mv2], [t1], lambda e: e.tensor_tensor(t1[:, n * 512:(n + 1) * 512], src_aps[n], mv2[:, n * 512:(n + 1) * 512], ALU.mult))
    k.op("dve", [x_, t1], [t1], lambda e: e.scalar_tensor_tensor(t1[:], x_[:], ALPHA, t1[:], ALU.mult, ALU.add))
    sq = stats[:].rearrange("p a b -> p (a b)")
    k.op("dve", [t1], [aggr], lambda e: e.reduce_sum(aggr[:, 0:1], t1[:], AX.X))
    k.op("dve", [aggr], [aggr], lambda e: e.tensor_scalar_mul(aggr[:, 0:1], aggr[:, 0:1], 1.0 / D))
    k.op("dve", [t1, aggr], [t1], lambda e: e.tensor_scalar(t1[:], t1[:], aggr[:, 0:1], None, ALU.subtract))
    k.op("act", [t1], [x_, aggr], lambda e: e.activation(x_[:], t1[:], AF.Square, accum_out=aggr[:, 1:2]))
    k.op("dve", [aggr], [aggr], lambda e: e.tensor_scalar(aggr[:, 2:3], aggr[:, 1:2], 1.0 / D, LN_EPS, ALU.mult, ALU.add))
    k.op("dve", [aggr], [aggr], lambda e: e.memset(aggr[:, 0:1], 0.0))
    k.op("act", [aggr], [aggr], lambda e: e.activation(aggr[:, 2:3], aggr[:, 2:3], AF.Sqrt))
    k.op("dve", [aggr], [aggr], lambda e: e.reciprocal(aggr[:, 2:3], aggr[:, 2:3]))
    k.op("dve", [t1, aggr], [t1], lambda e: e.tensor_scalar(t1[:], t1[:], aggr[:, 0:1], aggr[:, 2:3], ALU.subtract, ALU.mult))
    k.op("pool", [t1, g_], [t1], lambda e: e.tensor_tensor(t1[:], t1[:], g_[:], ALU.mult))
    k.op("pool", [t1, b_], [t1], lambda e: e.tensor_tensor(t1[:], t1[:], b_[:], ALU.add))


def routing(k, lg, rbb, comb_out, W):
    BIG = 1.0e4
    aff = W[:, 0:16]; sel = W[:, 16:32]; pr = W[:, 32:56]; sc = W[:, 56:60]; gm = W[:, 60:61]
    gs = W[:, 61:65]; pen = W[:, 65:69]; selm = W[:, 72:88]; m1 = W[:, 88:89]; oh = W[:, 96:112]
    sel2 = W[:, 112:128]; oh2 = W[:, 128:144]; ws = W[:, 144:145]
    Wr = W.res
    def dv(fn):
        k.op("dve", [Wr], [Wr], fn)
    lgs = W[:, 145:161] if False else W[:, 144:160]
    k.op("dve", [lg[1]], [Wr], lambda e: e.tensor_copy(sel, lg[0]))
    k.op("act", [Wr], [Wr], lambda e: e.activation(aff, sel, AF.Sigmoid))
    k.op("dve", [Wr, rbb], [Wr], lambda e: e.tensor_tensor(sel, aff, rbb[:], ALU.add))
    s3 = sel.rearrange("p (g e) -> p g e", g=4)
    pairs = [(0, 1), (0, 2), (0, 3), (1, 2), (1, 3), (2, 3)]
    p3 = pr.rearrange("p (n g) -> p n g", n=6)
    for n, (a, b) in enumerate(pairs):
        dv(lambda e: e.tensor_tensor(p3[:, n, :], s3[:, :, a], s3[:, :, b], ALU.add))
    dv(lambda e: e.tensor_tensor(sc, p3[:, 0, :], p3[:, 1, :], ALU.max))
    for n in range(2, 6):
        dv(lambda e: e.tensor_tensor(sc, sc, p3[:, n, :], ALU.max))
    dv(lambda e: e.reduce_max(gm, sc, AX.X))
    dv(lambda e: e.tensor_scalar(gs, sc, gm, None, ALU.is_ge))
    dv(lambda e: e.tensor_scalar(pen, gs, -1.0, BIG, ALU.add, ALU.mult))
    sm3 = selm.rearrange("p (g e) -> p g e", g=4)
    for g in range(4):
        dv(lambda e: e.tensor_scalar(sm3[:, g, :], s3[:, g, :], pen[:, g:g + 1], None, ALU.add))
    dv(lambda e: e.reduce_max(m1, selm, AX.X))
    dv(lambda e: e.tensor_scalar(oh, selm, m1, None, ALU.is_ge))
    dv(lambda e: e.scalar_tensor_tensor(sel2, oh, -BIG, selm, ALU.mult, ALU.add))
    dv(lambda e: e.reduce_max(m1, sel2, AX.X))
    dv(lambda e: e.tensor_scalar(oh2, sel2, m1, None, ALU.is_ge))
    dv(lambda e: e.tensor_tensor(oh, oh, oh2, ALU.add))
    dv(lambda e: e.tensor_tensor(oh, oh, aff, ALU.mult))
    dv(lambda e: e.reduce_sum(ws, oh, AX.X))
    dv(lambda e: e.reciprocal(ws, ws))
    k.op("dve", [Wr], [comb_out[1]], lambda e: e.tensor_scalar_mul(comb_out[0], oh, ws))


def post_mixer(k, layer, KD, mix_src, w_out_ap, xcur, modrow, ln_g, ln_b, router_w, router_b, S, idf, ntiles, xg=None, xout=None):
    with k.phase():
        wo = k.sb("pm_wo", [128, KD, 2048], BF16)
        stg = [k.sb("pm_stg%d" % i, [128, KD, 128], F32) for i in range(2)]
        for cb in range(16):
            st = stg[cb % 2]
            k.dma("sp", st[:], w_out_ap[:, cb * 128:(cb + 1) * 128].rearrange("(j p) c -> p j c", p=128), [], [st])
            k.op("act", [st], [wo], lambda e: e.activation(wo[:, :, cb * 128:(cb + 1) * 128], st[:], AF.Copy))
        m2 = k.sb("pm_m2", [128, D], F32); m3 = k.sb("pm_m3", [128, D], F32); m4 = k.sb("pm_m4", [128, D], F32)
        g_ = k.sb("pm_g", [128, D], F32); b_ = k.sb("pm_b", [128, D], F32)
        rw = k.sb("pm_rw", [128, 16, 16], F32)
        rbb = k.sb("pm_rbb", [128, 16], F32)
        load_bc(k, "sp", g_, ln_g[layer:layer + 1, :], [ln_g])
        load_bc(k, "sp", b_, ln_b[layer:layer + 1, :], [ln_b])
        k.dma("sp", rw[:], router_w[:].rearrange("(j p) e -> p j e", p=128), [router_w], [rw])
        load_bc(k, "sp", rbb, router_b[:], [router_b])
        xt = [k.sb("pm_x%d" % i, [128, D], F32) for i in range(3)]
        t1s = [k.sb("pm_t%d" % i, [128, D], F32) for i in range(2)]
        mt = [k.sb("pm_mt%d" % i, [128, KD, 128], BF16) for i in range(2)]
        hTb = [k.sb("pm_hTb%d" % i, [128, 16, 128], BF16) for i in range(2)]
        hTf = k.sb("pm_hTf", [128, 16, 128], F32)
        stats = k.sb("pm_stats", [128, 4, 6], F32)
        aggr = k.sb("pm_aggr", [128, 4], F32)
        W = k.sb("pm_W", [128, 160], F32)
        lgs = [k.sb("pm_lg%d" % i, [128, 16], F32) for i in range(2)]
        cb_ = [k.sb("pm_comb%d" % i, [128, 16], F32) for i in range(2)]
        pso = [k.ps("pm_pso%d" % i, [128, 512], F32) for i in range(4)]
        pst = [k.ps("pm_pst%d" % i, [128, 512], F32) for i in range(4)]

        def s0(i):
            if i == 0 or i == 32:
                r = 0 if i == 0 else 1
                load_bc(k, "sp", m2, modrow[layer, r:r + 1, 2 * D:3 * D], [modrow])
            x_, t1, m_ = xt[i % 3], t1s[i % 2], mt[i % 2]
            rows = slice(i * 128, (i + 1) * 128)
            if xg is None:
                k.dma("sp", x_[:], xcur[rows, :], [xcur], [x_])
            else:
                k.idma_gather(x_[:], xcur[:], xg[:, i:i + 1], [xcur, xg], [x_])
            k.dma("sp", m_[:], mix_src[:, :, rows].rearrange("j p t -> p j t"), [mix_src], [m_])
            for n in range(4):
                for j in range(KD):
                    k.op("pe", [m_, wo], [pso[n]], lambda e: e.matmul(pso[n][:], m_[:, j, :], wo[:, j, n * 512:(n + 1) * 512], start=(j == 0), stop=(j == KD - 1)))
            for n in range(4):
                k.op("dve", [pso[n], m2], [t1], lambda e: e.tensor_tensor(t1[:, n * 512:(n + 1) * 512], pso[n][:], m2[:, n * 512:(n + 1) * 512], ALU.mult))

        def s1(i):
            if i == 0 or i == 32:
                r = 0 if i == 0 else 1
                load_bc(k, "sp", m3, modrow[layer, r:r + 1, 3 * D:4 * D], [modrow])
                load_bc(k, "sp", m4, modrow[layer, r:r + 1, 4 * D:5 * D], [modrow])
            x_, t1 = xt[i % 3], t1s[i % 2]
            rows = slice(i * 128, (i + 1) * 128)
            k.op("dve", [x_, t1], [t1], lambda e: e.scalar_tensor_tensor(t1[:], x_[:], ALPHA, t1[:], ALU.mult, ALU.add))
            k.op("dve", [t1], [aggr], lambda e: e.reduce_sum(aggr[:, 0:1], t1[:], AX.X))
            k.op("dve", [aggr], [aggr], lambda e: e.tensor_scalar_mul(aggr[:, 0:1], aggr[:, 0:1], 1.0 / D))
            k.op("dve", [t1, aggr], [t1], lambda e: e.tensor_scalar(t1[:], t1[:], aggr[:, 0:1], None, ALU.subtract))
            k.op("act", [t1], [x_, aggr], lambda e: e.activation(x_[:], t1[:], AF.Square, accum_out=aggr[:, 1:2]))
            k.op("dve", [aggr], [aggr], lambda e: e.tensor_scalar(aggr[:, 2:3], aggr[:, 1:2], 1.0 / D, LN_EPS, ALU.mult, ALU.add))
            k.op("act", [aggr], [aggr], lambda e: e.activation(aggr[:, 2:3], aggr[:, 2:3], AF.Sqrt))
            k.op("dve", [aggr], [aggr], lambda e: e.reciprocal(aggr[:, 2:3], aggr[:, 2:3]))
            k.op("dve", [t1, aggr], [t1], lambda e: e.tensor_scalar_mul(t1[:], t1[:], aggr[:, 2:3]))
            k.op("pool", [t1, g_], [t1], lambda e: e.tensor_tensor(t1[:], t1[:], g_[:], ALU.mult))
            k.op("pool", [t1, b_], [t1], lambda e: e.tensor_tensor(t1[:], t1[:], b_[:], ALU.add))
            xo = xcur if xout is None else xout
            k.dma("pool", xo[rows, :], t1[:], [t1], [xo])
            k.op("dve", [t1, m4], [x_], lambda e: e.tensor_tensor(x_[:], t1[:], m4[:], ALU.mult))
            k.op("pool", [x_, m3], [x_], lambda e: e.tensor_tensor(x_[:], x_[:], m3[:], ALU.add))

        def s2(i):
            x_, hb, lg = xt[i % 3], hTb[i % 2], lgs[i % 2]
            for j in range(16):
                k.op("pe", [x_, idf], [pst[j // 4]], lambda e: e.transpose(pst[j // 4][:, (j % 4) * 128:(j % 4 + 1) * 128], x_[:, j * 128:(j + 1) * 128], idf[:]))
            for n in range(4):
                k.op("dve", [pst[n]], [hTf], lambda e: e.tensor_copy(hTf[:, n * 4:(n + 1) * 4, :], pst[n][:].rearrange("p (a t) -> p a t", a=4)))
                k.op("act", [hTf], [hb], lambda e: e.activation(hb[:, n * 4:(n + 1) * 4, :], hTf[:, n * 4:(n + 1) * 4, :], AF.Copy))
            k.dma("pool", S["hT_moe"][i], hb[:], [hb], [S["hT_moe"]])
            for j in range(16):
                k.op("pe", [hTf, rw], [pst[0]], lambda e: e.matmul(pst[0][:, 0:16], hTf[:, j, :], rw[:, j, :], start=(j == 0), stop=(j == 15)))
            k.op("dve", [pst[0]], [lg], lambda e: e.tensor_copy(lg[:], pst[0][:, 0:16]))

        def s3(i):
            lg, c_ = lgs[i % 2], cb_[i % 2]
            rows = slice(i * 128, (i + 1) * 128)
            routing(k, (lg[:], lg.res), rbb, (c_[:], c_.res), W)
            k.dma("pool", S["comb"][rows, :], c_[:], [c_], [S["comb"]])

        for step in range(ntiles + 3):
            if step < ntiles:
                s0(step)
            if 0 <= step - 1 < ntiles:
                s1(step - 1)
            if 0 <= step - 2 < ntiles:
                s2(step - 2)
            if 0 <= step - 3 < ntiles:
                s3(step - 3)


def phase_moe(k, layer, blocks, w_gate, w_up, w_down, xcur, modrow, ln_g, ln_b, S, out_dst, n_exp=16, gidx=None):
    with k.phase():
        TBmax = max(blocks) * 128
        hTb = k.sb("mo_hT", [128, 16, TBmax], BF16)
        yacc = k.sb("mo_y", [128, max(blocks), D], F32)
        hid = k.sb("mo_hid", [128, 8, TBmax], BF16)
        comb = k.sb("mo_comb", [128, max(blocks), 16], F32)
        wd = k.sb("mo_wd", [128, 8, 1024], BF16)
        wdf = [TT(wd.t, Res("wd%d" % i)) for i in range(8)]
        wg = [k.sb("mo_wg%d" % i, [128, 16, 128], BF16) for i in range(2)]
        wu = [k.sb("mo_wu%d" % i, [128, 16, 128], BF16) for i in range(2)]
        NSTG = 4
        stg = [k.sb("mo_stg%d" % i, [128, D], F32) for i in range(NSTG)]
        sg = [k.sb("mo_sg%d" % i, [128, 512], F32) for i in range(2)]
        xe = k.sb("mo_xe", [128, D], F32)
        gstg = [TT(stg[i][:].bitcast(BF16)[:, 0:D], stg[i].res) for i in range(2)] if gidx is not None else None
        stats = k.sb("mo_stats", [128, 4, 6], F32)
        aggr = k.sb("mo_aggr", [128, 4], F32)
        psg = [k.ps("mo_psg%d" % i, [128, 512], F32) for i in range(2)]
        psu = [k.ps("mo_psu%d" % i, [128, 512], F32) for i in range(2)]
        psy = [k.ps("mo_psy%d" % i, [128, 512], F32) for i in range(4)]
        sc = 0
        t0 = 0
        cnt = 0
        for nb in blocks:
            TB = nb * 128
            if gidx is None:
                for t in range(nb):
                    k.dma("sp", hTb[:, :, t * 128:(t + 1) * 128], S["hT_moe"][t0 + t], [S["hT_moe"]], [hTb])
                k.dma("sp", comb[:, 0:nb, :], S["comb"][t0 * 128:(t0 + nb) * 128, :].rearrange("(t p) e -> p t e", p=128), [S["comb"]], [comb])
            else:
                hview = S["hT_moe"][:].rearrange("n p j t -> (n p) (j t)")
                for t in range(nb):
                    gb_ = gstg[t % 2]
                    k.idma_gather(gb_[:], hview, gidx[:, t0 + t:t0 + t + 1], [S["hT_moe"], gidx], [gb_])
                    k.op("act", [gb_], [hTb], lambda e: e.activation(hTb[:, :, t * 128:(t + 1) * 128], gb_[:].rearrange("p (j c) -> p j c", j=16), AF.Copy))
                    k.idma_gather(comb[:, t, :], S["comb"][:], gidx[:, t0 + t:t0 + t + 1], [S["comb"], gidx], [comb])
            k.op("pool", [], [yacc], lambda e: e.memset(yacc[:], 0.0))
            subs = [(s0, min(512, TB - s0)) for s0 in range(0, TB, 512)]
            for ex in range(n_exp):
                for ft in range(8):
                    g_, u_ = wg[ft % 2], wu[ft % 2]
                    for (wsrc, wdst) in ((w_gate, g_), (w_up, u_)):
                        st = stg[sc % NSTG]; sc += 1
                        k.dma("sp", st[:].rearrange("p (j c) -> p j c", j=16), wsrc[layer, ex, :, ft * 128:(ft + 1) * 128].rearrange("(j p) c -> p j c", p=128), [], [st])
                        k.op("act", [st], [wdst], lambda e: e.activation(wdst[:].rearrange("p j c -> p (j c)"), st[:], AF.Copy))
                    for (s0, sn) in subs:
                        pg, pu, s_ = psg[cnt % 2], psu[cnt % 2], sg[cnt % 2]
                        cnt += 1
                        for j in range(16):
                            k.op("pe", [g_, hTb], [pg], lambda e: e.matmul(pg[:, 0:sn], g_[:, j, :], hTb[:, j, s0:s0 + sn], start=(j == 0), stop=(j == 15)))
                        for j in range(16):
                            k.op("pe", [u_, hTb], [pu], lambda e: e.matmul(pu[:, 0:sn], u_[:, j, :], hTb[:, j, s0:s0 + sn], start=(j == 0), stop=(j == 15)))
                        k.op("act", [pg], [s_], lambda e: e.activation(s_[:, 0:sn], pg[:, 0:sn], AF.Silu))
                        k.op("dve", [s_, pu], [hid], lambda e: e.tensor_tensor(hid[:, ft, s0:s0 + sn], s_[:, 0:sn], pu[:, 0:sn], ALU.mult))
                for hf in range(2):
                    for fc in range(8):
                        st = stg[sc % NSTG]; sc += 1
                        k.dma("sp", st[:, 0:1024], w_down[layer, ex, fc * 128:(fc + 1) * 128, hf * 1024:(hf + 1) * 1024], [], [st])
                        k.op("act", [st], [wdf[fc]], lambda e: e.activation(wd[:, fc, :], st[:, 0:1024], AF.Copy))
                    for t in range(nb):
                        for n2 in range(2):
                            n = hf * 2 + n2
                            py = psy[(t * 4 + n) % 4]
                            for fc in range(8):
                                k.op("pe", [hid, wdf[fc]], [py], lambda e: e.matmul(py[:], hid[:, fc, t * 128:(t + 1) * 128], wd[:, fc, n2 * 512:(n2 + 1) * 512], start=(fc == 0), stop=(fc == 7)))
                            k.op("dve", [py, comb, yacc], [yacc], lambda e: e.scalar_tensor_tensor(
                                yacc[:, t, n * 512:(n + 1) * 512], py[:], comb[:, t, ex:ex + 1], yacc[:, t, n * 512:(n + 1) * 512], ALU.mult, ALU.add))
            m5, g_, b_ = stg[0], stg[1], stg[2]
            xt = xe[:]
            load_bc(k, "sp", g_, ln_g[layer:layer + 1, :], [ln_g])
            load_bc(k, "sp", b_, ln_b[layer:layer + 1, :], [ln_b])
            for t in range(nb):
                ti = t0 + t
                if t == 0 or ti == 32:
                    r = 0 if ti < 32 else 1
                    load_bc(k, "sp", m5, modrow[layer, r:r + 1, 5 * D:6 * D], [modrow])
                rows = slice(ti * 128, (ti + 1) * 128)
                if gidx is None:
                    k.dma("sp", xt, xcur[rows, :], [xcur], [xe])
                else:
                    k.idma_gather(xt, xcur[:], gidx[:, ti:ti + 1], [xcur, gidx], [xe])
                resid_ln_ap(k, yacc, t, xe, xt, m5, g_, b_, stats, aggr)
                k.dma("pool", out_dst[rows, :], yacc[:, t, :], [yacc], [out_dst], is_output=True)
            t0 += nb


def resid_ln_ap(k, yacc, t, xres, xt, mv2, g_, b_, stats, aggr):
    y = yacc[:, t, :]
    k.op("dve", [yacc, mv2], [yacc], lambda e: e.tensor_tensor(y, y, mv2[:], ALU.mult))
    k.op("dve", [xres, yacc], [yacc], lambda e: e.scalar_tensor_tensor(y, xt, ALPHA, y, ALU.mult, ALU.add))
    k.op("dve", [yacc], [aggr], lambda e: e.reduce_sum(aggr[:, 0:1], y, AX.X))
    k.op("dve", [aggr], [aggr], lambda e: e.tensor_scalar_mul(aggr[:, 0:1], aggr[:, 0:1], 1.0 / D))
    k.op("dve", [yacc, aggr], [yacc], lambda e: e.tensor_scalar(y, y, aggr[:, 0:1], None, ALU.subtract))
    k.op("act", [yacc], [xres, aggr], lambda e: e.activation(xt, y, AF.Square, accum_out=aggr[:, 1:2]))
    k.op("dve", [aggr], [aggr], lambda e: e.tensor_scalar(aggr[:, 2:3], aggr[:, 1:2], 1.0 / D, LN_EPS, ALU.mult, ALU.add))
    k.op("dve", [aggr], [aggr], lambda e: e.memset(aggr[:, 0:1], 0.0))
    k.op("act", [aggr], [aggr], lambda e: e.activation(aggr[:, 2:3], aggr[:, 2:3], AF.Sqrt))
    k.op("dve", [aggr], [aggr], lambda e: e.reciprocal(aggr[:, 2:3], aggr[:, 2:3]))
    k.op("dve", [yacc, aggr], [yacc], lambda e: e.tensor_scalar(y, y, aggr[:, 0:1], aggr[:, 2:3], ALU.subtract, ALU.mult))
    k.op("pool", [yacc, g_], [yacc], lambda e: e.tensor_tensor(y, y, g_[:], ALU.mult))
    k.op("pool", [yacc, b_], [yacc], lambda e: e.tensor_tensor(y, y, b_[:], ALU.add))


TWO_PI = 6.283185307179586
SIN_SC = 6.28318


def s5_host_params(lam_re, lam_im, log_dt, b_re, b_im, c_re, c_im):
    f = np.float32
    def padl(a):
        o = a.reshape(2, 16, 4, 64).transpose(2, 0, 1, 3)
        o = np.broadcast_to(o[:, None], (4, 32, 2, 16, 64)).reshape(128, 2, 16, 64)
        return np.ascontiguousarray(o).astype(f)
    lamre_bc = padl(lam_re)
    lamim_bc = padl(lam_im)
    dt_bc = padl(np.broadcast_to(log_dt[:, :, None], (2, 64, 64)))
    def padb(bb):
        o = np.zeros((4, 32, 2, 16, 64), f)
        t = bb.reshape(2, 16, 4, 64, 16).transpose(2, 4, 0, 1, 3)
        o[:, 0:16] = t
        return o.reshape(128, 2, 16, 64)
    bre_pad = padb(b_re)
    bim_pad = padb(b_im)
    def pl(a):
        o = a.transpose(2, 0, 1).reshape(64, 128)
        return np.ascontiguousarray(np.concatenate([o, o], 0)).astype(f)
    lamre_p = pl(lam_re)
    lamim_p = pl(lam_im)
    dt_p = pl(np.broadcast_to(log_dt[:, :, None], (2, 64, 64)))
    cr = c_re.transpose(3, 0, 1, 2).reshape(64, 128, 16)
    ci = c_im.transpose(3, 0, 1, 2).reshape(64, 128, 16)
    craw1 = np.ascontiguousarray(np.concatenate([cr, ci], 0)).astype(f)
    craw2 = np.ascontiguousarray(np.concatenate([ci, cr], 0)).astype(f)
    pa = np.ascontiguousarray(np.stack([lamre_bc, lamim_bc, dt_bc, bre_pad, bim_pad], 1)).reshape(128, 5, 2048)
    pb = np.ascontiguousarray(np.stack([lamre_p, lamim_p, dt_p], 1))
    return pa, pb, craw1, craw2


def sincos_small(k, eng, turns, sn, cs, tmpi, tmpf):
    k.op(eng, [turns], [tmpi], lambda e: e.tensor_copy(tmpi[:], turns[:]))
    k.op(eng, [tmpi], [tmpf], lambda e: e.tensor_copy(tmpf[:], tmpi[:]))
    k.op(eng, [turns, tmpf], [tmpf], lambda e: e.tensor_tensor(tmpf[:], turns[:], tmpf[:], ALU.subtract))
    k.op("act", [tmpf], [sn], lambda e: e.activation(sn[:], tmpf[:], AF.Sin, scale=SIN_SC))
    k.op(eng, [tmpf], [tmpf], lambda e: e.tensor_scalar_add(tmpf[:], tmpf[:], 0.25))
    k.op(eng, [tmpf], [turns], lambda e: e.tensor_single_scalar(turns[:], tmpf[:], 0.5, ALU.is_gt))
    k.op(eng, [tmpf, turns], [tmpf], lambda e: e.tensor_tensor(tmpf[:], tmpf[:], turns[:], ALU.subtract))
    k.op("act", [tmpf], [cs], lambda e: e.activation(cs[:], tmpf[:], AF.Sin, scale=SIN_SC))


def phase_l1_inproj(k, hT_all, od_w_in, S, idb, jb):
    with k.phase():
        wb = k.sb("l1_wb", [128, 16, 1024], BF16)
        with k.phase():
            stg = [k.sb("l1_stg%d" % i, [128, 16, 128], F32) for i in range(2)]
            for cb in range(8):
                st = stg[cb % 2]
                k.dma("sp", st[:], od_w_in[:, cb * 128:(cb + 1) * 128].rearrange("(j p) c -> p j c", p=128), [], [st])
                k.op("act", [st], [wb], lambda e: e.activation(wb[:, :, cb * 128:(cb + 1) * 128], st[:], AF.Copy))
        uf = [k.sb("l1_uf%d" % i, [128, 1024], F32) for i in range(2)]
        upad = [k.sb("l1_up%d" % i, [128, 64, 32], BF16) for i in range(2)]
        for u_ in upad:
            k.op("pool", [], [u_], lambda e: e.memset(u_[:], 0.0))
        stF = [k.sb("l1_sF%d" % i, [128, 16, 128], BF16) for i in range(1)] * 2
        stB = [k.sb("l1_sB%d" % i, [128, 16, 128], BF16) for i in range(1)] * 2
        psu = [k.ps("l1_psu%d" % i, [128, 512], F32) for i in range(2)]
        pst = [k.ps("l1_pst%d" % i, [128, 512], F32) for i in range(4)]
        pc = 0
        for i in range(NTILE):
            u = i % 2
            f_, p_, sF, sB = uf[u], upad[u], stF[u], stB[u]
            for n in range(2):
                for j in range(16):
                    k.op("pe", [hT_all, wb], [psu[n]], lambda e: e.matmul(psu[n][:], hT_all[:, j, i * 128:(i + 1) * 128], wb[:, j, n * 512:(n + 1) * 512], start=(j == 0), stop=(j == 15)))
                k.op("act", [psu[n]], [f_], lambda e: e.activation(f_[:, n * 512:(n + 1) * 512], psu[n][:], AF.Copy))
                k.op("dve", [f_], [p_], lambda e: e.tensor_copy(p_[:, n * 32:(n + 1) * 32, 0:16], f_[:, n * 512:(n + 1) * 512].rearrange("p (g c) -> p g c", c=16)))
            if i < 32:
                k.dma("pool", S["u_tm"][i * 128:(i + 1) * 128, :], f_[:], [f_], [S["u_tm"]])
            pv = p_[:].rearrange("p (ch q) c -> p ch (q c)", q=4)
            for (mat, dst) in ((idb, sF), (jb, sB)):
                for c4 in range(4):
                    ps = pst[pc % 4]; pc += 1
                    for cc in range(4):
                        ch = c4 * 4 + cc
                        k.op("pe", [p_, mat], [ps], lambda e: e.matmul(ps[:, cc * 128:(cc + 1) * 128], pv[:, ch, :], mat[:], start=True, stop=True))
                    eng = "act" if c4 % 2 == 0 else "dve"
                    if eng == "act":
                        k.op("act", [ps], [dst], lambda e: e.activation(dst[:, c4 * 4:(c4 + 1) * 4, :], ps[:].rearrange("p (a t) -> p a t", a=4), AF.Copy))
                    else:
                        k.op("dve", [ps], [dst], lambda e: e.tensor_copy(dst[:, c4 * 4:(c4 + 1) * 4, :], ps[:].rearrange("p (a t) -> p a t", a=4)))
            cf = (i - 32) * 128 if i >= 32 else 256 + i * 128
            cbk = NT - (i + 1) * 128
            k.dma("pool", S["uTf"][:, :, cf:cf + 128].rearrange("j p t -> p j t"), sF[:], [sF], [S["uTf"]])
            k.dma("pool", S["uTb"][:, :, cbk:cbk + 128].rearrange("j p t -> p j t"), sB[:], [sB], [S["uTb"]])


def phase_s5(k, S, pa_d, pb_d, craw1_d, craw2_d, ngroups=64):
    with k.phase():
        BT1 = k.sb("s5_BT1", [128, 32, 128], BF16)
        BT2 = k.sb("s5_BT2", [128, 32, 128], BF16)
        M1 = k.sb("s5_M1", [128, 128, 16], BF16)
        M2 = k.sb("s5_M2", [128, 128, 16], BF16)
        rdec = k.sb("s5_rdec", [128, 128], F32)
        thn = k.sb("s5_thn", [128, 128], F32)
        with k.phase():
            pa = k.sb("s5_pa", [128, 5, 2048], F32)
            k.dma("sp", pa[:], pa_d[:], [pa_d], [pa])
            T = [k.sb("s5_T%d" % i, [128, 2048], F32) for i in range(8)]
            Ti = k.sb("s5_Ti", [128, 2048], I32)
            lre, lim, ldt, bre, bim = [TT(pa[:, j, :], pa.res) for j in range(5)]
            dt, ang, mag, sn, cs, t5, t6, t7 = T
            def P(fn, r, w):
                k.op("pool", r, w, fn)
            k.op("act", [pa], [dt], lambda e: e.activation(dt[:], pa[:, 2, :], AF.Exp))
            P(lambda e: e.tensor_tensor(ang[:], pa[:, 1, :], dt[:], ALU.mult), [pa, dt], [ang])
            P(lambda e: e.tensor_scalar_mul(ang[:], ang[:], 1.0 / TWO_PI), [ang], [ang])
            P(lambda e: e.tensor_tensor(mag[:], pa[:, 0, :], dt[:], ALU.mult), [pa, dt], [mag])
            k.op("act", [mag], [mag], lambda e: e.activation(mag[:], mag[:], AF.Exp))
            sincos_small(k, "pool", ang, sn, cs, Ti, t5)
            P(lambda e: e.tensor_tensor(cs[:], cs[:], mag[:], ALU.mult), [cs, mag], [cs])
            P(lambda e: e.tensor_scalar_add(cs[:], cs[:], -1.0), [cs], [cs])
            P(lambda e: e.tensor_tensor(sn[:], sn[:], mag[:], ALU.mult), [sn, mag], [sn])
            P(lambda e: e.tensor_tensor(t5[:], pa[:, 0, :], pa[:, 0, :], ALU.mult), [pa], [t5])
            P(lambda e: e.tensor_tensor(t6[:], pa[:, 1, :], pa[:, 1, :], ALU.mult), [pa], [t6])
            P(lambda e: e.tensor_tensor(t5[:], t5[:], t6[:], ALU.add), [t5, t6], [t5])
            k.op("dve", [t5], [t5], lambda e: e.reciprocal(t5[:], t5[:]))
            P(lambda e: e.tensor_tensor(t6[:], cs[:], pa[:, 0, :], ALU.mult), [cs, pa], [t6])
            P(lambda e: e.tensor_tensor(t7[:], sn[:], pa[:, 1, :], ALU.mult), [sn, pa], [t7])
            P(lambda e: e.tensor_tensor(t6[:], t6[:], t7[:], ALU.add), [t6, t7], [t6])
            P(lambda e: e.tensor_tensor(t6[:], t6[:], t5[:], ALU.mult), [t6, t5], [t6])
            P(lambda e: e.tensor_tensor(t7[:], sn[:], pa[:, 0, :], ALU.mult), [sn, pa], [t7])
            P(lambda e: e.tensor_tensor(mag[:], cs[:], pa[:, 1, :], ALU.mult), [cs, pa], [mag])
            P(lambda e: e.tensor_tensor(t7[:], t7[:], mag[:], ALU.subtract), [t7, mag], [t7])
            P(lambda e: e.tensor_tensor(t7[:], t7[:], t5[:], ALU.mult), [t7, t5], [t7])
            P(lambda e: e.tensor_tensor(dt[:], t6[:], pa[:, 3, :], ALU.mult), [t6, pa], [dt])
            P(lambda e: e.tensor_tensor(mag[:], t7[:], pa[:, 4, :], ALU.mult), [t7, pa], [mag])
            P(lambda e: e.tensor_tensor(dt[:], dt[:], mag[:], ALU.subtract), [dt, mag], [dt])
            P(lambda e: e.tensor_tensor(ang[:], t6[:], pa[:, 4, :], ALU.mult), [t6, pa], [ang])
            P(lambda e: e.tensor_tensor(mag[:], t7[:], pa[:, 3, :], ALU.mult), [t7, pa], [mag])
            P(lambda e: e.tensor_tensor(ang[:], ang[:], mag[:], ALU.add), [ang, mag], [ang])
            bre_v = dt[:].rearrange("p (a b) -> p a b", b=64)
            bim_v = ang[:].rearrange("p (a b) -> p a b", b=64)
            k.op("dve", [dt], [BT1], lambda e: e.tensor_copy(BT1[:, :, 0:64], bre_v))
            k.op("dve", [ang], [BT1], lambda e: e.tensor_copy(BT1[:, :, 64:128], bim_v))
            k.op("dve", [ang], [BT2], lambda e: e.tensor_copy(BT2[:, :, 0:64], bim_v))
            k.op("dve", [dt], [BT2], lambda e: e.tensor_scalar_mul(BT2[:, :, 64:128], bre_v, -1.0))
            pb = k.sb("s5_pb", [128, 3, 128], F32)
            k.dma("sp", pb[:], pb_d[:], [pb_d], [pb])
            k.op("act", [pb], [thn], lambda e: e.activation(thn[:], pb[:, 2, :], AF.Exp))
            k.op("dve", [pb, thn], [rdec], lambda e: e.tensor_tensor(rdec[:], pb[:, 0, :], thn[:], ALU.mult))
            k.op("act", [rdec], [rdec], lambda e: e.activation(rdec[:], rdec[:], AF.Exp))
            k.op("dve", [pb, thn], [thn], lambda e: e.tensor_tensor(thn[:], pb[:, 1, :], thn[:], ALU.mult))
            k.op("dve", [thn], [thn], lambda e: e.tensor_scalar_mul(thn[:], thn[:], 1.0 / TWO_PI))
            cr1 = T[0]; cr2 = T[1]
            k.dma("sp", cr1[:], craw1_d[:].rearrange("p a b -> p (a b)"), [craw1_d], [cr1])
            k.dma("sp", cr2[:], craw2_d[:].rearrange("p a b -> p (a b)"), [craw2_d], [cr2])
            M1f = M1[:].rearrange("p a b -> p (a b)")
            M2f = M2[:].rearrange("p a b -> p (a b)")
            k.op("dve", [cr1], [M1], lambda e: e.tensor_copy(M1f[0:64, :], cr1[0:64, :]))
            k.op("dve", [cr1], [M1], lambda e: e.tensor_scalar_mul(M1f[64:128, :], cr1[64:128, :], -1.0))
            k.op("dve", [cr2], [M2], lambda e: e.tensor_scalar_mul(M2f, cr2[:], -1.0))
        BT1z = k.sb("s5_BT1z", [128, 32, 128], BF16)
        BT2z = k.sb("s5_BT2z", [128, 32, 128], BF16)
        k.op("dve", [BT1], [BT1z], lambda e: e.tensor_copy(BT1z[64:128], BT1[64:128]))
        k.op("dve", [BT2], [BT2z], lambda e: e.tensor_copy(BT2z[64:128], BT2[64:128]))
        k.op("dve", [], [BT1z], lambda e: e.memset(BT1z[64:96], 0.0))
        k.op("dve", [], [BT2z], lambda e: e.memset(BT2z[64:96], 0.0))
        PI_SC = 3.14159
        MAGIC = 12582912.0
        iot_i = k.sb("s5_ioti", [128, NT], I32)
        k.op("pool", [], [iot_i], lambda e: e.iota(iot_i[:], pattern=[[1, NT]], base=0, channel_multiplier=0))
        ones = k.sb("s5_ones", [128, 512], F32)
        k.op("dve", [], [ones], lambda e: e.memset(ones[:], 1.0))
        ND = 4
        uT = [k.sb("s5_uT%d" % i, [128, NT], BF16) for i in range(2)]
        frq = [k.sb("s5_fr%d" % i, [128, 512], F32) for i in range(ND)]
        kiq = [k.sb("s5_ki%d" % i, [128, 512], I32) for i in range(2)]
        S2q = [k.sb("s5_S2%d" % i, [128, 512], F32) for i in range(ND)]
        C2q = [k.sb("s5_C2%d" % i, [128, 512], F32) for i in range(ND)]
        shq = [k.sb("s5_sh%d" % i, [128, 512], F32) for i in range(2)]
        bq = [k.sb("s5_bq%d" % i, [128, 512], F32) for i in range(3)]
        tq = [k.sb("s5_tq%d" % i, [128, 512], F32) for i in range(3)]
        wq = [k.sb("s5_w%d" % i, [128, 512], F32) for i in range(3)]
        z1q = [k.sb("s5_z1%d" % i, [128, 512], BF16) for i in range(3)]
        z2q = [k.sb("s5_z2%d" % i, [128, 512], BF16) for i in range(3)]
        Rtq = [k.sb("s5_Rt%d" % i, [128, 512], F32) for i in range(2)]
        y8 = [k.sb("s5_y8%d" % i, [128, NTILE, 128], F32) for i in range(2)]
        pp1 = [k.ps("s5_p1%d" % i, [128, 512], F32) for i in range(2)]
        pp2 = [k.ps("s5_p2%d" % i, [128, 512], F32) for i in range(2)]
        psy = [k.ps("s5_py%d" % i, [128, 512], F32) for i in range(4)]
        items = []
        for d in range(2):
            for g in range(ngroups):
                for bi, (t0, tn) in enumerate(TBLKS):
                    items.append((d, g, bi, t0, tn))
        NI = len(items)

        def stage_T(i):
            d, g, bi, t0, tn = items[i]
            dg = d * 64 + g
            fr_, ki_, S_, C_, sh_ = frq[i % ND], kiq[i % 2], S2q[i % ND], C2q[i % ND], shq[i % 2]
            if bi == 0:
                ch, q = g // 4, g % 4
                if q == 0:
                    usrc = S["uTf"] if d == 0 else S["uTb"]
                    ut = uT[(d * 16 + ch) % 2]
                    k.dma("sp", ut[:], usrc[ch], [usrc], [ut])
                Rt_ = Rtq[dg % 2]
                k.op("dve", [ones, rdec], [Rt_], lambda e: e.tensor_scalar_mul(Rt_[:], ones[:], rdec[:, dg:dg + 1]))
            k.op("act", [iot_i, thn], [fr_], lambda e: e.activation(fr_[:, 0:tn], iot_i[:, t0:t0 + tn], AF.Copy, scale=thn[:, dg:dg + 1]))
            kf_ = ki_[:].bitcast(F32)
            k.op("act", [fr_], [ki_], lambda e: e.activation(kf_[:, 0:tn], fr_[:, 0:tn], AF.Copy, bias=MAGIC))
            k.op("dve", [fr_, ki_], [fr_], lambda e: e.scalar_tensor_tensor(fr_[:, 0:tn], kf_[:, 0:tn], MAGIC, fr_[:, 0:tn], ALU.subtract, ALU.subtract))
            k.op("act", [fr_], [S_], lambda e: e.activation(S_[:, 0:tn], fr_[:, 0:tn], AF.Sin, scale=-SIN_SC))
            k.op("act", [fr_], [sh_], lambda e: e.activation(sh_[:, 0:tn], fr_[:, 0:tn], AF.Sin, scale=PI_SC))
            k.op("act", [sh_], [sh_], lambda e: e.activation(sh_[:, 0:tn], sh_[:, 0:tn], AF.Square))
            k.op("act", [sh_], [C_], lambda e: e.activation(C_[:, 0:tn], sh_[:, 0:tn], AF.Copy, scale=-2.0, bias=1.0))

        def stage_A(i):
            d, g, bi, t0, tn = items[i]
            ch, q = g // 4, g % 4
            dc = d * 16 + ch
            ut = uT[dc % 2]
            p1, p2, b_, t_ = pp1[i % 2], pp2[i % 2], bq[i % 3], tq[i % 3]
            S_, C_ = S2q[i % ND], C2q[i % ND]
            if q < 3:
                ps_ = slice(32 * q, 32 * q + 32)
                k.op("pe", [BT1, ut], [p1], lambda e: e.matmul(p1[:, 0:tn], BT1[ps_, dc, :], ut[ps_, t0:t0 + tn], start=True, stop=True))
                k.op("pe", [BT2, ut], [p2], lambda e: e.matmul(p2[:, 0:tn], BT2[ps_, dc, :], ut[ps_, t0:t0 + tn], start=True, stop=True))
            else:
                k.op("pe", [BT1z, ut], [p1], lambda e: e.matmul(p1[:, 0:tn], BT1z[64:128, dc, :], ut[64:128, t0:t0 + tn], start=True, stop=True))
                k.op("pe", [BT2z, ut], [p2], lambda e: e.matmul(p2[:, 0:tn], BT2z[64:128, dc, :], ut[64:128, t0:t0 + tn], start=True, stop=True))
            k.op("dve", [p1, C_], [b_], lambda e: e.tensor_tensor(b_[:, 0:tn], p1[:, 0:tn], C_[:, 0:tn], ALU.mult))
            k.op("dve", [p2, S_], [t_], lambda e: e.tensor_tensor(t_[:, 0:tn], p2[:, 0:tn], S_[:, 0:tn], ALU.mult))
            k.op("pool", [b_, t_], [b_], lambda e: e.tensor_tensor(b_[:, 0:tn], b_[:, 0:tn], t_[:, 0:tn], ALU.add))

        def stage_B(i):
            d, g, bi, t0, tn = items[i]
            dg = d * 64 + g
            b_, w_, z1_, z2_ = bq[i % 3], wq[i % 3], z1q[i % 3], z2q[i % 3]
            S_, C_ = S2q[i % ND], C2q[i % ND]
            Rt_ = Rtq[dg % 2]
            if bi == 0:
                init = 0.0
                rd = [Rt_, b_]
            else:
                wp = wq[(i - 1) % 3]
                ptn = items[i - 1][4]
                init = wp[:, ptn - 1:ptn]
                rd = [Rt_, b_, wp]
            k.op("dve", rd, [w_], lambda e: e.tensor_tensor_scan(w_[:, 0:tn], Rt_[:, 0:tn], b_[:, 0:tn], init, ALU.mult, ALU.add))
            k.op("dve", [w_, C_], [z1_], lambda e: e.tensor_tensor(z1_[:, 0:tn], w_[:, 0:tn], C_[:, 0:tn], ALU.mult))
            k.op("pool", [w_, S_], [z2_], lambda e: e.tensor_tensor(z2_[:, 0:tn], w_[:, 0:tn], S_[:, 0:tn], ALU.mult))
            pys = psy[(dg % 2) * 2:(dg % 2) * 2 + 2]
            for tt in range(tn // 128):
                ti = t0 // 128 + tt
                py = pys[ti // 17]
                cc = (ti % 17) * 16
                k.op("pe", [z1_, M1], [py], lambda e: e.matmul(py[:, cc:cc + 16], z1_[:, tt * 128:(tt + 1) * 128], M1[:, dg, :], start=True, stop=False))
                k.op("pe", [z2_, M2], [py], lambda e: e.matmul(py[:, cc:cc + 16], z2_[:, tt * 128:(tt + 1) * 128], M2[:, dg, :], start=False, stop=True))
            if bi == len(TBLKS) - 1:
                yb = y8[(dg // 8) % 2]
                for half in range(2):
                    py = pys[half]
                    k.op("act", [py], [yb], lambda e: e.activation(yb[:, half * 17:(half + 1) * 17, (g % 8) * 16:(g % 8 + 1) * 16],
                                                                   py[:, 0:272].rearrange("p (t c) -> p t c", c=16), AF.Copy))
                if g % 8 == 7:
                    ydst = S["yF"] if d == 0 else S["yB"]
                    g0 = (g // 8) * 128
                    k.dma("pool", ydst[:, g0:g0 + 128].rearrange("(t p) c -> p t c", p=128), yb[:], [yb], [ydst])

        for i in range(NI + 2):
            if i < NI:
                stage_T(i)
            if 0 <= i - 1 < NI:
                stage_A(i - 1)
            if 0 <= i - 2 < NI:
                stage_B(i - 2)


def phase_glu(k, S, d_skip, w_glu, b_glu, idf, jf, idb, ntiles=32, gtabs=None):
    with k.phase():
        wg = k.sb("gu_wg", [128, 8, 1024], BF16)
        stg = [k.sb("gu_stg%d" % i, [128, 8, 128], F32) for i in range(2)]
        for cb in range(8):
            st = stg[cb % 2]
            k.dma("sp", st[:], w_glu[:, cb * 128:(cb + 1) * 128].rearrange("(j p) c -> p j c", p=128), [], [st])
            k.op("act", [st], [wg], lambda e: e.activation(wg[:, :, cb * 128:(cb + 1) * 128], st[:], AF.Copy))
        dsk = k.sb("gu_d", [128, 1024], F32)
        load_bc(k, "sp", dsk, d_skip[:], [d_skip])
        bg = k.sb("gu_bg", [1, 1024], F32)
        k.dma("sp", bg[:], b_glu[:], [b_glu], [bg])
        ones = k.sb("gu_ones", [1, 128], BF16)
        k.op("dve", [], [ones], lambda e: e.memset(ones[:], 1.0))
        bgb = k.sb("gu_bgb", [1, 1024], BF16)
        k.op("dve", [bg], [bgb], lambda e: e.tensor_copy(bgb[:], bg[:]))
        ggT = k.sb("gu_ggT", [128, 8, 4096], BF16)
        yf = [k.sb("gu_yf%d" % i, [128, 1024], F32) for i in range(2)]
        yb = [k.sb("gu_yb%d" % i, [128, 1024], F32) for i in range(2)]
        uu = [k.sb("gu_u%d" % i, [128, 1024], F32) for i in range(2)]
        t2 = [k.sb("gu_t%d" % i, [128, 1024], F32) for i in range(2)]
        gb = [k.sb("gu_gb%d" % i, [128, 1024], BF16) for i in range(2)]
        gT = [k.sb("gu_gT%d" % i, [128, 8, 128], BF16) for i in range(2)]
        psa = [k.ps("gu_psa%d" % i, [128, 512], F32) for i in range(2)]
        pst = [k.ps("gu_pst%d" % i, [128, 1024], BF16) for i in range(2)]
        psz = [k.ps("gu_psz%d" % i, [128, 512], F32) for i in range(2)]
        for i in range(ntiles):
            u = i % 2
            f_, b_, u_, t_, g_, gt_ = yf[u], yb[u], uu[u], t2[u], gb[u], gT[u]
            rows = slice(i * 128, (i + 1) * 128)
            if gtabs is None:
                k.dma("sp", f_[:], S["yF"][256 + i * 128:256 + (i + 1) * 128, :], [S["yF"]], [f_])
                r0 = NT - (i + 1) * 128
                k.dma("sp", b_[:], S["yB"][r0:r0 + 128, :], [S["yB"]], [b_])
                k.dma("sp", u_[:], S["u_tm"][rows, :], [S["u_tm"]], [u_])
            else:
                gF, gB, gN = gtabs
                k.idma_gather(f_[:], S["yF"][:], gF[:, i:i + 1], [S["yF"], gF], [f_])
                k.idma_gather(b_[:], S["yB"][:], gB[:, i:i + 1], [S["yB"], gB], [b_])
                k.idma_gather(u_[:], S["u_tm"][:], gN[:, i:i + 1], [S["u_tm"], gN], [u_])
            for n in range(2):
                k.op("pe", [f_, idf], [psa[n]], lambda e: e.matmul(psa[n][:], idf[:], f_[:, n * 512:(n + 1) * 512], start=True, stop=False))
                k.op("pe", [b_, jf], [psa[n]], lambda e: e.matmul(psa[n][:], jf[:], b_[:, n * 512:(n + 1) * 512], start=False, stop=True))
            k.op("pool", [u_, dsk], [u_], lambda e: e.tensor_tensor(u_[:], u_[:], dsk[:], ALU.mult))
            for n in range(2):
                k.op("dve", [psa[n], u_], [t_], lambda e: e.tensor_tensor(t_[:, n * 512:(n + 1) * 512], psa[n][:], u_[:, n * 512:(n + 1) * 512], ALU.add))
            k.op("pool", [t_], [f_], lambda e: e.tensor_tensor(f_[:], t_[:], t_[:], ALU.mult))
            k.op("dve", [f_], [f_], lambda e: e.tensor_scalar(f_[:], f_[:], 0.044715, 1.0, ALU.mult, ALU.add))
            k.op("pool", [f_, t_], [f_], lambda e: e.tensor_tensor(f_[:], f_[:], t_[:], ALU.mult))
            k.op("act", [f_], [f_], lambda e: e.activation(f_[:], f_[:], AF.Sigmoid, scale=1.5957691216057308))
            k.op("dve", [f_, t_], [t_], lambda e: e.tensor_tensor(t_[:], t_[:], f_[:], ALU.mult))
            k.op("pool", [t_], [g_], lambda e: e.tensor_copy(g_[:], t_[:]))
            for j in range(8):
                k.op("pe", [g_, idb], [pst[u]], lambda e: e.transpose(pst[u][:, j * 128:(j + 1) * 128], g_[:, j * 128:(j + 1) * 128], idb[:]))
            k.op("act", [pst[u]], [gt_], lambda e: e.activation(gt_[:], pst[u][:].rearrange("p (j t) -> p j t", j=8), AF.Copy))
            for n in range(2):
                for j in range(8):
                    k.op("pe", [gt_, wg], [psz[n]], lambda e: e.matmul(psz[n][:], gt_[:, j, :], wg[:, j, n * 512:(n + 1) * 512], start=(j == 0), stop=False))
                k.op("pe", [ones, bgb], [psz[n]], lambda e: e.matmul(psz[n][:], ones[0:1, :], bgb[0:1, n * 512:(n + 1) * 512], start=False, stop=True))
                k.op("act", [psz[n]], [f_], lambda e: e.activation(f_[:, n * 512:(n + 1) * 512], psz[n][:], AF.Sigmoid))
            k.op("dve", [f_, t_], [g_], lambda e: e.tensor_tensor(g_[:], t_[:], f_[:], ALU.mult))
            for j in range(8):
                k.op("pe", [g_, idb], [pst[u]], lambda e: e.transpose(pst[u][:, j * 128:(j + 1) * 128], g_[:, j * 128:(j + 1) * 128], idb[:]))
            k.op("act", [pst[u]], [ggT], lambda e: e.activation(ggT[:, :, i * 128:(i + 1) * 128], pst[u][:].rearrange("p (j t) -> p j t", j=8), AF.Copy))
        for j in range(8):
            k.dma("pool", S["ggT"][j, :, 0:ntiles * 128], ggT[:, j, 0:ntiles * 128], [ggT], [S["ggT"]])


def build_program():
    nc = bass.Bass("TRN2", target_bir_lowering=False)
    with ExitStack() as es:
        k = K(nc, es)
        dr = lambda n, s, dt=F32, kind="ExternalInput": k.dram(n, s, dt, kind=kind)
        xin = dr("xin", [NT, D])
        cT = dr("cT", [128, 32]); identf = dr("identf", [128, 128]); jmatf = dr("jmatf", [128, 128])
        ada_w = dr("ada_w", [2, D, 6 * D]); ada_b = dr("ada_b", [2, 6 * D])
        ev_w_in = dr("ev_w_in", [D, 6176]); gate_w2 = dr("gate_w2", [2, 16, 512]); gate_b = dr("gate_b", [2, 512])
        norm_g = dr("norm_g", [1, 256]); ropet = dr("ropet", [4096, 2, 256]); glac = dr("glac", [2, 3, 128, 128])
        biasg = dr("biasg", [8, 64, 8, 512]); ev_w_out = dr("ev_w_out", [D, D])
        ln_mix_g = dr("ln_mix_g", [2, D]); ln_mix_b = dr("ln_mix_b", [2, D]); ln_ffn_g = dr("ln_ffn_g", [2, D]); ln_ffn_b = dr("ln_ffn_b", [2, D])
        router_w = dr("router_w", [D, 16]); router_b = dr("router_b", [1, 16])
        w_gate = dr("w_gate", [2, 16, D, 1024]); w_up = dr("w_up", [2, 16, D, 1024]); w_down = dr("w_down", [2, 16, 1024, D])
        od_w_in = dr("od_w_in", [D, 1024]); od_w_glu = dr("od_w_glu", [1024, 1024]); od_b_glu = dr("od_b_glu", [1, 1024])
        od_w_out = dr("od_w_out", [1024, D]); od_d = dr("od_d", [1, 1024])
        s5pa = dr("s5pa", [128, 5, 2048]); s5pb = dr("s5pb", [128, 3, 128]); s5c1 = dr("s5c1", [128, 128, 16]); s5c2 = dr("s5c2", [128, 128, 16])
        yout = dr("yout", [2048, D], kind="ExternalOutput")
        idxtab = dr("idxtab", [128, 48], mybir.dt.uint32)
        xloc = dr("xloc", [2048, D], kind="Internal")
        modrow = dr("modrow", [2, 2, 6 * D], kind="Internal")
        xall = dr("xall", [NT, D], kind="Internal")
        S = {}
        for n, s, dt in (("qaT", [8, 128, NT], BF16), ("kaT", [8, 128, NT], BF16), ("va", [NT, 1024], BF16), ("qb", [NT, 512], F32),
                         ("kb", [NT, 512], F32), ("qbr", [NT, 512], F32), ("kbr", [NT, 512], F32), ("sp", [2, NT, 512], F32),
                         ("ob", [2, NT, 1024], F32), ("vb", [NT, 1024], BF16), ("rb", [NT, 1024], F32), ("lrT", [2, 16, NT], F32),
                         ("mixT", [16, 128, NT], BF16), ("hT_moe", [NTILE, 128, 16, 128], BF16), ("comb", [NT, 16], F32),
                         ("u_tm", [4096, 1024], F32), ("uTf", [16, 128, NT], BF16), ("uTb", [16, 128, NT], BF16),
                         ("yF", [NT, 1024], F32), ("yB", [NT, 1024], F32), ("ggT", [8, 128, NT], BF16)):
            S[n] = dr(n, s, dt, kind="Internal")
        idf = k.sb("idf", [128, 128], F32); idb = k.sb("idb", [128, 128], BF16)
        jf = k.sb("jf", [128, 128], F32); jb = k.sb("jb", [128, 128], BF16)
        k.dma("sp", idf[:], identf[:], [identf], [idf])
        k.dma("sp", jf[:], jmatf[:], [jmatf], [jf])
        k.op("dve", [idf], [idb], lambda e: e.tensor_copy(idb[:], idf[:]))
        k.op("dve", [jf], [jb], lambda e: e.tensor_copy(jb[:], jf[:]))
        for i in range(NTILE):
            k.dma("sp", xall[i * 128:(i + 1) * 128, :], xin[i * 128:(i + 1) * 128, :], [xin], [xall])
        phase_mod(k, 0, ada_w, ada_b, cT, modrow)
        with k.phase():
            hT_all = k.sb("hT_all", [128, 16, NT], BF16)
            build_hT(k, 0, xall, modrow, hT_all, idb, 0, 1)
            phase_inproj0(k, hT_all, ev_w_in, S)
        phase_na(k, S, biasg, idb)
        phase_gla_prep(k, S, gate_w2, gate_b, ropet)
        phase_gla(k, S, glac, idf)
        phase_gatenorm(k, S, norm_g, idb)
        import os
        STOP = int(os.environ.get("MK_STOP", "99"))
        SKIP0 = int(os.environ.get("MK_SKIP0", "0"))
        post_mixer(k, 0, 16, S["mixT"], ev_w_out, xall, modrow, ln_mix_g, ln_mix_b, router_w, router_b, S, idf, NTILE)
        if STOP == 1:
            k.finish(); return nc
        phase_moe(k, 0, [9, 9, 8, 8] if STOP > 2 else [2], w_gate, w_up, w_down, xall, modrow, ln_ffn_g, ln_ffn_b, S, xall)
        if STOP == 2:
            k.finish(); return nc
        phase_mod(k, 1, ada_w, ada_b, cT, modrow)
        with k.phase():
            hT_all = k.sb("hT_all1", [128, 16, NT], BF16)
            build_hT(k, 1, xall, modrow, hT_all, idb, 0, 1)
            phase_l1_inproj(k, hT_all, od_w_in, S, idb, jb)
        if STOP == 3:
            k.finish(); return nc
        phase_s5(k, S, s5pa, s5pb, s5c1, s5c2)
        if STOP == 4:
            k.finish(); return nc
        gidx = k.sb("gidx", [128, 48], mybir.dt.uint32)
        k.dma("sp", gidx[:], idxtab[:], [idxtab], [gidx])
        gN = TT(gidx[:, 0:16], gidx.res); gF = TT(gidx[:, 16:32], gidx.res); gB = TT(gidx[:, 32:48], gidx.res)
        phase_glu(k, S, od_d, od_w_glu, od_b_glu, idf, jf, idb, ntiles=16, gtabs=(gF, gB, gN))
        if STOP == 5:
            k.finish(); return nc
        post_mixer(k, 1, 8, S["ggT"], od_w_out, xall, modrow, ln_mix_g, ln_mix_b, router_w, router_b, S, idf, 16, xg=gN, xout=xloc)
        phase_moe(k, 1, [8, 8], w_gate, w_up, w_down, xloc, modrow, ln_ffn_g, ln_ffn_b, S, yout)
        k.finish()
    return nc


def kernel(x, c, ctx, c_ctx, ada_w, ada_b, ln_mix_g, ln_mix_b, ln_ffn_g, ln_ffn_b,
           ev_w_in, ev_gate_w2, ev_gate_b, ev_rpb, ev_norm_g, ev_w_out,
           od_w_in, od_lam_re, od_lam_im, od_log_dt, od_b_re, od_b_im, od_c_re, od_c_im, od_d,
           od_w_glu, od_b_glu, od_w_out, router_w, router_b, moe_w_gate, moe_w_up, moe_w_down):
    import os
    f = lambda a: np.ascontiguousarray(np.asarray(a, dtype=np.float32))
    x, c, ctx, c_ctx = f(x), f(c), f(ctx), f(c_ctx)
    pa, pb, c1, c2 = s5_host_params(f(od_lam_re)[0], f(od_lam_im)[0], f(od_log_dt)[0], f(od_b_re)[0], f(od_b_im)[0], f(od_c_re)[0], f(od_c_im)[0])
    shared = {"identf": np.eye(128, dtype=np.float32), "jmatf": np.ascontiguousarray(np.eye(128, dtype=np.float32)[::-1]),
              "ada_w": f(ada_w), "ada_b": f(ada_b), "ev_w_in": f(ev_w_in)[0],
              "gate_w2": f(ev_gate_w2)[0], "gate_b": f(ev_gate_b)[0], "norm_g": f(ev_norm_g)[0][None], "ropet": rope_tables_host(), "glac": gla_consts_host(),
              "biasg": na_bias_host(f(ev_rpb)[0]), "ev_w_out": f(ev_w_out)[0], "ln_mix_g": f(ln_mix_g), "ln_mix_b": f(ln_mix_b),
              "ln_ffn_g": f(ln_ffn_g), "ln_ffn_b": f(ln_ffn_b), "router_w": f(router_w), "router_b": f(router_b)[None],
              "w_gate": f(moe_w_gate), "w_up": f(moe_w_up), "w_down": f(moe_w_down),
              "od_w_in": f(od_w_in)[0], "od_w_glu": f(od_w_glu)[0], "od_b_glu": f(od_b_glu)[0][None], "od_w_out": f(od_w_out)[0],
              "od_d": f(od_d)[0].reshape(1, 1024), "s5pa": pa, "s5pb": pb, "s5c1": c1, "s5c2": c2}
    ncores = int(os.environ.get("MK_NCORES", "8"))
    in_maps = []
    for core in range(ncores):
        b = core % 4
        m = dict(shared)
        m["xin"] = np.ascontiguousarray(np.concatenate([x[b], ctx[b]], 0))
        m["cT"] = np.ascontiguousarray(np.concatenate([c[b].reshape(16, 128).T, c_ctx.reshape(16, 128).T], 1))
        half = core // 4
        gi = (half * 16 + np.arange(16))[None, :]
        pp = np.arange(128)[:, None]
        m["idxtab"] = np.ascontiguousarray(np.concatenate([gi * 128 + pp, 256 + gi * 128 + pp, NT - (gi + 1) * 128 + pp], 1).astype(np.uint32))
        in_maps.append(m)
    nc = build_program()
    res = run_bass_kernel_spmd(nc, in_maps, core_ids=list(range(ncores)))
    out = np.zeros((4, 4096, D), np.float32)
    for core in range(ncores):
        b, half = core % 4, core // 4
        out[b, half * 2048:(half + 1) * 2048] = res.results[core]["yout"]
    return out
```
